# Optimizing a Trainium2 kernel written in Bass

```python
import jax, jax.numpy as jnp
from jax import lax
import numpy as np

D_MODEL = 1024
BATCH = 16
SEQ = 2048
DEPTH = 2

CHUNK = 64
N_PREV_CHUNKS = 8
BAND = (N_PREV_CHUNKS + 1) * CHUNK
HEAD_DIM = 64
N_HEADS_ATTN = 8
N_HEADS_RWKV = 8
D_ATTN = N_HEADS_ATTN * HEAD_DIM
D_RWKV = N_HEADS_RWKV * HEAD_DIM
REL_CLIP = 128
DECAY_LORA = 64
AAA_LORA = 64
MV_LORA = 32
GATE_LORA = 160
N_GROUPS = 4
EXPERTS_PER_GROUP = 8
N_EXPERTS = N_GROUPS * EXPERTS_PER_GROUP
TOP_K_IN_GROUP = 2
D_EXPERT = 256
D_PLE = 256
DEEPNORM_ALPHA = (2 * DEPTH) ** 0.25
DEEPNORM_BETA = (8 * DEPTH) ** -0.25
LN_EPS = 1e-5
GN_EPS = 64e-5
NEG_INF = -1e30

Q0 = 0
K0 = D_ATTN
V0 = 2 * D_ATTN
RW0 = 3 * D_ATTN
RW_COLS = 3 * D_RWKV + DECAY_LORA + AAA_LORA + GATE_LORA
GATE0 = RW0 + RW_COLS
N_IN = GATE0 + 2 * D_MODEL

kernel_name = "hybrid_chunkattn_rwkv7_hiermoe_deepnorm"


def layer_norm(x, g, b):
    x32 = x.astype(jnp.float32)
    mu = jnp.mean(x32, axis=-1, keepdims=True)
    var = jnp.mean(jnp.square(x32 - mu), axis=-1, keepdims=True)
    y = (x32 - mu) * lax.rsqrt(var + LN_EPS)
    return (y * g.astype(jnp.float32) + b.astype(jnp.float32)).astype(x.dtype)


def chunked_band_attention(q, k, v, rel_bias):
    B, S, _ = q.shape
    nc = S // CHUNK
    q = q.reshape(B, nc, CHUNK, N_HEADS_ATTN, HEAD_DIM)
    pad = ((0, 0), (N_PREV_CHUNKS * CHUNK, 0), (0, 0))
    kp = jnp.pad(k, pad).reshape(B, nc + N_PREV_CHUNKS, CHUNK, N_HEADS_ATTN, HEAD_DIM)
    vp = jnp.pad(v, pad).reshape(B, nc + N_PREV_CHUNKS, CHUNK, N_HEADS_ATTN, HEAD_DIM)
    kb = jnp.concatenate([kp[:, j:j + nc] for j in range(N_PREV_CHUNKS + 1)], axis=2)
    vb = jnp.concatenate([vp[:, j:j + nc] for j in range(N_PREV_CHUNKS + 1)], axis=2)
    scores = jnp.einsum("bcqhd,bckhd->bhcqk", q, kb).astype(jnp.float32) * (HEAD_DIM ** -0.5)
    qi = jnp.arange(CHUNK)[:, None]
    kj = jnp.arange(BAND)[None, :]
    dist = qi - (kj - N_PREV_CHUNKS * CHUNK)
    idx = jnp.clip(dist, -REL_CLIP, REL_CLIP) + REL_CLIP
    bias = rel_bias[:, idx].astype(jnp.float32)
    key_chunk = jnp.arange(nc)[:, None] - N_PREV_CHUNKS + (jnp.arange(BAND) // CHUNK)[None, :]
    valid = key_chunk >= 0
    scores = jnp.where(valid[None, None, :, None, :], scores + bias[None, :, None], NEG_INF)
    prob = jax.nn.softmax(scores, axis=-1).astype(v.dtype)
    out = jnp.einsum("bhcqk,bckhd->bcqhd", prob, vb)
    return out.reshape(B, S, D_ATTN)


def rwkv7_scan(r, w, k, v, a, b):
    B, S, H, N = r.shape
    xs = tuple(jnp.swapaxes(t.astype(jnp.float32), 0, 1) for t in (r, w, k, v, a, b))

    def step(state, inp):
        rt, wt, kt, vt, at, bt = inp
        sa = jnp.einsum("bhij,bhj->bhi", state, at)
        state = state * wt[:, :, None, :] + sa[..., None] * bt[:, :, None, :] + vt[..., None] * kt[:, :, None, :]
        return state, jnp.einsum("bhij,bhj->bhi", state, rt)

    state0 = jnp.zeros((B, H, N, N), jnp.float32)
    _, ys = lax.scan(step, state0, xs)
    return jnp.swapaxes(ys, 0, 1)


def rwkv7_time_mix(cols, mu, v_first, decay_base, decay_up, aaa_base, aaa_up, gate_up,
                   k_k, k_a, r_k, gn_g, gn_b, vres):
    B, S, _ = cols.shape
    prev = jnp.pad(cols, ((0, 0), (1, 0), (0, 0)))[:, :-1]
    cols = cols + (prev - cols) * mu
    splits = [D_RWKV, 2 * D_RWKV, 3 * D_RWKV, 3 * D_RWKV + DECAY_LORA, 3 * D_RWKV + DECAY_LORA + AAA_LORA]
    r, k, v, wd, ad, gd = jnp.split(cols, splits, axis=-1)
    w_log = -jax.nn.softplus(-(decay_base + jnp.tanh(wd) @ decay_up)) - 0.5
    decay = jnp.exp(-jnp.exp(w_log.astype(jnp.float32)))
    a = jax.nn.sigmoid(aaa_base + ad @ aaa_up)
    g = jax.nn.sigmoid(gd) @ gate_up
    heads = lambda t: t.reshape(B, S, N_HEADS_RWKV, HEAD_DIM)
    kk = heads(k * k_k).astype(jnp.float32)
    kk = kk * lax.rsqrt(jnp.maximum(jnp.sum(kk * kk, axis=-1, keepdims=True), 1e-24))
    k = k * (1.0 + (a - 1.0) * k_a)
    if vres is None:
        v_first = v
    else:
        vres_base, vres_down, vres_up = vres
        v = v + (v_first - v) * jax.nn.sigmoid(vres_base + (v @ vres_down) @ vres_up)
    r_h, k_h, v_h, a_h = heads(r), heads(k), heads(v), heads(a)
    y = rwkv7_scan(r_h, heads(decay), k_h, v_h, -kk, kk * a_h.astype(jnp.float32))
    m = jnp.mean(y, axis=-1, keepdims=True)
    var = jnp.mean(jnp.square(y - m), axis=-1, keepdims=True)
    yn = ((y - m) * lax.rsqrt(var + GN_EPS)).reshape(B, S, D_RWKV)
    yn = yn * gn_g.astype(jnp.float32) + gn_b.astype(jnp.float32)
    bonus = (jnp.sum(r_h * k_h * r_k, axis=-1, keepdims=True) * v_h).reshape(B, S, D_RWKV)
    out = (yn.astype(cols.dtype) + bonus) * g
    return out, v_first


def hier_moe(x, w_grp, b_grp, w_exp, b_exp, e_gate, e_up, e_down):
    B, S, D = x.shape
    T = B * S
    xt = x.reshape(T, D)
    glog = (xt @ w_grp + b_grp).astype(jnp.float32)
    gprob = jax.nn.softmax(glog, axis=-1)
    grp = jnp.argmax(glog, axis=-1)
    g_gate = jnp.take_along_axis(gprob, grp[:, None], axis=1)
    elog = (xt @ w_exp + b_exp).astype(jnp.float32).reshape(T, N_GROUPS, EXPERTS_PER_GROUP)
    sel = jnp.take_along_axis(elog, grp[:, None, None], axis=1)[:, 0]
    top_v, top_i = lax.top_k(sel, TOP_K_IN_GROUP)
    wk = jax.nn.softmax(top_v, axis=-1) * g_gate
    eid = grp[:, None] * EXPERTS_PER_GROUP + top_i
    comb = jnp.sum(jax.nn.one_hot(eid, N_EXPERTS, dtype=jnp.float32) * wk[..., None], axis=1).astype(x.dtype)
    out = jnp.zeros_like(xt)
    for e in range(N_EXPERTS):
        h = jax.nn.silu(xt @ e_gate[e]) * (xt @ e_up[e])
        out = out + (h * comb[:, e:e + 1]) @ e_down[e]
    return out.reshape(B, S, D)


def setup_inputs(seed: int = 0) -> dict:
    key = jax.random.key(seed)
    keys = iter(jax.random.split(key, 48))
    nrm = lambda shape, s: jax.random.normal(next(keys), shape, jnp.float32) * s
    uni = lambda shape, lo, hi: jax.random.uniform(next(keys), shape, jnp.float32, lo, hi)
    col_scale = jnp.ones((N_IN,), jnp.float32)
    col_scale = col_scale.at[V0:RW0].set(DEEPNORM_BETA)
    col_scale = col_scale.at[RW0 + 2 * D_RWKV:RW0 + 3 * D_RWKV].set(DEEPNORM_BETA)
    return {
        "x": nrm((BATCH, SEQ, D_MODEL), 1.0),
        "p": nrm((DEPTH, BATCH, SEQ, D_PLE), 1.0),
        "ln_in_g": 1.0 + nrm((D_MODEL,), 0.02),
        "ln_in_b": nrm((D_MODEL,), 0.02),
        "rel_bias": nrm((N_HEADS_ATTN, 2 * REL_CLIP + 1), 0.1),
        "w_in": nrm((DEPTH, D_MODEL, N_IN), D_MODEL ** -0.5) * col_scale,
        "tok_mix": uni((DEPTH, RW_COLS), 0.0, 1.0),
        "decay_base": uni((DEPTH, D_RWKV), -6.0, -1.0),
        "decay_up": nrm((DEPTH, DECAY_LORA, D_RWKV), DECAY_LORA ** -0.5),
        "aaa_base": nrm((DEPTH, D_RWKV), 0.1),
        "aaa_up": nrm((DEPTH, AAA_LORA, D_RWKV), AAA_LORA ** -0.5),
        "gate_up": nrm((DEPTH, GATE_LORA, D_RWKV), GATE_LORA ** -0.5),
        "k_k": 0.85 + nrm((DEPTH, D_RWKV), 0.02),
        "k_a": 1.0 + nrm((DEPTH, D_RWKV), 0.02),
        "r_k": nrm((DEPTH, N_HEADS_RWKV, HEAD_DIM), 0.1),
        "vres_base": nrm((DEPTH - 1, D_RWKV), 0.1),
        "vres_down": nrm((DEPTH - 1, D_RWKV, MV_LORA), D_RWKV ** -0.5),
        "vres_up": nrm((DEPTH - 1, MV_LORA, D_RWKV), MV_LORA ** -0.5),
        "gn_g": 1.0 + nrm((DEPTH, D_RWKV), 0.02),
        "gn_b": nrm((DEPTH, D_RWKV), 0.02),
        "w_branch_attn": nrm((DEPTH, D_ATTN, D_MODEL), D_ATTN ** -0.5 * DEEPNORM_BETA),
        "w_branch_rwkv": nrm((DEPTH, D_RWKV, D_MODEL), D_RWKV ** -0.5 * DEEPNORM_BETA),
        "w_out": nrm((DEPTH, D_MODEL, D_MODEL), D_MODEL ** -0.5 * DEEPNORM_BETA),
        "router_grp": nrm((DEPTH, D_MODEL, N_GROUPS), D_MODEL ** -0.5),
        "router_grp_bias": nrm((DEPTH, N_GROUPS), 0.01),
        "router_exp": nrm((DEPTH, D_MODEL, N_EXPERTS), D_MODEL ** -0.5),
        "router_exp_bias": nrm((DEPTH, N_EXPERTS), 0.01),
        "exp_gate": nrm((DEPTH, N_EXPERTS, D_MODEL, D_EXPERT), D_MODEL ** -0.5),
        "exp_up": nrm((DEPTH, N_EXPERTS, D_MODEL, D_EXPERT), D_MODEL ** -0.5),
        "exp_down": nrm((DEPTH, N_EXPERTS, D_EXPERT, D_MODEL), D_EXPERT ** -0.5 * DEEPNORM_BETA),
        "ple_proj": nrm((DEPTH, D_PLE, D_MODEL), D_PLE ** -0.5 * DEEPNORM_BETA),
        "ple_gate": nrm((DEPTH, D_MODEL, D_MODEL), D_MODEL ** -0.5),
        "ln_g": 1.0 + nrm((DEPTH, 3, D_MODEL), 0.02),
        "ln_b": nrm((DEPTH, 3, D_MODEL), 0.02),
    }


def reference(x, p, ln_in_g, ln_in_b, rel_bias, w_in, tok_mix, decay_base, decay_up,
              aaa_base, aaa_up, gate_up, k_k, k_a, r_k, vres_base, vres_down, vres_up,
              gn_g, gn_b, w_branch_attn, w_branch_rwkv, w_out, router_grp, router_grp_bias,
              router_exp, router_exp_bias, exp_gate, exp_up, exp_down, ple_proj, ple_gate,
              ln_g, ln_b):
    x = layer_norm(x, ln_in_g, ln_in_b)
    v_first = None
    for i in range(DEPTH):
        proj = x @ w_in[i]
        y_attn = chunked_band_attention(proj[..., Q0:K0], proj[..., K0:V0], proj[..., V0:RW0], rel_bias)
        vres = None if i == 0 else (vres_base[i - 1], vres_down[i - 1], vres_up[i - 1])
        y_rwkv, v_first = rwkv7_time_mix(proj[..., RW0:GATE0], tok_mix[i], v_first, decay_base[i],
                                         decay_up[i], aaa_base[i], aaa_up[i], gate_up[i], k_k[i],
                                         k_a[i], r_k[i], gn_g[i], gn_b[i], vres)
        gate_a = jax.nn.sigmoid(proj[..., GATE0:GATE0 + D_MODEL])
        gate_b = jax.nn.sigmoid(proj[..., GATE0 + D_MODEL:])
        mixed = (gate_a * (y_attn @ w_branch_attn[i]) + gate_b * (y_rwkv @ w_branch_rwkv[i])) @ w_out[i]
        x = layer_norm(DEEPNORM_ALPHA * x + mixed, ln_g[i, 0], ln_b[i, 0])
        moe = hier_moe(x, router_grp[i], router_grp_bias[i], router_exp[i], router_exp_bias[i],
                       exp_gate[i], exp_up[i], exp_down[i])
        x = layer_norm(DEEPNORM_ALPHA * x + moe, ln_g[i, 1], ln_b[i, 1])
        ple = (p[i] @ ple_proj[i]) * jax.nn.sigmoid(x @ ple_gate[i])
        x = layer_norm(DEEPNORM_ALPHA * x + ple, ln_g[i, 2], ln_b[i, 2])
    return x
```

```python
import contextlib
import numpy as np
import concourse.bass as bass
import concourse.mybir as mybir
from concourse.bass_utils import run_bass_kernel_spmd

F32 = mybir.dt.float32
BF16 = mybir.dt.bfloat16
AF = mybir.ActivationFunctionType
ALU = mybir.AluOpType

D = 1024
KC = 8
NIN = 5408
ALPHA = 4.0 ** 0.25
LN_EPS = 1e-5
GN_EPS = 64e-5
C0 = float(np.exp(-0.5))
N_DMA_SEMS = 40
SAME_ENGINE_SYNC = True
GRAN = 64
STOP = None
ABLK = None
BIS = 0


class Op:
    __slots__ = ("eng", "fn", "deps", "dma", "idx", "signal", "tick", "dsem", "dval", "prev_dval", "sw", "gen")

    def __init__(self, eng, fn, dma):
        self.eng = eng
        self.fn = fn
        self.dma = dma
        self.deps = set()
        self.signal = False
        self.tick = 0
        self.dsem = None
        self.dval = 0
        self.prev_dval = 0
        self.sw = None
        self.gen = 0


class Prog:
    ENGS = ("pe", "act", "dve", "pool", "sp")

    def __init__(self, nc):
        self.nc = nc
        self.ops = []
        self.res = {}
        self.ndma = 0
        self.swgen = {}

    def add(self, eng, fn, reads=(), writes=(), dma=False, sw=None):
        op = Op(eng, fn, dma)
        op.idx = len(self.ops)
        res = self.res
        psr = [r for r in reads if r[0] == "ps"]
        if psr:
            reads = [r for r in reads if r[0] != "ps"]
            writes = list(writes) + psr
        for r in reads:
            st = res.get(r)
            if st is not None and st[0] is not None:
                op.deps.add(st[0])
        for w in writes:
            st = res.get(w)
            if st is not None:
                if st[0] is not None:
                    op.deps.add(st[0])
                op.deps.update(st[1])
        for r in reads:
            st = res.get(r)
            if st is None:
                res[r] = [None, [op.idx]]
            else:
                st[1].append(op.idx)
        for w in writes:
            res[w] = [op.idx, []]
        op.deps.discard(op.idx)
        if dma and sw is not None:
            op.sw = sw
            op.gen = self.swgen.get(sw, 0)
            self.swgen[sw] = op.gen + 1
        elif dma:
            op.dsem = self.ndma % N_DMA_SEMS
            self.ndma += 1
        self.ops.append(op)
        return op

    def emit(self):
        nc = self.nc
        ops = self.ops
        for op in ops:
            for d in op.deps:
                dop = ops[d]
                if dop.dma:
                    continue
                if dop.eng == op.eng and (dop.eng == "pe" or not SAME_ENGINE_SYNC):
                    continue
                dop.signal = True
        ticks = {e: 0 for e in self.ENGS}
        dcount = [0] * N_DMA_SEMS
        for op in ops:
            if op.dma and op.sw is not None:
                continue
            if op.dma:
                op.prev_dval = dcount[op.dsem]
                dcount[op.dsem] += 16
                op.dval = dcount[op.dsem]
            elif op.signal:
                ticks[op.eng] += 1
                op.tick = ticks[op.eng]
        per_eng = {e: [o for o in ops if o.eng == e] for e in self.ENGS}
        with contextlib.ExitStack() as es:
            esem = {e: es.enter_context(nc.semaphore("s_" + e)) for e in ("pe", "act", "dve", "pool")}
            dsems = [es.enter_context(nc.semaphore("d%d" % i)) for i in range(N_DMA_SEMS)]
            swsems = {}
            for i, slot in enumerate(sorted(self.swgen.keys(), key=str)):
                swsems[slot] = [es.enter_context(nc.semaphore("w%d_%d" % (i, j))) for j in range(2)]
            block = es.enter_context(nc.Block())

            def run(engname, eng):
                waited = {}

                def wait(key, sem, val):
                    if waited.get(key, 0) >= val:
                        return
                    eng.wait_ge(sem, val)
                    waited[key] = val

                for op in per_eng[engname]:
                    for d in sorted(op.deps):
                        dop = ops[d]
                        if dop.dma and dop.sw is not None:
                            wait(("sw", dop.sw, dop.gen), swsems[dop.sw][dop.gen % 2], 16)
                        elif dop.dma:
                            wait(("d", dop.dsem), dsems[dop.dsem], dop.dval)
                        else:
                            if dop.eng == engname and (engname == "pe" or not SAME_ENGINE_SYNC):
                                continue
                            wait(("e", dop.eng), esem[dop.eng], dop.tick)
                    if op.dma and op.sw is not None:
                        if op.gen >= 1:
                            wait(("sw", op.sw, op.gen - 1), swsems[op.sw][(op.gen - 1) % 2], 16)
                            eng.sem_clear(swsems[op.sw][(op.gen - 1) % 2])
                        op.fn(eng).then_inc(swsems[op.sw][op.gen % 2], 16)
                    elif op.dma:
                        if op.prev_dval > 0:
                            wait(("d", op.dsem), dsems[op.dsem], op.prev_dval)
                        op.fn(eng).then_inc(dsems[op.dsem], 16)
                    else:
                        ins = op.fn(eng)
                        if op.signal:
                            ins.then_inc(esem[engname], 1)
                for op in per_eng[engname]:
                    if op.dma and op.sw is None:
                        wait(("d", op.dsem), dsems[op.dsem], op.dval)

            if per_eng["pe"]:
                block.tensor(lambda e: run("pe", e))
            if per_eng["act"]:
                block.scalar(lambda e: run("act", e))
            if per_eng["dve"]:
                block.vector(lambda e: run("dve", e))
            if per_eng["pool"]:
                block.gpsimd(lambda e: run("pool", e))
            if per_eng["sp"]:
                block.sync(lambda e: run("sp", e))


class Tile:
    def __init__(self, arena, off, nwords, dtype, shape, parts=128):
        self.off = off
        self.n = nwords
        self.esz = 4 if dtype == F32 else 2
        ne = int(np.prod(shape))
        nwx = ne if dtype == F32 else (ne + 1) // 2
        v = arena[0:parts, off:off + nwx]
        if dtype != F32:
            v = v.bitcast(dtype)[:, 0:ne]
        self.shape = tuple(shape)
        if len(shape) == 2:
            v = v.rearrange("p (a b) -> p a b", b=shape[1])
        elif len(shape) == 3:
            v = v.rearrange("p (a b c) -> p a b c", b=shape[1], c=shape[2])
        self.ap = v
        self.inner = int(np.prod(shape[1:])) if len(shape) > 1 else 1

    def k(self, lo=None, hi=None):
        if lo is None:
            lo, hi = 0, self.n * 4 // self.esz
        w0 = self.off + (lo * self.esz) // 4
        w1 = self.off + (hi * self.esz + 3) // 4
        return [("sb", g) for g in range(w0 // GRAN, (w1 - 1) // GRAN + 1)]

    def ks(self, i, n=1):
        return self.k(i * self.inner, (i + n) * self.inner)


class Arena:
    def __init__(self, nc, es, nwords):
        self.t = es.enter_context(nc.sbuf_tensor("arena", [128, nwords], F32))
        self.ap = self.t[:]
        self.nwords = nwords
        self.cur = 0

    def alloc(self, shape, dtype=F32, parts=128):
        ne = int(np.prod(shape))
        nw = ne if dtype == F32 else (ne + 1) // 2
        nw = (nw + GRAN - 1) // GRAN * GRAN
        assert self.cur + nw <= self.nwords, ("arena overflow", self.cur, nw, self.nwords)
        t = Tile(self.ap, self.cur, nw, dtype, shape, parts)
        self.cur += nw
        return t


def build_nc(S, NSEQ, DEPTH=2, dbg=None):
    NB = S // 512
    NT = S // 128
    nc = bass.Bass("TRN2", target_bir_lowering=False)
    dram = lambda name, shape, kind="ExternalInput": nc.dram_tensor(name, shape, F32, kind=kind).ap()
    x_d = dram("x", [NSEQ, S, D])
    p_d = dram("p", [DEPTH, NSEQ, S, 256])
    ln_in_g = dram("ln_in_g", [D]); ln_in_b = dram("ln_in_b", [D])
    bias_d = dram("bias_tab", [8, 5, 128, 128])
    w_in = dram("w_in", [DEPTH, D, NIN])
    tok_mix = dram("tok_mix", [DEPTH, 1824])
    decay_base = dram("decay_base", [DEPTH, 512]); decay_up = dram("decay_up", [DEPTH, 64, 512])
    aaa_base = dram("aaa_base", [DEPTH, 512]); aaa_up = dram("aaa_up", [DEPTH, 64, 512])
    gate_up = dram("gate_up", [DEPTH, 160, 512])
    k_k = dram("k_k", [DEPTH, 512]); k_a = dram("k_a", [DEPTH, 512]); r_k = dram("r_k", [DEPTH, 512])
    vres_base = dram("vres_base", [DEPTH - 1, 512]); vres_down = dram("vres_down", [DEPTH - 1, 512, 32])
    vres_up = dram("vres_up", [DEPTH - 1, 32, 512])
    gn_g = dram("gn_g", [DEPTH, 512]); gn_b = dram("gn_b", [DEPTH, 512])
    w_a = dram("w_branch_attn", [DEPTH, 512, D]); w_b = dram("w_branch_rwkv", [DEPTH, 512, D])
    w_out = dram("w_out", [DEPTH, D, D])
    router_w = dram("router_w", [DEPTH, D, 36]); router_b = dram("router_b", [DEPTH, 36])
    exp_gate = dram("exp_gate", [DEPTH, 32, D, 256]); exp_up = dram("exp_up", [DEPTH, 32, D, 256])
    exp_down = dram("exp_down", [DEPTH, 32, 256, D])
    ple_proj = dram("ple_proj", [DEPTH, 256, D]); ple_gate = dram("ple_gate", [DEPTH, D, D])
    ln_g = dram("ln_g", [DEPTH, 3, D]); ln_b = dram("ln_b", [DEPTH, 3, D])
    y_d = dram("y", [NSEQ, S, D], kind="ExternalOutput")
    xs_d = dram("xs_scr", [S, D], kind="Internal")
    dramh = lambda name, shape: nc.dram_tensor(name, shape, BF16, kind="Internal").ap()
    xT_d = dramh("xT_scr", [KC, 128, S])
    w_in_h = dramh("w_in_h", [DEPTH, D, NIN]); w_a_h = dramh("w_a_h", [DEPTH, 512, D]); w_b_h = dramh("w_b_h", [DEPTH, 512, D])
    w_out_h = dramh("w_out_h", [DEPTH, D, D]); exp_gate_h = dramh("exp_gate_h", [DEPTH, 32, D, 256])
    exp_up_h = dramh("exp_up_h", [DEPTH, 32, D, 256]); exp_down_h = dramh("exp_down_h", [DEPTH, 32, 256, D])
    ple_proj_h = dramh("ple_proj_h", [DEPTH, 256, D]); ple_gate_h = dramh("ple_gate_h", [DEPTH, D, D])
    bias_h = dramh("bias_h", [8, 5, 128, 128])
    vf_d = dram("vf_scr", [4, 128, S], kind="Internal")
    dbg_d = {}
    if dbg:
        for name, shape in dbg.items():
            dbg_d[name] = dram("dbg_" + name, shape, kind="ExternalOutput")

    P = Prog(nc)
    es = contextlib.ExitStack()
    with es:
        A = Arena(nc, es, 53000)
        psum = es.enter_context(nc.psum_tensor("ps", [128, 8, 512], F32))
        bank_ctr = [0]

        def bank():
            b = bank_ctr[0] % 6
            bank_ctr[0] += 1
            return b

        def bank2():
            b = bank_ctr[0] % 6
            if b % 2:
                bank_ctr[0] += 1
                b = bank_ctr[0] % 6
            bank_ctr[0] += 2
            return b

        PK = lambda b: [("ps", b)]

        def MM(out, lhsT, rhs, start, stop, reads, writes):
            P.add("pe", lambda e: e.matmul(out, lhsT=lhsT, rhs=rhs, start=start, stop=stop), reads, writes)

        def TR(out, in_, ident, reads, writes):
            P.add("pe", lambda e: e.transpose(out, in_, ident), reads, writes)

        def ACT(out, in_, func, reads, writes, bias=None, scale=None):
            kw = {}
            if bias is not None:
                kw["bias"] = bias
            if scale is not None:
                kw["scale"] = scale
            P.add("act", lambda e: e.activation(out=out, in_=in_, func=func, **kw), reads, writes)

        def TT(eng, out, in0, in1, op, reads, writes):
            P.add(eng, lambda e: e.tensor_tensor(out=out, in0=in0, in1=in1, op=op), reads, writes)

        def TS(eng, out, in0, s1, op0, reads, writes, s2=None, op1=None):
            if op1 is None:
                P.add(eng, lambda e: e.tensor_scalar(out=out, in0=in0, scalar1=s1, scalar2=None, op0=op0), reads, writes)
            else:
                P.add(eng, lambda e: e.tensor_scalar(out=out, in0=in0, scalar1=s1, scalar2=s2, op0=op0, op1=op1), reads, writes)

        def STT(eng, out, in0, scalar, in1, op0, op1, reads, writes):
            P.add(eng, lambda e: e.scalar_tensor_tensor(out=out, in0=in0, scalar=scalar, in1=in1, op0=op0, op1=op1), reads, writes)

        def CP(eng, out, in_, reads, writes):
            if eng == "act":
                P.add("dve", lambda e: e.tensor_copy(out=out, in_=in_), reads, writes)
            else:
                P.add(eng, lambda e: e.tensor_copy(out=out, in_=in_), reads, writes)

        def RECIP(out, in_, reads, writes):
            P.add("dve", lambda e: e.reciprocal(out=out, in_=in_), reads, writes)

        def MEMSET(eng, ap, val, writes):
            P.add(eng, lambda e: e.memset(ap, val), (), writes)

        def DMA(eng, out, in_, reads, writes, sw=None, **kw):
            P.add(eng, lambda e: e.dma_start(out=out, in_=in_, **kw), reads, writes, dma=True, sw=sw)

        ident = A.alloc([128]); identb = A.alloc([128], BF16)
        onesbd = A.alloc([128])
        maskU = A.alloc([4, 128], parts=64)
        maskL = A.alloc([2, 64], parts=64)
        identT = A.alloc([2, 64], parts=64)
        rmask = A.alloc([512])
        biasT = A.alloc([40, 128], BF16)
        KI = ident.k(); KIB = identb.k()
        MEMSET("pool", ident.ap, 0.0, KI)
        P.add("pool", lambda e: e.affine_select(out=ident.ap, in_=ident.ap, pattern=[[-1, 128]], compare_op=ALU.not_equal,
                                                fill=1.0, base=0, channel_multiplier=1), KI, KI)
        CP("pool", identb.ap, ident.ap, KI, KIB)
        MEMSET("pool", onesbd.ap, 0.0, onesbd.k())
        MEMSET("pool", onesbd.ap[0:64, 0:64], 1.0, onesbd.k())
        MEMSET("pool", onesbd.ap[64:128, 64:128], 1.0, onesbd.k())
        MEMSET("pool", maskU.ap, 1.0, maskU.k())
        for q in range(4):
            P.add("pool", (lambda q: lambda e: e.affine_select(out=maskU.ap[:, q, 0:64], in_=maskU.ap[:, q, 0:64], pattern=[[1, 64]],
                  compare_op=ALU.is_ge, fill=0.0, base=-1, channel_multiplier=-1))(q), maskU.k(), maskU.k())
            P.add("pool", (lambda q: lambda e: e.affine_select(out=maskU.ap[:, q, 64:128], in_=maskU.ap[:, q, 64:128], pattern=[[1, 64]],
                  compare_op=ALU.is_ge, fill=0.0, base=0, channel_multiplier=-1))(q), maskU.k(), maskU.k())
        MEMSET("pool", maskL.ap, 1.0, maskL.k())
        for q in range(2):
            P.add("pool", (lambda q: lambda e: e.affine_select(out=maskL.ap[:, q, :], in_=maskL.ap[:, q, :], pattern=[[-1, 64]],
                  compare_op=ALU.is_ge, fill=0.0, base=-1, channel_multiplier=1))(q), maskL.k(), maskL.k())
        CP("pool", identT.ap[:, 0, :], ident.ap[0:64, 0:64], KI, identT.k())
        CP("pool", identT.ap[:, 1, :], ident.ap[0:64, 0:64], KI, identT.k())
        MEMSET("pool", rmask.ap, 1.0, rmask.k())
        MEMSET("pool", rmask.ap.rearrange("p (c t) -> p c t", t=64)[:, :, 0:1], 0.0, rmask.k())

        xstg = [A.alloc([KC, 128], BF16) for _ in range(2)]
        xsc = [0]
        NWB = 4
        wpool = [A.alloc([KC, 512], BF16) for _ in range(NWB)]
        wctr = [0]

        def wbuf():
            t = wpool[wctr[0] % NWB]
            t.idx = wctr[0] % NWB
            wctr[0] += 1
            return t

        def cast_load(dst, src, dkeys):
            DMA("sp", dst, src, ["wh_all"], dkeys)

        lnp = A.alloc([2, D])
        comb = A.alloc([NT, 32])
        prm = A.alloc([16, 4])
        PRM = {n: i for i, n in enumerate(["decay_base", "aaa_base", "k_k", "k_a", "r_k", "gn_g", "gn_b", "vres_base"])}
        mu = A.alloc([2, 16])
        lora = A.alloc([3, 512])
        vrd = A.alloc([4, 32]); vru = A.alloc([512], parts=32)
        rw = A.alloc([KC, 36]); rb = A.alloc([36])
        lastc = A.alloc([16])
        Sst = A.alloc([4, 64])

        mark_common = A.cur

        def precast():
            stg = [A.alloc([4096]) for _ in range(4)]
            outb = [A.alloc([4096], BF16) for _ in range(4)]
            cnt = [0]
            allk = []

            def chunk(src3, dst3):
                a, b_ = src3.shape[1], src3.shape[2]
                i = cnt[0]; cnt[0] += 1
                st_ = stg[i % 4]; ob = outb[i % 4]
                sv = st_.ap[:, 0:a * b_].rearrange("p (a b) -> p a b", b=b_)
                ov = ob.ap[:, 0:a * b_].rearrange("p (a b) -> p a b", b=b_)
                DMA("sp", sv, src3, (), st_.k())
                CP("pool" if i % 4 == 3 else "dve", ov, sv, st_.k(), ob.k())
                key = ("whc", i)
                DMA("sp", dst3, ov, ob.k(), [key])
                allk.append(key)

            def cast2d(src2, dst2):
                R, C = src2.shape
                nrb = R // 128
                if C <= 4096:
                    Astep = max(1, 4096 // C)
                    sv = src2.rearrange("(a p) n -> p a n", p=128)
                    dv = dst2.rearrange("(a p) n -> p a n", p=128)
                    for a0 in range(0, nrb, Astep):
                        a1 = min(nrb, a0 + Astep)
                        chunk(sv[:, a0:a1, :], dv[:, a0:a1, :])
                else:
                    sv = src2.rearrange("(a p) n -> p a n", p=128)
                    dv = dst2.rearrange("(a p) n -> p a n", p=128)
                    for a0 in range(nrb):
                        for c0 in range(0, C, 4096):
                            c1 = min(C, c0 + 4096)
                            chunk(sv[:, a0:a0 + 1, c0:c1], dv[:, a0:a0 + 1, c0:c1])

            cast2d(w_in.rearrange("l k n -> (l k) n"), w_in_h.rearrange("l k n -> (l k) n"))
            cast2d(w_a.rearrange("l k n -> (l k) n"), w_a_h.rearrange("l k n -> (l k) n"))
            cast2d(w_b.rearrange("l k n -> (l k) n"), w_b_h.rearrange("l k n -> (l k) n"))
            cast2d(w_out.rearrange("l k n -> (l k) n"), w_out_h.rearrange("l k n -> (l k) n"))
            cast2d(ple_proj.rearrange("l k n -> (l k) n"), ple_proj_h.rearrange("l k n -> (l k) n"))
            cast2d(ple_gate.rearrange("l k n -> (l k) n"), ple_gate_h.rearrange("l k n -> (l k) n"))
            cast2d(bias_d.rearrange("h j k q -> (h j k) q"), bias_h.rearrange("h j k q -> (h j k) q"))
            DMA("sp", stg[0].ap[0:1, 0:8], ln_in_g[0:8].rearrange("(o n) -> o n", o=1), allk, stg[0].k() + ["wh_all"])
            A.cur = mark_common


        def load_ln(gsrc, bsrc, lp=None):
            lp = lp or lnp
            DMA("sp", lp.ap[:, 0, :], gsrc.partition_broadcast(128), (), lp.ks(0))
            DMA("sp", lp.ap[:, 1, :], bsrc.partition_broadcast(128), (), lp.ks(1))

        def layer_norm_g(h, hk, out_ap, out_keys, st, mv, lp=None):
            lp = lp or lnp
            for c in range(2):
                P.add("dve", (lambda c: lambda e: e.bn_stats(out=st.ap[:, c, :], in_=h[:, c * 512:(c + 1) * 512]))(c), hk, st.k())
            P.add("dve", lambda e: e.bn_aggr(out=mv.ap[:, 0:2], in_=st.ap), st.k(), mv.k())
            TS("dve", mv.ap[:, 2:3], mv.ap[:, 1:2], LN_EPS, ALU.add, mv.k(), mv.k())
            yield
            ACT(mv.ap[:, 2:3], mv.ap[:, 2:3], AF.Sqrt, mv.k(), mv.k())
            yield
            RECIP(mv.ap[:, 2:3], mv.ap[:, 2:3], mv.k(), mv.k())
            TS("dve", h, h, mv.ap[:, 0:1], ALU.subtract, hk + mv.k(), hk, s2=mv.ap[:, 2:3], op1=ALU.mult)
            yield
            TT("dve", h, h, lp.ap[:, 0, :], ALU.mult, hk + lp.ks(0), hk)
            yield
            TT("pool", out_ap, h, lp.ap[:, 1, :], ALU.add, hk + lp.ks(1), out_keys)
            yield

        def layer_norm(h, hk, out_ap, out_keys, st, mv, lp=None):
            for _ in layer_norm_g(h, hk, out_ap, out_keys, st, mv, lp):
                pass

        def interleave(gens, width):
            gens = list(gens)
            nxt = 0
            active = []
            while nxt < len(gens) or active:
                while len(active) < width and nxt < len(gens):
                    active.append(gens[nxt])
                    nxt += 1
                for g_ in list(active):
                    try:
                        next(g_)
                    except StopIteration:
                        active.remove(g_)

        def to_feature_major(xt_tok, xk, tok0, f32_out=None, sb=None, to_dram=True):
            if to_dram:
                stg = xstg[xsc[0] % 2]
                xsc[0] += 1
            for half in range(2):
                b = bank()
                for cc in range(4):
                    c = half * 4 + cc
                    TR(psum[:, b, cc * 128:(cc + 1) * 128], xt_tok[:, c * 128:(c + 1) * 128], ident.ap, xk + KI, PK(b))
                src = psum[:, b, :].rearrange("p (c t) -> p c t", t=128)
                if sb is not None:
                    tl, ncols, col0 = sb
                    wk = []
                    for c in range(half * 4, half * 4 + 4):
                        wk += tl.k(c * ncols + col0, c * ncols + col0 + 128)
                    CP("dve", tl.ap[:, half * 4:half * 4 + 4, col0:col0 + 128], src, PK(b), wk)
                if to_dram:
                    CP("dve", stg.ap[:, half * 4:half * 4 + 4, :], src, PK(b), stg.ks(half * 4, 4))
                if f32_out is not None:
                    CP("dve", f32_out.ap[:, half * 4:half * 4 + 4, :], src, PK(b), f32_out.ks(half * 4, 4))
            if to_dram:
                DMA("sp", xT_d[:, :, tok0:tok0 + 128].rearrange("c p t -> p c t"), stg.ap, stg.k(), [("xTd", tok0 // 128)])

        def load_layer_params(l):
            fm = lambda src: src.rearrange("(c p) -> p c", p=128)
            lst = [("decay_base", decay_base[l]), ("aaa_base", aaa_base[l]), ("k_k", k_k[l]), ("k_a", k_a[l]),
                   ("r_k", r_k[l]), ("gn_g", gn_g[l]), ("gn_b", gn_b[l])]
            if l > 0:
                lst.append(("vres_base", vres_base[l - 1]))
            for name, src in lst:
                DMA("sp", prm.ap[:, PRM[name], :], fm(src), (), prm.k(), allow_slow_non_contiguous=True)
            DMA("sp", mu.ap[:, 0, 0:14], tok_mix[l, 0:1792].rearrange("(c p) -> p c", p=128), (), mu.k(), allow_slow_non_contiguous=True)
            DMA("sp", mu.ap[0:32, 0, 14:15], tok_mix[l, 1792:1824].rearrange("(p o) -> p o", o=1), (), mu.k())
            TS("pool", mu.ap[:, 1, 0:14], mu.ap[:, 0, 0:14], -1.0, ALU.mult, mu.k(), mu.k(), s2=1.0, op1=ALU.add)
            TS("pool", mu.ap[0:32, 1, 14:15], mu.ap[0:32, 0, 14:15], -1.0, ALU.mult, mu.k(), mu.k(), s2=1.0, op1=ALU.add)
            DMA("sp", lora.ap[0:64, 0, :], decay_up[l], (), lora.ks(0))
            DMA("sp", lora.ap[64:128, 0, :], aaa_up[l], (), lora.ks(0))
            DMA("sp", lora.ap[:, 1, :], gate_up[l, 0:128, :], (), lora.ks(1))
            DMA("sp", lora.ap[0:32, 2, :], gate_up[l, 128:160, :], (), lora.ks(2))
            if l > 0:
                DMA("sp", vrd.ap, vres_down[l - 1].rearrange("(c p) r -> p c r", p=128), (), vrd.k())
                DMA("sp", vru.ap, vres_up[l - 1], (), vru.k())
            DMA("sp", rw.ap, router_w[l].rearrange("(k p) n -> p k n", p=128), (), rw.k())
            DMA("sp", rb.ap, router_b[l].partition_broadcast(128), (), rb.k())

        def wload(src_ap, shape):
            t = wbuf()
            n = int(np.prod(shape))
            v = t.ap.rearrange("p a b -> p (a b)")[:, 0:n]
            if len(shape) == 2:
                v = v.rearrange("p (a b) -> p a b", b=shape[1])
            cast_load(v, src_ap, t.k())
            return v, t.k()

        def phaseA(s, l):
            A.cur = mark_common
            kT = A.alloc([4, 1024], BF16); vring = A.alloc([8, 8, 65], BF16)
            xTa = A.alloc([KC, 1024], BF16)
            yAT = A.alloc([4, 512], BF16); yBT = A.alloc([4, 512], BF16)
            st6s = [A.alloc([2, 6]) for _ in range(2)]; mv4s = [A.alloc([4]) for _ in range(2)]
            lgs = [A.alloc([36]) for _ in range(2)]; rts = [A.alloc([64]) for _ in range(2)]
            twd = A.alloc([512]); tgd1 = A.alloc([512]); tgd2 = A.alloc([512], parts=32)
            v_all = A.alloc([4, 512]); vd = A.alloc([512], parts=32)
            mark_att = A.cur
            qT = A.alloc([4, 512], BF16)
            PT = [A.alloc([4, 128], BF16) for _ in range(6)]
            ytok = A.alloc([512]); rc = A.alloc([8])
            end_att = A.cur
            A.cur = mark_att
            NSL = 4
            tokT = [A.alloc([3, 128], parts=64) for _ in range(NSL)]
            AT = [A.alloc([4, 128], parts=64) for _ in range(NSL)]
            X0 = [A.alloc([2, 64], parts=64) for _ in range(NSL)]
            Xn = [[A.alloc([4, 64], parts=64) for _ in range(2)] for _ in range(NSL)]
            Tt = [[A.alloc([2, 64], parts=64) for _ in range(2)] for _ in range(NSL)]
            G0 = [A.alloc([2, 64], parts=64) for _ in range(NSL)]
            A.cur = max(A.cur, end_att)
            mark_r = A.cur
            t_r = A.alloc([512]); t_k = A.alloc([512]); t_sg = A.alloc([512]); t_cum = A.alloc([512])
            t_pin = A.alloc([512]); t_inv = A.alloc([512]); t_a = A.alloc([512]); t_kk = A.alloc([512])
            t_sq = A.alloc([512]); t_km = A.alloc([512]); t_bon = A.alloc([512])
            bt = A.alloc([512]); kt = A.alloc([512]); yT = A.alloc([512]); gnt = A.alloc([512])
            ar = A.alloc([8, 2, 64]); pC = A.alloc([8]); t_vf = A.alloc([512])
            Wsb = A.alloc([2, 64], parts=64); Usb = A.alloc([2, 64], parts=64); Stmp = A.alloc([64])
            end_r = A.cur
            A.cur = mark_r
            sga = A.alloc([512]); sgb = A.alloc([512]); m1 = A.alloc([512]); m2 = A.alloc([512])
            zT = A.alloc([8, 512], BF16)
            hbufs = [A.alloc([D]) for _ in range(2)]; x0s = [A.alloc([D]) for _ in range(2)]; x1fms = [A.alloc([KC, 128]) for _ in range(2)]
            A.cur = max(A.cur, end_r)
            print("phaseA arena words", A.cur, "of", A.nwords)

            load_layer_params(l)
            load_ln(ln_g[l, 0], ln_b[l, 0])
            MEMSET("pool", lastc.ap, 0.0, lastc.k())
            MEMSET("pool", Sst.ap, 0.0, Sst.k())
            MEMSET("pool", vring.ap[:, :, :, 64:65], 1.0, vring.k())
            wv = w_in_h[l].rearrange("(k p) n -> p k n", p=128)

            def shift(pc_ap, np_, mc, out_ap, out_keys, b):
                m = mu.ap[0:np_, 0, mc:mc + 1]; om = mu.ap[0:np_, 1, mc:mc + 1]
                TS("dve", out_ap, pc_ap, om, ALU.mult, PK(b) + mu.k(), out_keys)
                STT("dve", out_ap[:, 1:512], pc_ap[:, 0:511], m, out_ap[:, 1:512], ALU.mult, ALU.add, PK(b) + out_keys + mu.k(), out_keys)
                STT("dve", out_ap[:, 0:1], lastc.ap[0:np_, mc:mc + 1], m, out_ap[:, 0:1], ALU.mult, ALU.add, lastc.k() + out_keys + mu.k(), out_keys)
                CP("dve", lastc.ap[0:np_, mc:mc + 1], pc_ap[:, 511:512], PK(b), lastc.k())

            def load_xblock(bb):
                rp_ = (bb % 2) * 512
                wk_ = []
                for c in range(KC):
                    wk_ += xTa.k(c * 1024 + rp_, c * 1024 + rp_ + 512)
                DMA("sp", xTa.ap[:, :, rp_:rp_ + 512], xT_d[:, :, bb * 512:(bb + 1) * 512].rearrange("c p t -> p c t"),
                    [("xTd", bb * 4 + i) for i in range(4)], wk_)

            load_xblock(0)
            for b in range(NB):
                t0 = b * 512
                xr = (b % 2) * 512
                if b + 1 < NB:
                    load_xblock(b + 1)
                xk = []
                for c in range(KC):
                    xk += xTa.k(c * 1024 + xr, c * 1024 + xr + 512)

                def proj_fm(wt, wk, col, ncol=128, kparts=128):
                    bk = bank()
                    for k in range(KC):
                        MM(psum[0:ncol, bk, :], wt[:, k, col:col + ncol], xTa.ap[:, k, xr:xr + 512], k == 0, k == KC - 1, wk + xk, PK(bk))
                    return bk

                wt, wk = wload(wv[:, :, 0:512], [KC, 512])
                for c in range(4):
                    bk = proj_fm(wt, wk, c * 128)
                    TS("dve", qT.ap[:, c, :], psum[:, bk, :], 0.125, ALU.mult, PK(bk), qT.ks(c))
                wt, wk = wload(wv[:, :, 512:1024], [KC, 512])
                rp = (b % 2) * 512
                for c in range(4):
                    bk = proj_fm(wt, wk, c * 128)
                    CP("act", kT.ap[:, c, rp:rp + 512], psum[:, bk, :], PK(bk), kT.k(c * 1024 + rp, c * 1024 + rp + 512))
                wt, wk = wload(wv[:, :, 1024:1536], [KC, 512])
                for tt in range(4):
                    bk = bank()
                    for k in range(KC):
                        MM(psum[:, bk, :], xTa.ap[:, k, xr + tt * 128:xr + tt * 128 + 128], wt[:, k, :], k == 0, k == KC - 1, wk + xk, PK(bk))
                    slot = (b % 2) * 4 + tt
                    CP("dve", vring.ap[:, slot, :, 0:64], psum[:, bk, :].rearrange("p (h d) -> p h d", d=64), PK(bk), vring.ks(slot))

                for qi in range(4):
                    qb = b * 4 + qi
                    js = [j for j in range(5) if qb - 4 + j >= 0]
                    for hg in range(2):
                        pts = {}
                        for ji, j in enumerate(js):
                            kb = qb - 4 + j
                            ro = (kb % 8) * 128
                            bk = bank()
                            for hh in range(4):
                                h = hg * 4 + hh
                                c = h // 2; r0 = (h % 2) * 64
                                MM(psum[:, bk, hh * 128:(hh + 1) * 128], kT.ap[r0:r0 + 64, c, ro:ro + 128], qT.ap[r0:r0 + 64, c, qi * 128:(qi + 1) * 128],
                                   True, False, kT.k(c * 1024 + ro, c * 1024 + ro + 128) + qT.ks(c), PK(bk))
                                MM(psum[:, bk, hh * 128:(hh + 1) * 128], identb.ap, biasT.ap[:, h * 5 + j, :], False, True, KIB + biasT.ks(h * 5 + j), PK(bk))
                            pt = PT[ji]
                            ACT(pt.ap.rearrange("p h q -> p (h q)"), psum[:, bk, :], AF.Exp, PK(bk), pt.k())
                            pts[j] = pt
                        bk = bank()
                        pv = psum[:, bk, 0:260].rearrange("p (h d) -> p h d", d=65)
                        for hh in range(4):
                            h = hg * 4 + hh
                            for ji, j in enumerate(js):
                                kb = qb - 4 + j
                                slot = kb % 8
                                MM(pv[:, hh, :], pts[j].ap[:, hh, :], vring.ap[:, slot, h, :], ji == 0, ji == len(js) - 1,
                                   pts[j].k() + vring.ks(slot), PK(bk))
                        RECIP(rc.ap[:, hg * 4:hg * 4 + 4].rearrange("p (h o) -> p h o", o=1), pv[:, :, 64:65], PK(bk), rc.k())
                        TT("dve", ytok.ap[:, hg * 256:(hg + 1) * 256].rearrange("p (h d) -> p h d", d=64), pv[:, :, 0:64],
                           rc.ap[:, hg * 4:hg * 4 + 4].rearrange("p (h o) -> p h o", o=1).to_broadcast([128, 4, 64]), ALU.mult,
                           PK(bk) + rc.k(), ytok.k())
                    bk = bank()
                    for c in range(4):
                        TR(psum[:, bk, c * 128:(c + 1) * 128], ytok.ap[:, c * 128:(c + 1) * 128], ident.ap, ytok.k() + KI, PK(bk))
                    wkeys = []
                    for c in range(4):
                        wkeys += yAT.k(c * 512 + qi * 128, c * 512 + qi * 128 + 128)
                    CP("act", yAT.ap[:, :, qi * 128:(qi + 1) * 128], psum[:, bk, :].rearrange("p (c t) -> p c t", t=128), PK(bk), wkeys)

                if BIS == 4:
                    continue
                wt_r, wk_r = wload(wv[:, :, 1536:2048], [KC, 512])
                wt_k, wk_k = wload(wv[:, :, 2048:2560], [KC, 512])
                wt_v, wk_v = wload(wv[:, :, 2560:3072], [KC, 512])
                for hp in range(4):
                    bk = proj_fm(wt_v, wk_v, hp * 128)
                    shift(psum[:, bk, :], 128, 8 + hp, v_all.ap[:, hp, :], v_all.ks(hp), bk)
                wt_l, wk_l = wload(wv[:, :, 3072:3360], [KC, 288])
                bk = proj_fm(wt_l, wk_l, 0)
                shift(psum[:, bk, :], 128, 12, twd.ap, twd.k(), bk)
                bk = proj_fm(wt_l, wk_l, 128)
                shift(psum[:, bk, :], 128, 13, tgd1.ap, tgd1.k(), bk)
                bk = proj_fm(wt_l, wk_l, 256, ncol=32)
                shift(psum[0:32, bk, :], 32, 14, tgd2.ap, tgd2.k(), bk)
                ACT(twd.ap[0:64, :], twd.ap[0:64, :], AF.Tanh, twd.k(), twd.k())
                ACT(tgd1.ap, tgd1.ap, AF.Sigmoid, tgd1.k(), tgd1.k())
                ACT(tgd2.ap, tgd2.ap, AF.Sigmoid, tgd2.k(), tgd2.k())
                if l == 0:
                    for hp in range(4):
                        DMA("sp", vf_d[hp, :, t0:t0 + 512], v_all.ap[:, hp, :], v_all.ks(hp), [("vf", hp, b)])
                else:
                    bk = bank()
                    for hp in range(4):
                        MM(psum[0:32, bk, :], vrd.ap[:, hp, :], v_all.ap[:, hp, :], hp == 0, hp == 3, vrd.k() + v_all.ks(hp), PK(bk))
                    CP("act", vd.ap, psum[0:32, bk, :], PK(bk), vd.k())

                def pp(name, hp):
                    return prm.ap[:, PRM[name], hp:hp + 1]

                c3 = lambda t: t.ap.rearrange("p (c t) -> p c t", t=64)
                arv = ar.ap

                def prep1(hp):
                    va = v_all.ap[:, hp, :]; vk = v_all.ks(hp)
                    bk = proj_fm(wt_r, wk_r, hp * 128)
                    shift(psum[:, bk, :], 128, hp, t_r.ap, t_r.k(), bk)
                    yield
                    bk = proj_fm(wt_k, wk_k, hp * 128)
                    shift(psum[:, bk, :], 128, 4 + hp, t_k.ap, t_k.k(), bk)
                    yield
                    if l > 0:
                        bk = bank()
                        MM(psum[:, bk, :], vru.ap[0:32, hp * 128:(hp + 1) * 128], vd.ap, True, True, vru.k() + vd.k(), PK(bk))
                        ACT(t_sq.ap, psum[:, bk, :], AF.Sigmoid, PK(bk) + prm.k(), t_sq.k(), bias=pp("vres_base", hp))
                        DMA("sp", t_vf.ap, vf_d[hp, :, t0:t0 + 512], [("vf", hp, b)], t_vf.k())
                        TT("dve", t_vf.ap, t_vf.ap, va, ALU.subtract, t_vf.k() + vk, t_vf.k())
                        yield
                        TT("dve", t_vf.ap, t_vf.ap, t_sq.ap, ALU.mult, t_vf.k() + t_sq.k(), t_vf.k())
                        TT("dve", va, va, t_vf.ap, ALU.add, vk + t_vf.k(), vk)
                        yield
                    bk = bank()
                    MM(psum[:, bk, :], lora.ap[0:64, 0, hp * 128:(hp + 1) * 128], twd.ap[0:64, :], True, True, lora.ks(0) + twd.k(), PK(bk))
                    ACT(t_sg.ap, psum[:, bk, :], AF.Sigmoid, PK(bk) + prm.k(), t_sg.k(), bias=pp("decay_base", hp))
                    yield
                    P.add("dve", lambda e: e.tensor_tensor_scan(out=t_cum.ap, data0=rmask.ap, data1=t_sg.ap, initial=0.0, op0=ALU.mult, op1=ALU.add),
                          rmask.k() + t_sg.k(), t_cum.k())
                    yield
                    ACT(t_pin.ap, t_cum.ap, AF.Exp, t_cum.k(), t_pin.k(), scale=-C0)
                    ACT(t_inv.ap, t_cum.ap, AF.Exp, t_cum.k(), t_inv.k(), scale=C0)
                    TT("dve", t_sg.ap, t_cum.ap, t_sg.ap, ALU.subtract, t_cum.k() + t_sg.k(), t_sg.k())
                    yield
                    ACT(t_cum.ap, t_sg.ap, AF.Exp, t_sg.k(), t_cum.k(), scale=-C0)
                    bk = bank()
                    MM(psum[:, bk, :], lora.ap[64:128, 0, hp * 128:(hp + 1) * 128], twd.ap[64:128, :], True, True, lora.ks(0) + twd.k(), PK(bk))
                    ACT(t_a.ap, psum[:, bk, :], AF.Sigmoid, PK(bk) + prm.k(), t_a.k(), bias=pp("aaa_base", hp))
                    yield
                    TS("dve", t_kk.ap, t_k.ap, pp("k_k", hp), ALU.mult, t_k.k() + prm.k(), t_kk.k())
                    TT("pool", t_sq.ap, t_kk.ap, t_kk.ap, ALU.mult, t_kk.k(), t_sq.k())
                    yield
                    bk = bank()
                    MM(psum[:, bk, :], onesbd.ap, t_sq.ap, True, True, onesbd.k() + t_sq.k(), PK(bk))
                    TS("dve", t_sq.ap, psum[:, bk, :], 1e-24, ALU.max, PK(bk), t_sq.k())
                    yield
                    ACT(t_sq.ap, t_sq.ap, AF.Sqrt, t_sq.k(), t_sq.k())
                    yield
                    RECIP(t_sq.ap, t_sq.ap, t_sq.k(), t_sq.k())
                    TT("dve", t_kk.ap, t_kk.ap, t_sq.ap, ALU.mult, t_kk.k() + t_sq.k(), t_kk.k())
                    yield
                    TS("dve", t_km.ap, t_a.ap, 1.0, ALU.subtract, t_a.k() + prm.k(), t_km.k(), s2=pp("k_a", hp), op1=ALU.mult)
                    STT("dve", t_km.ap, t_km.ap, 1.0, t_k.ap, ALU.add, ALU.mult, t_km.k() + t_k.k(), t_km.k())
                    yield
                    STT("dve", t_sq.ap, t_r.ap, pp("r_k", hp), t_km.ap, ALU.mult, ALU.mult, t_r.k() + t_km.k() + prm.k(), t_sq.k())
                    TT("dve", t_a.ap, t_a.ap, t_kk.ap, ALU.mult, t_a.k() + t_kk.k(), t_a.k())
                    yield

                def prep2(hp):
                    va = v_all.ap[:, hp, :]; vk = v_all.ks(hp)
                    bk = bank()
                    MM(psum[:, bk, :], onesbd.ap, t_sq.ap, True, True, onesbd.k() + t_sq.k(), PK(bk))
                    TT("dve", t_bon.ap, psum[:, bk, :], va, ALU.mult, PK(bk) + vk, t_bon.k())
                    CP("dve", pC.ap, c3(t_pin)[:, :, 63], t_pin.k(), pC.k())
                    STT("dve", arv[:, :, 0, :], c3(t_kk), -1.0, c3(t_cum), ALU.mult, ALU.mult, t_kk.k() + t_cum.k(), ar.k())
                    TT("pool", arv[:, :, 1, :], c3(t_r), c3(t_pin), ALU.mult, t_r.k() + t_pin.k(), ar.k())
                    TT("dve", bt.ap, t_a.ap, t_inv.ap, ALU.mult, t_a.k() + t_inv.k(), bt.k())
                    TT("pool", kt.ap, t_km.ap, t_inv.ap, ALU.mult, t_km.k() + t_inv.k(), kt.k())

                def scan_gen(hp):
                    va = v_all.ap[:, hp, :]; vk = v_all.ks(hp)
                    atv4 = lambda at: at.ap.rearrange("p (h m) x -> p h m x", h=2)

                    def stage1(c, sl):
                        cs = slice(c * 64, (c + 1) * 64)
                        tk_ = tokT[sl]; at = AT[sl]; x0t = X0[sl]; g0 = G0[sl]
                        bk = bank()
                        TR(psum[0:64, bk, 0:128], va[:, cs], ident.ap, vk + KI, PK(bk))
                        TR(psum[0:64, bk, 128:256], bt.ap[:, cs], ident.ap, bt.k() + KI, PK(bk))
                        TR(psum[0:64, bk, 256:384], kt.ap[:, cs], ident.ap, kt.k() + KI, PK(bk))
                        CP("dve", tk_.ap, psum[0:64, bk, 0:384].rearrange("p (a b) -> p a b", b=128), PK(bk), tk_.k())
                        bk = bank2()
                        for h2 in range(2):
                            rows = slice(h2 * 64, h2 * 64 + 64)
                            rhs = arv[rows, c, :, :].rearrange("p a b -> p (a b)")
                            MM(psum[0:64, bk + h2, 0:128], bt.ap[rows, cs], rhs, True, True, bt.k() + ar.k(), PK(bk + h2))
                            MM(psum[0:64, bk + h2, 128:256], kt.ap[rows, cs], rhs, True, True, kt.k() + ar.k(), PK(bk + h2))
                        TT("dve", at.ap.rearrange("p (h m) x -> p h (m x)", h=2), psum[0:64, bk:bk + 2, 0:256],
                           maskU.ap.rearrange("p (h m) x -> p h (m x)", h=2), ALU.mult, PK(bk) + PK(bk + 1) + maskU.k(), at.k())
                        yield
                        bk = bank2()
                        for h2 in range(2):
                            rows = slice(h2 * 64, h2 * 64 + 64)
                            MM(psum[0:64, bk + h2, 0:64], arv[rows, c, 0, :], bt.ap[rows, cs], True, True, bt.k() + ar.k(), PK(bk + h2))
                        TT("dve", x0t.ap, psum[0:64, bk:bk + 2, 0:64], maskL.ap, ALU.mult, PK(bk) + PK(bk + 1) + maskL.k(), x0t.k())
                        bk = bank()
                        for h2 in range(2):
                            MM(psum[0:64, bk, h2 * 64:(h2 + 1) * 64], at.ap[:, h2 * 2 + 1, 0:64], tk_.ap[:, 0, h2 * 64:(h2 + 1) * 64], True, True, at.k() + tk_.k(), PK(bk))
                        CP("dve", g0.ap, psum[0:64, bk, 0:128].rearrange("p (a b) -> p a b", b=64), PK(bk), g0.k())
                        tt_cur = Tt[sl][0]
                        TT("pool", tt_cur.ap, atv4(at)[:, :, 0, 0:64], identT.ap, ALU.add, at.k() + identT.k(), tt_cur.k())
                        yield
                        Xc = x0t.ap; Xtc = atv4(at)[:, :, 0, 0:64]
                        xck = x0t.k(); xtk = at.k()
                        for lvl in range(1, 6):
                            xn = Xn[sl][lvl % 2]
                            bk = bank()
                            for h2 in range(2):
                                MM(psum[0:64, bk, h2 * 64:(h2 + 1) * 64], Xtc[:, h2, :], Xc[:, h2, :], True, True, xck + xtk, PK(bk))
                                if lvl < 5:
                                    MM(psum[0:64, bk, 128 + h2 * 64:128 + (h2 + 1) * 64], Xc[:, h2, :], Xtc[:, h2, :], True, True, xck + xtk, PK(bk))
                            ncol = 256 if lvl < 5 else 128
                            CP("dve", xn.ap.rearrange("p a b -> p (a b)")[:, 0:ncol], psum[0:64, bk, 0:ncol], PK(bk), xn.k())
                            yield
                            Xc = xn.ap[:, 0:2, :]; Xtc = xn.ap[:, 2:4, :]; xck = xn.k(); xtk = xn.k()
                            bk = bank()
                            for h2 in range(2):
                                MM(psum[0:64, bk, h2 * 64:(h2 + 1) * 64], Xc[:, h2, :], tt_cur.ap[:, h2, :], True, True, xck + tt_cur.k(), PK(bk))
                            tt_new = Tt[sl][lvl % 2]
                            TT("dve", tt_new.ap, psum[0:64, bk, 0:128].rearrange("p (a b) -> p a b", b=64), tt_cur.ap, ALU.add, PK(bk) + tt_cur.k(), tt_new.k())
                            tt_cur = tt_new
                            yield

                    def stage2(c, sl):
                        tk_ = tokT[sl]; at = AT[sl]; g0 = G0[sl]; tt_cur = Tt[sl][1]
                        sk = Sst.ks(hp)
                        bk = bank2()
                        for h2 in range(2):
                            rows = slice(h2 * 64, h2 * 64 + 64)
                            MM(psum[0:64, bk + h2, 0:64], arv[rows, c, 0, :], Sst.ap[rows, hp, :], True, True, ar.k() + sk, PK(bk + h2))
                        TT("dve", Wsb.ap, psum[0:64, bk:bk + 2, 0:64], g0.ap, ALU.add, PK(bk) + PK(bk + 1) + g0.k(), Wsb.k())
                        yield
                        bk = bank()
                        for h2 in range(2):
                            MM(psum[0:64, bk, h2 * 64:(h2 + 1) * 64], tt_cur.ap[:, h2, :], Wsb.ap[:, h2, :], True, True, tt_cur.k() + Wsb.k(), PK(bk))
                        CP("dve", Usb.ap, psum[0:64, bk, 0:128].rearrange("p (a b) -> p a b", b=64), PK(bk), Usb.k())
                        TS("pool", Stmp.ap, Sst.ap[:, hp, :], pC.ap[:, c:c + 1], ALU.mult, sk + pC.k(), Stmp.k())
                        yield
                        for h2 in range(2):
                            rows = slice(h2 * 64, h2 * 64 + 64)
                            yo = psum[rows, 6, c * 64:(c + 1) * 64]
                            if h2 == 0:
                                MM(yo, Sst.ap[rows, hp, :], arv[rows, c, 1, :], True, False, sk + ar.k(), PK(6))
                            else:
                                MM(psum[rows, 7, c * 64:(c + 1) * 64], Sst.ap[rows, hp, :], arv[rows, c, 1, :], True, True, sk + ar.k(), PK(7))
                            MM(yo, Usb.ap[:, h2, :], at.ap[:, h2 * 2, 64:128], h2 == 1, False, Usb.k() + at.k(), PK(6))
                            MM(yo, tk_.ap[:, 0, h2 * 64:(h2 + 1) * 64], at.ap[:, h2 * 2 + 1, 64:128], False, True, tk_.k() + at.k(), PK(6))
                        bk = bank()
                        for h2 in range(2):
                            rows = slice(h2 * 64, h2 * 64 + 64)
                            so = psum[rows, bk, 0:64]
                            MM(so, tk_.ap[:, 1, h2 * 64:(h2 + 1) * 64], Usb.ap[:, h2, :], True, False, tk_.k() + Usb.k(), PK(bk))
                            MM(so, tk_.ap[:, 2, h2 * 64:(h2 + 1) * 64], tk_.ap[:, 0, h2 * 64:(h2 + 1) * 64], False, True, tk_.k(), PK(bk))
                        STT("dve", Sst.ap[:, hp, :], psum[:, bk, 0:64], pC.ap[:, c:c + 1], Stmp.ap, ALU.mult, ALU.add, PK(bk) + pC.k() + Stmp.k(), sk)
                        yield


                    s1_next = 0; s1_active = []; s1_done = set(); s2_c = 0; s2_gen = None; s2_done = 0
                    while s2_done < 8:
                        while len(s1_active) < 3 and s1_next < 8 and s1_next - s2_done < NSL:
                            s1_active.append((s1_next, stage1(s1_next, s1_next % NSL)))
                            s1_next += 1
                        for item in list(s1_active):
                            cc_, g_ = item
                            try:
                                next(g_)
                            except StopIteration:
                                s1_active.remove(item)
                                s1_done.add(cc_)
                        if s2_gen is None and s2_c < 8 and s2_c in s1_done:
                            s2_gen = stage2(s2_c, s2_c % NSL)
                        if s2_gen is not None:
                            try:
                                next(s2_gen)
                            except StopIteration:
                                s2_gen = None
                                s2_c += 1
                                s2_done += 1
                        yield

                def finalize(hp):
                    CP("dve", yT.ap, psum[:, 6, :], PK(6), yT.k())
                    TT("dve", yT.ap[64:128, :], psum[64:128, 7, :], yT.ap[64:128, :], ALU.add, PK(7) + yT.k(), yT.k())
                    bk = bank()
                    MM(psum[:, bk, :], onesbd.ap, yT.ap, True, True, onesbd.k() + yT.k(), PK(bk))
                    STT("dve", yT.ap, psum[:, bk, :], -1.0 / 64, yT.ap, ALU.mult, ALU.add, PK(bk) + yT.k(), yT.k())
                    TT("pool", gnt.ap, yT.ap, yT.ap, ALU.mult, yT.k(), gnt.k())
                    bk = bank()
                    MM(psum[:, bk, :], onesbd.ap, gnt.ap, True, True, onesbd.k() + gnt.k(), PK(bk))
                    TS("dve", gnt.ap, psum[:, bk, :], 1.0 / 64, ALU.mult, PK(bk), gnt.k(), s2=GN_EPS, op1=ALU.add)
                    ACT(gnt.ap, gnt.ap, AF.Sqrt, gnt.k(), gnt.k())
                    RECIP(gnt.ap, gnt.ap, gnt.k(), gnt.k())
                    TT("dve", yT.ap, yT.ap, gnt.ap, ALU.mult, yT.k() + gnt.k(), yT.k())
                    TS("dve", yT.ap, yT.ap, pp("gn_g", hp), ALU.mult, yT.k() + prm.k(), yT.k(), s2=pp("gn_b", hp), op1=ALU.add)
                    TT("pool", yT.ap, yT.ap, t_bon.ap, ALU.add, yT.k() + t_bon.k(), yT.k())
                    bk = bank()
                    MM(psum[:, bk, :], lora.ap[:, 1, hp * 128:(hp + 1) * 128], tgd1.ap, True, False, lora.ks(1) + tgd1.k(), PK(bk))
                    MM(psum[:, bk, :], lora.ap[0:32, 2, hp * 128:(hp + 1) * 128], tgd2.ap, False, True, lora.ks(2) + tgd2.k(), PK(bk))
                    TT("dve", yBT.ap[:, hp, :], psum[:, bk, :], yT.ap, ALU.mult, PK(bk) + yT.k(), yBT.ks(hp))

                for _ in prep1(0):
                    pass
                prep2(0)
                for hp in range(4):
                    gens = [scan_gen(hp)]
                    if hp + 1 < 4:
                        gens.append(prep1(hp + 1))
                    interleave(gens, 2)
                    finalize(hp)
                    if hp + 1 < 4:
                        prep2(hp + 1)

                for half in range(2):
                    t = wbuf()
                    mhv = t.ap.rearrange("p a b -> p (a b)").rearrange("p (w k n) -> p w k n", w=2, k=4)
                    cast_load(mhv[:, 0], w_a_h[l][:, half * 512:(half + 1) * 512].rearrange("(k p) n -> p k n", p=128), t.k())
                    cast_load(mhv[:, 1], w_b_h[l][:, half * 512:(half + 1) * 512].rearrange("(k p) n -> p k n", p=128), t.k())
                    mhk = t.k()
                    wga, wgak = wload(wv[:, :, 3360 + half * 512:3360 + (half + 1) * 512], [KC, 512])
                    wgb, wgbk = wload(wv[:, :, 4384 + half * 512:4384 + (half + 1) * 512], [KC, 512])
                    for cc in range(4):
                        c = half * 4 + cc
                        bka = bank()
                        for k in range(4):
                            MM(psum[:, bka, :], mhv[:, 0, k, cc * 128:(cc + 1) * 128], yAT.ap[:, k, :], k == 0, k == 3, mhk + yAT.ks(k), PK(bka))
                        bkg = proj_fm(wga, wgak, cc * 128)
                        ACT(sga.ap, psum[:, bkg, :], AF.Sigmoid, PK(bkg), sga.k())
                        TT("dve", m1.ap, psum[:, bka, :], sga.ap, ALU.mult, PK(bka) + sga.k(), m1.k())
                        bkb = bank()
                        for k in range(4):
                            MM(psum[:, bkb, :], mhv[:, 1, k, cc * 128:(cc + 1) * 128], yBT.ap[:, k, :], k == 0, k == 3, mhk + yBT.ks(k), PK(bkb))
                        bkg = proj_fm(wgb, wgbk, cc * 128)
                        ACT(sgb.ap, psum[:, bkg, :], AF.Sigmoid, PK(bkg), sgb.k())
                        TT("dve", m2.ap, psum[:, bkb, :], sgb.ap, ALU.mult, PK(bkb) + sgb.k(), m2.k())
                        TT("pool", zT.ap[:, c, :], m1.ap, m2.ap, ALU.add, m1.k() + m2.k(), zT.ks(c))
                wo = []
                for kh in range(2):
                    wo.append(wload(w_out_h[l][kh * 512:(kh + 1) * 512, :].rearrange("(k p) n -> p k n", p=128), [4, 1024]))
                def ln1_tile(tt):
                    ti = b * 4 + tt
                    hbuf = hbufs[tt % 2]; x0 = x0s[tt % 2]; x1fm = x1fms[tt % 2]
                    st6 = st6s[tt % 2]; mv4 = mv4s[tt % 2]; lg = lgs[tt % 2]; rt = rts[tt % 2]
                    DMA("sp", x0.ap, xs_d[ti * 128:(ti + 1) * 128, :], [("xs", ti)], x0.k())
                    for nh in range(2):
                        bk = bank()
                        for k in range(KC):
                            wvv, wkk = wo[k // 4]
                            MM(psum[:, bk, :], zT.ap[:, k, tt * 128:(tt + 1) * 128], wvv[:, k % 4, nh * 512:(nh + 1) * 512], k == 0, k == KC - 1,
                               zT.ks(k) + wkk, PK(bk))
                        STT("dve", hbuf.ap[:, nh * 512:(nh + 1) * 512], x0.ap[:, nh * 512:(nh + 1) * 512], ALPHA, psum[:, bk, :], ALU.mult, ALU.add,
                            x0.k() + PK(bk), hbuf.k())
                    yield
                    yield from layer_norm_g(hbuf.ap, hbuf.k(), hbuf.ap, hbuf.k(), st6, mv4)
                    DMA("sp", xs_d[ti * 128:(ti + 1) * 128, :], hbuf.ap, hbuf.k(), [("xs", ti)])
                    to_feature_major(hbuf.ap, hbuf.k(), ti * 128, f32_out=x1fm)
                    yield
                    yield from router_g(l, ti, x1fm, lg, rt)

                interleave([ln1_tile(tt) for tt in range(4)], 2)

        def router_g(l, ti, x1fm, lg, rt):
            bk = bank()
            for k in range(KC):
                MM(psum[:, bk, 0:36], x1fm.ap[:, k, :], rw.ap[:, k, :], k == 0, k == KC - 1, x1fm.ks(k) + rw.k(), PK(bk))
            TT("dve", lg.ap, psum[:, bk, 0:36], rb.ap, ALU.add, PK(bk) + rb.k(), lg.k())
            K = lg.k() + rt.k()
            r = rt.ap
            g4 = lg.ap[:, 0:4]
            P.add("dve", lambda e: e.reduce_max(out=r[:, 0:1], in_=g4, axis=mybir.AxisListType.X), K, K)
            TS("dve", r[:, 4:8], g4, r[:, 0:1], ALU.subtract, K, K)
            ACT(r[:, 4:8], r[:, 4:8], AF.Exp, K, K)
            P.add("dve", lambda e: e.reduce_sum(out=r[:, 1:2], in_=r[:, 4:8], axis=mybir.AxisListType.X), K, K)
            RECIP(r[:, 1:2], r[:, 1:2], K, K)
            yield
            TS("dve", r[:, 8:12], g4, r[:, 0:1], ALU.is_ge, K, K)
            ev = lg.ap[:, 4:36].rearrange("p (g e) -> p g e", e=8)
            TT("dve", r[:, 32:64].rearrange("p (g e) -> p g e", e=8), ev, r[:, 8:12].rearrange("p (g o) -> p g o", o=1).to_broadcast([128, 4, 8]),
               ALU.mult, K, K)
            P.add("dve", lambda e: e.tensor_reduce(out=r[:, 12:20], in_=r[:, 32:64].rearrange("p (g e) -> p e g", e=8), axis=mybir.AxisListType.X,
                                                   op=ALU.add), K, K)
            sel = r[:, 12:20]
            P.add("dve", lambda e: e.reduce_max(out=r[:, 2:3], in_=sel, axis=mybir.AxisListType.X), K, K)
            TS("dve", r[:, 20:28], sel, r[:, 2:3], ALU.is_ge, K, K)
            STT("dve", r[:, 32:40], r[:, 20:28], -1e30, sel, ALU.mult, ALU.add, K, K)
            P.add("dve", lambda e: e.reduce_max(out=r[:, 3:4], in_=r[:, 32:40], axis=mybir.AxisListType.X), K, K)
            TS("dve", r[:, 40:48], sel, r[:, 3:4], ALU.is_ge, K, K)
            yield
            TT("dve", r[:, 48:49], r[:, 3:4], r[:, 2:3], ALU.subtract, K, K)
            ACT(r[:, 48:49], r[:, 48:49], AF.Exp, K, K)
            TS("dve", r[:, 48:49], r[:, 48:49], 1.0, ALU.add, K, K)
            RECIP(r[:, 49:50], r[:, 48:49], K, K)
            yield
            TS("dve", r[:, 50:51], r[:, 49:50], -1.0, ALU.mult, K, K, s2=1.0, op1=ALU.add)
            TT("dve", r[:, 49:51], r[:, 49:51], r[:, 1:2].to_broadcast([128, 2]), ALU.mult, K, K)
            TT("dve", r[:, 40:48], r[:, 40:48], r[:, 20:28], ALU.subtract, K, K)
            TS("dve", r[:, 40:48], r[:, 40:48], r[:, 50:51], ALU.mult, K, K)
            STT("dve", r[:, 40:48], r[:, 20:28], r[:, 49:50], r[:, 40:48], ALU.mult, ALU.add, K, K)
            cv = comb.ap[:, ti, :].rearrange("p (g e) -> p g e", e=8)
            TT("dve", cv, r[:, 8:12].rearrange("p (g o) -> p g o", o=1).to_broadcast([128, 4, 8]),
               r[:, 40:48].rearrange("p (o e) -> p o e", o=1).to_broadcast([128, 4, 8]), ALU.mult, K, comb.ks(ti))
            yield

        def phaseB(s, l, last):
            A.cur = mark_common
            acc = A.alloc([NT, D])
            xT = A.alloc([KC, S], BF16)
            for c in range(KC):
                DMA("sp", xT.ap[:, c, :], xT_d[c], [("xTd", i) for i in range(NT)], xT.ks(c))
            hT = [A.alloc([2, 512], BF16) for _ in range(2)]
            sil = [A.alloc([512]) for _ in range(2)]
            x1s = [A.alloc([D]) for _ in range(2)]; st6s = [A.alloc([2, 6]) for _ in range(2)]; mv4s = [A.alloc([4]) for _ in range(2)]
            pts = [A.alloc([256]) for _ in range(2)]; pTs = [A.alloc([2, 128], BF16) for _ in range(2)]
            gsigs = [A.alloc([D]) for _ in range(2)]; lnp3 = A.alloc([2, D])
            assert Sst.off + Sst.n - lora.off >= 2048
            stgB = [Tile(A.ap, lora.off + i * 1024, 1024, F32, [1024]) for i in range(2)]
            sbc = [0]

            def stage_cast(dst, src, dkeys):
                a, b_ = src.shape[1], src.shape[2]
                step = max(1, 1024 // b_)
                for a0 in range(0, a, step):
                    a1 = min(a, a0 + step)
                    i = sbc[0]; sbc[0] += 1
                    st_ = stgB[i % 2]
                    sv = st_.ap[:, 0:(a1 - a0) * b_].rearrange("p (a b) -> p a b", b=b_)
                    DMA("sp", sv, src[:, a0:a1, :], (), st_.k())
                    CP("pool", dst[:, a0:a1, :], sv, st_.k(), dkeys)
            print("phaseB arena words", A.cur, "of", A.nwords)
            def load_expert(e):
                t = wbuf()
                gu = t.ap
                stage_cast(gu[:, :, 0:256], exp_gate[l, e].rearrange("(k p) n -> p k n", p=128), t.k())
                stage_cast(gu[:, :, 256:512], exp_up[l, e].rearrange("(k p) n -> p k n", p=128), t.k())
                t2 = wbuf()
                wd = t2.ap.rearrange("p a b -> p (a b)")[:, 0:2048].rearrange("p (k n) -> p k n", k=2)
                stage_cast(wd, exp_down[l, e].rearrange("(k p) n -> p k n", p=128), t2.k())
                return gu, t.k(), wd, t2.k()

            nxt_w = load_expert(0)
            for e in range(32):
                gu, guk, wd, wdk = nxt_w
                if e + 1 < 32:
                    nxt_w = load_expert(e + 1)
                for b in range(NB):
                    t0 = b * 512
                    xk = []
                    for c in range(KC):
                        xk += xT.k(c * S + t0, c * S + t0 + 512)
                    ht = hT[b % 2]
                    for dc in range(2):
                        bg = bank()
                        for k in range(KC):
                            MM(psum[:, bg, :], gu[:, k, dc * 128:(dc + 1) * 128], xT.ap[:, k, t0:t0 + 512], k == 0, k == KC - 1, guk + xk, PK(bg))
                        bu = bank()
                        for k in range(KC):
                            MM(psum[:, bu, :], gu[:, k, 256 + dc * 128:256 + (dc + 1) * 128], xT.ap[:, k, t0:t0 + 512], k == 0, k == KC - 1, guk + xk, PK(bu))
                        sl = sil[dc]
                        ACT(sl.ap, psum[:, bg, :], AF.Silu, PK(bg), sl.k())
                        TT("dve", ht.ap[:, dc, :], psum[:, bu, :], sl.ap, ALU.mult, PK(bu) + sl.k(), ht.ks(dc))
                    for tt in range(4):
                        ti = b * 4 + tt
                        for nh in range(2):
                            bk = bank()
                            for k in range(2):
                                MM(psum[:, bk, :], ht.ap[:, k, tt * 128:(tt + 1) * 128], wd[:, k, nh * 512:(nh + 1) * 512], k == 0, k == 1, ht.ks(k) + wdk, PK(bk))
                            av = acc.ap[:, ti, nh * 512:(nh + 1) * 512]
                            ak = acc.k(ti * D + nh * 512, ti * D + (nh + 1) * 512)
                            if e == 0:
                                TS("dve", av, psum[:, bk, :], comb.ap[:, ti, e:e + 1], ALU.mult, PK(bk) + comb.ks(ti), ak)
                            else:
                                STT("dve", av, psum[:, bk, :], comb.ap[:, ti, e:e + 1], av, ALU.mult, ALU.add, PK(bk) + comb.ks(ti) + ak, ak)
            wpg = []
            for kh in range(2):
                wpg.append(wload(ple_gate_h[l][kh * 512:(kh + 1) * 512, :].rearrange("(k p) n -> p k n", p=128), [4, 1024]))
            wpp, wppk = wload(ple_proj_h[l].rearrange("(k p) n -> p k n", p=128), [2, 1024])
            load_ln(ln_g[l, 1], ln_b[l, 1])
            load_ln(ln_g[l, 2], ln_b[l, 2], lnp3)
            def ep_tile(ti):
                hk = acc.ks(ti)
                h = acc.ap[:, ti, :]
                x1 = x1s[ti % 2]; st6 = st6s[ti % 2]; mv4 = mv4s[ti % 2]; pt = pts[ti % 2]; pT = pTs[ti % 2]; gsig = gsigs[ti % 2]
                DMA("sp", x1.ap, xs_d[ti * 128:(ti + 1) * 128, :], [("xs", ti)], x1.k())
                DMA("sp", pt.ap, p_d[l, s, ti * 128:(ti + 1) * 128, :], (), pt.k())
                STT("dve", h, x1.ap, ALPHA, h, ALU.mult, ALU.add, x1.k() + hk, hk)
                yield from layer_norm_g(h, hk, h, hk, st6, mv4)
                to_feature_major(h, hk, ti * 128, sb=(xT, S, ti * 128), to_dram=False)
                bk = bank()
                for c in range(2):
                    TR(psum[:, bk, c * 128:(c + 1) * 128], pt.ap[:, c * 128:(c + 1) * 128], ident.ap, pt.k() + KI, PK(bk))
                CP("dve", pT.ap, psum[:, bk, 0:256].rearrange("p (c t) -> p c t", t=128), PK(bk), pT.k())
                yield
                xk = []
                for c in range(KC):
                    xk += xT.k(c * S + ti * 128, c * S + ti * 128 + 128)
                for nh in range(2):
                    bk = bank()
                    for k in range(KC):
                        wvv, wkk = wpg[k // 4]
                        MM(psum[:, bk, :], xT.ap[:, k, ti * 128:(ti + 1) * 128], wvv[:, k % 4, nh * 512:(nh + 1) * 512], k == 0, k == KC - 1, xk + wkk, PK(bk))
                    ACT(gsig.ap[:, nh * 512:(nh + 1) * 512], psum[:, bk, :], AF.Sigmoid, PK(bk), gsig.k())
                    bk = bank()
                    for k in range(2):
                        MM(psum[:, bk, :], pT.ap[:, k, :], wpp[:, k, nh * 512:(nh + 1) * 512], k == 0, k == 1, pT.k() + wppk, PK(bk))
                    TT("dve", gsig.ap[:, nh * 512:(nh + 1) * 512], psum[:, bk, :], gsig.ap[:, nh * 512:(nh + 1) * 512], ALU.mult, PK(bk) + gsig.k(), gsig.k())
                    yield
                STT("dve", h, h, ALPHA, gsig.ap, ALU.mult, ALU.add, hk + gsig.k(), hk)
                yield from layer_norm_g(h, hk, h, hk, st6, mv4, lnp3)
                if last:
                    DMA("sp", y_d[s, ti * 128:(ti + 1) * 128, :], h, hk, [("y", s, ti)])
                else:
                    DMA("sp", xs_d[ti * 128:(ti + 1) * 128, :], h, hk, [("xs", ti)])
                    to_feature_major(h, hk, ti * 128)
                yield

            interleave([ep_tile(ti) for ti in range(NT)], 2)

        for s in range(NSEQ if STOP != "c" else 0):
            A.cur = mark_common
            load_ln(ln_in_g, ln_in_b)
            p0_x = [A.alloc([D]) for _ in range(2)]
            p0_st = A.alloc([2, 6]); p0_mv = A.alloc([4])
            for i in range(NT):
                xt = p0_x[i % 2]
                DMA("sp", xt.ap, x_d[s, i * 128:(i + 1) * 128, :], (), xt.k())
                if BIS != 1:
                    layer_norm(xt.ap, xt.k(), xt.ap, xt.k(), p0_st, p0_mv)
                DMA("sp", xs_d[i * 128:(i + 1) * 128, :], xt.ap, xt.k(), [("xs", i)])
                if BIS not in (1, 2):
                    to_feature_major(xt.ap, xt.k(), i * 128)
            if s == 0:
                precast()
                cast_load(biasT.ap, bias_h.rearrange("h j k q -> k (h j) q"), biasT.k())
            if STOP == "p0":
                break
            for l in range(DEPTH):
                phaseA(s, l)
                if STOP == "A":
                    break
                phaseB(s, l, l == DEPTH - 1)
                if STOP == "B":
                    break
            if STOP:
                break
        P.emit()
    return nc


REL_CLIP = 128
_NC_CACHE = {}


def _bias_table(rel_bias):
    k = np.arange(128)[:, None]
    q = np.arange(128)[None, :]
    tabs = []
    for j in range(5):
        dist = q - k + 128 * (4 - j)
        idx = np.clip(dist, -REL_CLIP, REL_CLIP) + REL_CLIP
        t = rel_bias[:, idx]
        kc = (k // 64) + 2 * (j - 4)
        qc = q // 64
        valid = (kc <= qc) & (kc >= qc - 8)
        t = np.where(valid[None], t, np.float32(-1e30))
        tabs.append(t)
    return np.ascontiguousarray(np.stack(tabs, axis=1).astype(np.float32))


def prep_inputs(inputs, S, NSEQ, ncores):
    f = lambda a: np.ascontiguousarray(np.asarray(a, dtype=np.float32))
    shared = {
        "ln_in_g": f(inputs["ln_in_g"]), "ln_in_b": f(inputs["ln_in_b"]),
        "bias_tab": _bias_table(f(inputs["rel_bias"])),
        "w_in": f(inputs["w_in"]), "tok_mix": f(inputs["tok_mix"]),
        "decay_base": f(inputs["decay_base"]), "decay_up": f(inputs["decay_up"]),
        "aaa_base": f(inputs["aaa_base"]), "aaa_up": f(inputs["aaa_up"]), "gate_up": f(inputs["gate_up"]),
        "k_k": f(inputs["k_k"]), "k_a": f(inputs["k_a"]), "r_k": f(inputs["r_k"]).reshape(-1, 512),
        "vres_base": f(inputs["vres_base"]), "vres_down": f(inputs["vres_down"]), "vres_up": f(inputs["vres_up"]),
        "gn_g": f(inputs["gn_g"]), "gn_b": f(inputs["gn_b"]),
        "w_branch_attn": f(inputs["w_branch_attn"]), "w_branch_rwkv": f(inputs["w_branch_rwkv"]), "w_out": f(inputs["w_out"]),
        "router_w": np.ascontiguousarray(np.concatenate([f(inputs["router_grp"]), f(inputs["router_exp"])], axis=-1)),
        "router_b": np.ascontiguousarray(np.concatenate([f(inputs["router_grp_bias"]), f(inputs["router_exp_bias"])], axis=-1)),
        "exp_gate": f(inputs["exp_gate"]), "exp_up": f(inputs["exp_up"]), "exp_down": f(inputs["exp_down"]),
        "ple_proj": f(inputs["ple_proj"]), "ple_gate": f(inputs["ple_gate"]),
        "ln_g": f(inputs["ln_g"]), "ln_b": f(inputs["ln_b"]),
    }
    x = f(inputs["x"]); p = f(inputs["p"])
    maps = []
    for c in range(ncores):
        m = dict(shared)
        m["x"] = np.ascontiguousarray(x[c * NSEQ:(c + 1) * NSEQ])
        m["p"] = np.ascontiguousarray(p[:, c * NSEQ:(c + 1) * NSEQ])
        maps.append(m)
    return maps


def kernel(**inputs):
    x = np.asarray(inputs["x"])
    B, S, _ = x.shape
    ncores = 8
    NSEQ = B // ncores
    key = (S, NSEQ)
    if key not in _NC_CACHE:
        _NC_CACHE[key] = build_nc(S, NSEQ)
    nc = _NC_CACHE[key]
    maps = prep_inputs(inputs, S, NSEQ, ncores)
    res = run_bass_kernel_spmd(nc, maps, core_ids=list(range(ncores)))
    return np.concatenate([np.asarray(r["y"], dtype=np.float32) for r in res.results], axis=0)
```

```python
import contextlib
import numpy as np
import concourse.bass as bass
import concourse.mybir as mybir
from concourse.bass_utils import run_bass_kernel_spmd

F32 = mybir.dt.float32
BF16 = mybir.dt.bfloat16
AF = mybir.ActivationFunctionType
ALU = mybir.AluOpType

D = 1024
KC = 8
NIN = 5408
ALPHA = 4.0 ** 0.25
LN_EPS = 1e-5
GN_EPS = 64e-5
C0 = float(np.exp(-0.5))
N_DMA_SEMS = 40
SAME_ENGINE_SYNC = True
GRAN = 64
STOP = None
ABLK = None
BIS = 0


class Op:
    __slots__ = ("eng", "fn", "deps", "dma", "idx", "signal", "tick", "dsem", "dval", "prev_dval", "sw", "gen")

    def __init__(self, eng, fn, dma):
        self.eng = eng
        self.fn = fn
        self.dma = dma
        self.deps = set()
        self.signal = False
        self.tick = 0
        self.dsem = None
        self.dval = 0
        self.prev_dval = 0
        self.sw = None
        self.gen = 0


class Prog:
    ENGS = ("pe", "act", "dve", "pool", "sp")

    def __init__(self, nc):
        self.nc = nc
        self.ops = []
        self.res = {}
        self.ndma = 0
        self.swgen = {}

    def add(self, eng, fn, reads=(), writes=(), dma=False, sw=None):
        op = Op(eng, fn, dma)
        op.idx = len(self.ops)
        res = self.res
        psr = [r for r in reads if r[0] == "ps"]
        if psr:
            reads = [r for r in reads if r[0] != "ps"]
            writes = list(writes) + psr
        for r in reads:
            st = res.get(r)
            if st is not None and st[0] is not None:
                op.deps.add(st[0])
        for w in writes:
            st = res.get(w)
            if st is not None:
                if st[0] is not None:
                    op.deps.add(st[0])
                op.deps.update(st[1])
        for r in reads:
            st = res.get(r)
            if st is None:
                res[r] = [None, [op.idx]]
            else:
                st[1].append(op.idx)
        for w in writes:
            res[w] = [op.idx, []]
        op.deps.discard(op.idx)
        if dma and sw is not None:
            op.sw = sw
            op.gen = self.swgen.get(sw, 0)
            self.swgen[sw] = op.gen + 1
        elif dma:
            op.dsem = self.ndma % N_DMA_SEMS
            self.ndma += 1
        self.ops.append(op)
        return op

    def emit(self):
        nc = self.nc
        ops = self.ops
        for op in ops:
            for d in op.deps:
                dop = ops[d]
                if dop.dma:
                    continue
                if dop.eng == op.eng and (dop.eng == "pe" or not SAME_ENGINE_SYNC):
                    continue
                dop.signal = True
        ticks = {e: 0 for e in self.ENGS}
        dcount = [0] * N_DMA_SEMS
        for op in ops:
            if op.dma and op.sw is not None:
                continue
            if op.dma:
                op.prev_dval = dcount[op.dsem]
                dcount[op.dsem] += 16
                op.dval = dcount[op.dsem]
            elif op.signal:
                ticks[op.eng] += 1
                op.tick = ticks[op.eng]
        per_eng = {e: [o for o in ops if o.eng == e] for e in self.ENGS}
        with contextlib.ExitStack() as es:
            esem = {e: es.enter_context(nc.semaphore("s_" + e)) for e in ("pe", "act", "dve", "pool")}
            dsems = [es.enter_context(nc.semaphore("d%d" % i)) for i in range(N_DMA_SEMS)]
            swsems = {}
            for i, slot in enumerate(sorted(self.swgen.keys(), key=str)):
                swsems[slot] = [es.enter_context(nc.semaphore("w%d_%d" % (i, j))) for j in range(2)]
            block = es.enter_context(nc.Block())

            def run(engname, eng):
                waited = {}

                def wait(key, sem, val):
                    if waited.get(key, 0) >= val:
                        return
                    eng.wait_ge(sem, val)
                    waited[key] = val

                for op in per_eng[engname]:
                    for d in sorted(op.deps):
                        dop = ops[d]
                        if dop.dma and dop.sw is not None:
                            wait(("sw", dop.sw, dop.gen), swsems[dop.sw][dop.gen % 2], 16)
                        elif dop.dma:
                            wait(("d", dop.dsem), dsems[dop.dsem], dop.dval)
                        else:
                            if dop.eng == engname and (engname == "pe" or not SAME_ENGINE_SYNC):
                                continue
                            wait(("e", dop.eng), esem[dop.eng], dop.tick)
                    if op.dma and op.sw is not None:
                        if op.gen >= 1:
                            wait(("sw", op.sw, op.gen - 1), swsems[op.sw][(op.gen - 1) % 2], 16)
                            eng.sem_clear(swsems[op.sw][(op.gen - 1) % 2])
                        op.fn(eng).then_inc(swsems[op.sw][op.gen % 2], 16)
                    elif op.dma:
                        if op.prev_dval > 0:
                            wait(("d", op.dsem), dsems[op.dsem], op.prev_dval)
                        op.fn(eng).then_inc(dsems[op.dsem], 16)
                    else:
                        ins = op.fn(eng)
                        if op.signal:
                            ins.then_inc(esem[engname], 1)
                for op in per_eng[engname]:
                    if op.dma and op.sw is None:
                        wait(("d", op.dsem), dsems[op.dsem], op.dval)

            if per_eng["pe"]:
                block.tensor(lambda e: run("pe", e))
            if per_eng["act"]:
                block.scalar(lambda e: run("act", e))
            if per_eng["dve"]:
                block.vector(lambda e: run("dve", e))
            if per_eng["pool"]:
                block.gpsimd(lambda e: run("pool", e))
            if per_eng["sp"]:
                block.sync(lambda e: run("sp", e))


class Tile:
    def __init__(self, arena, off, nwords, dtype, shape, parts=128):
        self.off = off
        self.n = nwords
        self.esz = 4 if dtype == F32 else 2
        ne = int(np.prod(shape))
        nwx = ne if dtype == F32 else (ne + 1) // 2
        v = arena[0:parts, off:off + nwx]
        if dtype != F32:
            v = v.bitcast(dtype)[:, 0:ne]
        self.shape = tuple(shape)
        if len(shape) == 2:
            v = v.rearrange("p (a b) -> p a b", b=shape[1])
        elif len(shape) == 3:
            v = v.rearrange("p (a b c) -> p a b c", b=shape[1], c=shape[2])
        self.ap = v
        self.inner = int(np.prod(shape[1:])) if len(shape) > 1 else 1

    def k(self, lo=None, hi=None):
        if lo is None:
            lo, hi = 0, self.n * 4 // self.esz
        w0 = self.off + (lo * self.esz) // 4
        w1 = self.off + (hi * self.esz + 3) // 4
        return [("sb", g) for g in range(w0 // GRAN, (w1 - 1) // GRAN + 1)]

    def ks(self, i, n=1):
        return self.k(i * self.inner, (i + n) * self.inner)


class Arena:
    def __init__(self, nc, es, nwords):
        self.t = es.enter_context(nc.sbuf_tensor("arena", [128, nwords], F32))
        self.ap = self.t[:]
        self.nwords = nwords
        self.cur = 0

    def alloc(self, shape, dtype=F32, parts=128):
        ne = int(np.prod(shape))
        nw = ne if dtype == F32 else (ne + 1) // 2
        nw = (nw + GRAN - 1) // GRAN * GRAN
        assert self.cur + nw <= self.nwords, ("arena overflow", self.cur, nw, self.nwords)
        t = Tile(self.ap, self.cur, nw, dtype, shape, parts)
        self.cur += nw
        return t


def build_nc(S, NSEQ, DEPTH=2, dbg=None):
    NB = S // 512
    NT = S // 128
    nc = bass.Bass("TRN2", target_bir_lowering=False)
    dram = lambda name, shape, kind="ExternalInput": nc.dram_tensor(name, shape, F32, kind=kind).ap()
    x_d = dram("x", [NSEQ, S, D])
    p_d = dram("p", [DEPTH, NSEQ, S, 256])
    ln_in_g = dram("ln_in_g", [D]); ln_in_b = dram("ln_in_b", [D])
    bias_d = dram("bias_tab", [8, 5, 128, 128])
    w_in = dram("w_in", [DEPTH, D, NIN])
    tok_mix = dram("tok_mix", [DEPTH, 1824])
    decay_base = dram("decay_base", [DEPTH, 512]); decay_up = dram("decay_up", [DEPTH, 64, 512])
    aaa_base = dram("aaa_base", [DEPTH, 512]); aaa_up = dram("aaa_up", [DEPTH, 64, 512])
    gate_up = dram("gate_up", [DEPTH, 160, 512])
    k_k = dram("k_k", [DEPTH, 512]); k_a = dram("k_a", [DEPTH, 512]); r_k = dram("r_k", [DEPTH, 512])
    vres_base = dram("vres_base", [DEPTH - 1, 512]); vres_down = dram("vres_down", [DEPTH - 1, 512, 32])
    vres_up = dram("vres_up", [DEPTH - 1, 32, 512])
    gn_g = dram("gn_g", [DEPTH, 512]); gn_b = dram("gn_b", [DEPTH, 512])
    w_a = dram("w_branch_attn", [DEPTH, 512, D]); w_b = dram("w_branch_rwkv", [DEPTH, 512, D])
    w_out = dram("w_out", [DEPTH, D, D])
    router_w = dram("router_w", [DEPTH, D, 36]); router_b = dram("router_b", [DEPTH, 36])
    exp_gate = dram("exp_gate", [DEPTH, 32, D, 256]); exp_up = dram("exp_up", [DEPTH, 32, D, 256])
    exp_down = dram("exp_down", [DEPTH, 32, 256, D])
    ple_proj = dram("ple_proj", [DEPTH, 256, D]); ple_gate = dram("ple_gate", [DEPTH, D, D])
    ln_g = dram("ln_g", [DEPTH, 3, D]); ln_b = dram("ln_b", [DEPTH, 3, D])
    y_d = dram("y", [NSEQ, S, D], kind="ExternalOutput")
    xs_d = dram("xs_scr", [S, D], kind="Internal")
    dramh = lambda name, shape: nc.dram_tensor(name, shape, BF16, kind="Internal").ap()
    xT_d = dramh("xT_scr", [KC, 128, S])
    w_in_h = dramh("w_in_h", [DEPTH, D, NIN]); w_a_h = dramh("w_a_h", [DEPTH, 512, D]); w_b_h = dramh("w_b_h", [DEPTH, 512, D])
    w_out_h = dramh("w_out_h", [DEPTH, D, D]); exp_gate_h = dramh("exp_gate_h", [DEPTH, 32, D, 256])
    exp_up_h = dramh("exp_up_h", [DEPTH, 32, D, 256]); exp_down_h = dramh("exp_down_h", [DEPTH, 32, 256, D])
    ple_proj_h = dramh("ple_proj_h", [DEPTH, 256, D]); ple_gate_h = dramh("ple_gate_h", [DEPTH, D, D])
    bias_h = dramh("bias_h", [8, 5, 128, 128])
    vf_d = dram("vf_scr", [4, 128, S], kind="Internal")
    dbg_d = {}
    if dbg:
        for name, shape in dbg.items():
            dbg_d[name] = dram("dbg_" + name, shape, kind="ExternalOutput")

    P = Prog(nc)
    es = contextlib.ExitStack()
    with es:
        A = Arena(nc, es, 53000)
        psum = es.enter_context(nc.psum_tensor("ps", [128, 8, 512], F32))
        bank_ctr = [0]

        def bank():
            b = bank_ctr[0] % 6
            bank_ctr[0] += 1
            return b

        def bank2():
            b = bank_ctr[0] % 6
            if b % 2:
                bank_ctr[0] += 1
                b = bank_ctr[0] % 6
            bank_ctr[0] += 2
            return b

        PK = lambda b: [("ps", b)]

        def MM(out, lhsT, rhs, start, stop, reads, writes):
            P.add("pe", lambda e: e.matmul(out, lhsT=lhsT, rhs=rhs, start=start, stop=stop), reads, writes)

        def TR(out, in_, ident, reads, writes):
            P.add("pe", lambda e: e.transpose(out, in_, ident), reads, writes)

        def ACT(out, in_, func, reads, writes, bias=None, scale=None):
            kw = {}
            if bias is not None:
                kw["bias"] = bias
            if scale is not None:
                kw["scale"] = scale
            P.add("act", lambda e: e.activation(out=out, in_=in_, func=func, **kw), reads, writes)

        def TT(eng, out, in0, in1, op, reads, writes):
            P.add(eng, lambda e: e.tensor_tensor(out=out, in0=in0, in1=in1, op=op), reads, writes)

        def TS(eng, out, in0, s1, op0, reads, writes, s2=None, op1=None):
            if op1 is None:
                P.add(eng, lambda e: e.tensor_scalar(out=out, in0=in0, scalar1=s1, scalar2=None, op0=op0), reads, writes)
            else:
                P.add(eng, lambda e: e.tensor_scalar(out=out, in0=in0, scalar1=s1, scalar2=s2, op0=op0, op1=op1), reads, writes)

        def STT(eng, out, in0, scalar, in1, op0, op1, reads, writes):
            P.add(eng, lambda e: e.scalar_tensor_tensor(out=out, in0=in0, scalar=scalar, in1=in1, op0=op0, op1=op1), reads, writes)

        def CP(eng, out, in_, reads, writes):
            if eng == "act":
                P.add("dve", lambda e: e.tensor_copy(out=out, in_=in_), reads, writes)
            else:
                P.add(eng, lambda e: e.tensor_copy(out=out, in_=in_), reads, writes)

        def RECIP(out, in_, reads, writes):
            P.add("dve", lambda e: e.reciprocal(out=out, in_=in_), reads, writes)

        def MEMSET(eng, ap, val, writes):
            P.add(eng, lambda e: e.memset(ap, val), (), writes)

        def DMA(eng, out, in_, reads, writes, sw=None, **kw):
            P.add(eng, lambda e: e.dma_start(out=out, in_=in_, **kw), reads, writes, dma=True, sw=sw)

        ident = A.alloc([128]); identb = A.alloc([128], BF16)
        onesbd = A.alloc([128])
        maskU = A.alloc([4, 128], parts=64)
        maskL = A.alloc([2, 64], parts=64)
        identT = A.alloc([2, 64], parts=64)
        rmask = A.alloc([512])
        biasT = A.alloc([40, 128], BF16)
        KI = ident.k(); KIB = identb.k()
        MEMSET("pool", ident.ap, 0.0, KI)
        P.add("pool", lambda e: e.affine_select(out=ident.ap, in_=ident.ap, pattern=[[-1, 128]], compare_op=ALU.not_equal,
                                                fill=1.0, base=0, channel_multiplier=1), KI, KI)
        CP("pool", identb.ap, ident.ap, KI, KIB)
        MEMSET("pool", onesbd.ap, 0.0, onesbd.k())
        MEMSET("pool", onesbd.ap[0:64, 0:64], 1.0, onesbd.k())
        MEMSET("pool", onesbd.ap[64:128, 64:128], 1.0, onesbd.k())
        MEMSET("pool", maskU.ap, 1.0, maskU.k())
        for q in range(4):
            P.add("pool", (lambda q: lambda e: e.affine_select(out=maskU.ap[:, q, 0:64], in_=maskU.ap[:, q, 0:64], pattern=[[1, 64]],
                  compare_op=ALU.is_ge, fill=0.0, base=-1, channel_multiplier=-1))(q), maskU.k(), maskU.k())
            P.add("pool", (lambda q: lambda e: e.affine_select(out=maskU.ap[:, q, 64:128], in_=maskU.ap[:, q, 64:128], pattern=[[1, 64]],
                  compare_op=ALU.is_ge, fill=0.0, base=0, channel_multiplier=-1))(q), maskU.k(), maskU.k())
        MEMSET("pool", maskL.ap, 1.0, maskL.k())
        for q in range(2):
            P.add("pool", (lambda q: lambda e: e.affine_select(out=maskL.ap[:, q, :], in_=maskL.ap[:, q, :], pattern=[[-1, 64]],
                  compare_op=ALU.is_ge, fill=0.0, base=-1, channel_multiplier=1))(q), maskL.k(), maskL.k())
        CP("pool", identT.ap[:, 0, :], ident.ap[0:64, 0:64], KI, identT.k())
        CP("pool", identT.ap[:, 1, :], ident.ap[0:64, 0:64], KI, identT.k())
        MEMSET("pool", rmask.ap, 1.0, rmask.k())
        MEMSET("pool", rmask.ap.rearrange("p (c t) -> p c t", t=64)[:, :, 0:1], 0.0, rmask.k())

        xstg = [A.alloc([KC, 128], BF16) for _ in range(2)]
        xsc = [0]
        NWB = 4
        wpool = [A.alloc([KC, 512], BF16) for _ in range(NWB)]
        wctr = [0]

        def wbuf():
            t = wpool[wctr[0] % NWB]
            t.idx = wctr[0] % NWB
            wctr[0] += 1
            return t

        def cast_load(dst, src, dkeys):
            DMA("sp", dst, src, ["wh_all"], dkeys)

        lnp = A.alloc([2, D])
        comb = A.alloc([NT, 32])
        prm = A.alloc([16, 4])
        PRM = {n: i for i, n in enumerate(["decay_base", "aaa_base", "k_k", "k_a", "r_k", "gn_g", "gn_b", "vres_base"])}
        mu = A.alloc([2, 16])
        lora = A.alloc([3, 512])
        vrd = A.alloc([4, 32]); vru = A.alloc([512], parts=32)
        rw = A.alloc([KC, 36]); rb = A.alloc([36])
        lastc = A.alloc([16])
        Sst = A.alloc([4, 64])

        mark_common = A.cur

        def precast():
            stg = [A.alloc([4096]) for _ in range(4)]
            outb = [A.alloc([4096], BF16) for _ in range(4)]
            cnt = [0]
            allk = []

            def chunk(src3, dst3):
                a, b_ = src3.shape[1], src3.shape[2]
                i = cnt[0]; cnt[0] += 1
                st_ = stg[i % 4]; ob = outb[i % 4]
                sv = st_.ap[:, 0:a * b_].rearrange("p (a b) -> p a b", b=b_)
                ov = ob.ap[:, 0:a * b_].rearrange("p (a b) -> p a b", b=b_)
                DMA("sp", sv, src3, (), st_.k())
                CP("pool" if i % 4 == 3 else "dve", ov, sv, st_.k(), ob.k())
                key = ("whc", i)
                DMA("sp", dst3, ov, ob.k(), [key])
                allk.append(key)

            def cast2d(src2, dst2):
                R, C = src2.shape
                nrb = R // 128
                if C <= 4096:
                    Astep = max(1, 4096 // C)
                    sv = src2.rearrange("(a p) n -> p a n", p=128)
                    dv = dst2.rearrange("(a p) n -> p a n", p=128)
                    for a0 in range(0, nrb, Astep):
                        a1 = min(nrb, a0 + Astep)
                        chunk(sv[:, a0:a1, :], dv[:, a0:a1, :])
                else:
                    sv = src2.rearrange("(a p) n -> p a n", p=128)
                    dv = dst2.rearrange("(a p) n -> p a n", p=128)
                    for a0 in range(nrb):
                        for c0 in range(0, C, 4096):
                            c1 = min(C, c0 + 4096)
                            chunk(sv[:, a0:a0 + 1, c0:c1], dv[:, a0:a0 + 1, c0:c1])

            cast2d(w_in.rearrange("l k n -> (l k) n"), w_in_h.rearrange("l k n -> (l k) n"))
            cast2d(w_a.rearrange("l k n -> (l k) n"), w_a_h.rearrange("l k n -> (l k) n"))
            cast2d(w_b.rearrange("l k n -> (l k) n"), w_b_h.rearrange("l k n -> (l k) n"))
            cast2d(w_out.rearrange("l k n -> (l k) n"), w_out_h.rearrange("l k n -> (l k) n"))
            cast2d(ple_proj.rearrange("l k n -> (l k) n"), ple_proj_h.rearrange("l k n -> (l k) n"))
            cast2d(ple_gate.rearrange("l k n -> (l k) n"), ple_gate_h.rearrange("l k n -> (l k) n"))
            cast2d(bias_d.rearrange("h j k q -> (h j k) q"), bias_h.rearrange("h j k q -> (h j k) q"))
            DMA("sp", stg[0].ap[0:1, 0:8], ln_in_g[0:8].rearrange("(o n) -> o n", o=1), allk, stg[0].k() + ["wh_all"])
            A.cur = mark_common


        def load_ln(gsrc, bsrc, lp=None):
            lp = lp or lnp
            DMA("sp", lp.ap[:, 0, :], gsrc.partition_broadcast(128), (), lp.ks(0))
            DMA("sp", lp.ap[:, 1, :], bsrc.partition_broadcast(128), (), lp.ks(1))

        def layer_norm_g(h, hk, out_ap, out_keys, st, mv, lp=None):
            lp = lp or lnp
            for c in range(2):
                P.add("dve", (lambda c: lambda e: e.bn_stats(out=st.ap[:, c, :], in_=h[:, c * 512:(c + 1) * 512]))(c), hk, st.k())
            P.add("dve", lambda e: e.bn_aggr(out=mv.ap[:, 0:2], in_=st.ap), st.k(), mv.k())
            TS("dve", mv.ap[:, 2:3], mv.ap[:, 1:2], LN_EPS, ALU.add, mv.k(), mv.k())
            yield
            ACT(mv.ap[:, 2:3], mv.ap[:, 2:3], AF.Sqrt, mv.k(), mv.k())
            yield
            RECIP(mv.ap[:, 2:3], mv.ap[:, 2:3], mv.k(), mv.k())
            TS("dve", h, h, mv.ap[:, 0:1], ALU.subtract, hk + mv.k(), hk, s2=mv.ap[:, 2:3], op1=ALU.mult)
            yield
            TT("dve", h, h, lp.ap[:, 0, :], ALU.mult, hk + lp.ks(0), hk)
            yield
            TT("pool", out_ap, h, lp.ap[:, 1, :], ALU.add, hk + lp.ks(1), out_keys)
            yield

        def layer_norm(h, hk, out_ap, out_keys, st, mv, lp=None):
            for _ in layer_norm_g(h, hk, out_ap, out_keys, st, mv, lp):
                pass

        def interleave(gens, width):
            gens = list(gens)
            nxt = 0
            active = []
            while nxt < len(gens) or active:
                while len(active) < width and nxt < len(gens):
                    active.append(gens[nxt])
                    nxt += 1
                for g_ in list(active):
                    try:
                        next(g_)
                    except StopIteration:
                        active.remove(g_)

        def to_feature_major(xt_tok, xk, tok0, f32_out=None, sb=None, to_dram=True):
            if to_dram:
                stg = xstg[xsc[0] % 2]
                xsc[0] += 1
            for half in range(2):
                b = bank()
                for cc in range(4):
                    c = half * 4 + cc
                    TR(psum[:, b, cc * 128:(cc + 1) * 128], xt_tok[:, c * 128:(c + 1) * 128], ident.ap, xk + KI, PK(b))
                src = psum[:, b, :].rearrange("p (c t) -> p c t", t=128)
                if sb is not None:
                    tl, ncols, col0 = sb
                    wk = []
                    for c in range(half * 4, half * 4 + 4):
                        wk += tl.k(c * ncols + col0, c * ncols + col0 + 128)
                    CP("dve", tl.ap[:, half * 4:half * 4 + 4, col0:col0 + 128], src, PK(b), wk)
                if to_dram:
                    CP("dve", stg.ap[:, half * 4:half * 4 + 4, :], src, PK(b), stg.ks(half * 4, 4))
                if f32_out is not None:
                    CP("dve", f32_out.ap[:, half * 4:half * 4 + 4, :], src, PK(b), f32_out.ks(half * 4, 4))
            if to_dram:
                DMA("sp", xT_d[:, :, tok0:tok0 + 128].rearrange("c p t -> p c t"), stg.ap, stg.k(), [("xTd", tok0 // 128)])

        def load_layer_params(l):
            fm = lambda src: src.rearrange("(c p) -> p c", p=128)
            lst = [("decay_base", decay_base[l]), ("aaa_base", aaa_base[l]), ("k_k", k_k[l]), ("k_a", k_a[l]),
                   ("r_k", r_k[l]), ("gn_g", gn_g[l]), ("gn_b", gn_b[l])]
            if l > 0:
                lst.append(("vres_base", vres_base[l - 1]))
            for name, src in lst:
                DMA("sp", prm.ap[:, PRM[name], :], fm(src), (), prm.k(), allow_slow_non_contiguous=True)
            DMA("sp", mu.ap[:, 0, 0:14], tok_mix[l, 0:1792].rearrange("(c p) -> p c", p=128), (), mu.k(), allow_slow_non_contiguous=True)
            DMA("sp", mu.ap[0:32, 0, 14:15], tok_mix[l, 1792:1824].rearrange("(p o) -> p o", o=1), (), mu.k())
            TS("pool", mu.ap[:, 1, 0:14], mu.ap[:, 0, 0:14], -1.0, ALU.mult, mu.k(), mu.k(), s2=1.0, op1=ALU.add)
            TS("pool", mu.ap[0:32, 1, 14:15], mu.ap[0:32, 0, 14:15], -1.0, ALU.mult, mu.k(), mu.k(), s2=1.0, op1=ALU.add)
            DMA("sp", lora.ap[0:64, 0, :], decay_up[l], (), lora.ks(0))
            DMA("sp", lora.ap[64:128, 0, :], aaa_up[l], (), lora.ks(0))
            DMA("sp", lora.ap[:, 1, :], gate_up[l, 0:128, :], (), lora.ks(1))
            DMA("sp", lora.ap[0:32, 2, :], gate_up[l, 128:160, :], (), lora.ks(2))
            if l > 0:
                DMA("sp", vrd.ap, vres_down[l - 1].rearrange("(c p) r -> p c r", p=128), (), vrd.k())
                DMA("sp", vru.ap, vres_up[l - 1], (), vru.k())
            DMA("sp", rw.ap, router_w[l].rearrange("(k p) n -> p k n", p=128), (), rw.k())
            DMA("sp", rb.ap, router_b[l].partition_broadcast(128), (), rb.k())

        def wload(src_ap, shape):
            t = wbuf()
            n = int(np.prod(shape))
            v = t.ap.rearrange("p a b -> p (a b)")[:, 0:n]
            if len(shape) == 2:
                v = v.rearrange("p (a b) -> p a b", b=shape[1])
            cast_load(v, src_ap, t.k())
            return v, t.k()

        def phaseA(s, l):
            A.cur = mark_common
            kT = A.alloc([4, 1024], BF16); vring = A.alloc([8, 8, 65], BF16)
            xTa = A.alloc([KC, 1024], BF16)
            yAT = A.alloc([4, 512], BF16); yBT = A.alloc([4, 512], BF16)
            st6s = [A.alloc([2, 6]) for _ in range(2)]; mv4s = [A.alloc([4]) for _ in range(2)]
            lgs = [A.alloc([36]) for _ in range(2)]; rts = [A.alloc([64]) for _ in range(2)]
            twd = A.alloc([512]); tgd1 = A.alloc([512]); tgd2 = A.alloc([512], parts=32)
            v_all = A.alloc([4, 512]); vd = A.alloc([512], parts=32)
            mark_att = A.cur
            qT = A.alloc([4, 512], BF16)
            PT = [A.alloc([4, 128], BF16) for _ in range(6)]
            ytok = A.alloc([512]); rc = A.alloc([8])
            end_att = A.cur
            A.cur = mark_att
            NSL = 4
            tokT = [A.alloc([3, 128], parts=64) for _ in range(NSL)]
            AT = [A.alloc([4, 128], parts=64) for _ in range(NSL)]
            X0 = [A.alloc([2, 64], parts=64) for _ in range(NSL)]
            Xn = [[A.alloc([4, 64], parts=64) for _ in range(2)] for _ in range(NSL)]
            Tt = [[A.alloc([2, 64], parts=64) for _ in range(2)] for _ in range(NSL)]
            G0 = [A.alloc([2, 64], parts=64) for _ in range(NSL)]
            A.cur = max(A.cur, end_att)
            mark_r = A.cur
            t_r = A.alloc([512]); t_k = A.alloc([512]); t_sg = A.alloc([512]); t_cum = A.alloc([512])
            t_pin = A.alloc([512]); t_inv = A.alloc([512]); t_a = A.alloc([512]); t_kk = A.alloc([512])
            t_sq = A.alloc([512]); t_km = A.alloc([512]); t_bon = A.alloc([512])
            bt = A.alloc([512]); kt = A.alloc([512]); yT = A.alloc([512]); gnt = A.alloc([512])
            ar = A.alloc([8, 2, 64]); pC = A.alloc([8]); t_vf = A.alloc([512])
            Wsb = A.alloc([2, 64], parts=64); Usb = A.alloc([2, 64], parts=64); Stmp = A.alloc([64])
            end_r = A.cur
            A.cur = mark_r
            sga = A.alloc([512]); sgb = A.alloc([512]); m1 = A.alloc([512]); m2 = A.alloc([512])
            zT = A.alloc([8, 512], BF16)
            hbufs = [A.alloc([D]) for _ in range(2)]; x0s = [A.alloc([D]) for _ in range(2)]; x1fms = [A.alloc([KC, 128]) for _ in range(2)]
            A.cur = max(A.cur, end_r)
            print("phaseA arena words", A.cur, "of", A.nwords)

            load_layer_params(l)
            load_ln(ln_g[l, 0], ln_b[l, 0])
            MEMSET("pool", lastc.ap, 0.0, lastc.k())
            MEMSET("pool", Sst.ap, 0.0, Sst.k())
            MEMSET("pool", vring.ap[:, :, :, 64:65], 1.0, vring.k())
            wv = w_in_h[l].rearrange("(k p) n -> p k n", p=128)

            def shift(pc_ap, np_, mc, out_ap, out_keys, b):
                m = mu.ap[0:np_, 0, mc:mc + 1]; om = mu.ap[0:np_, 1, mc:mc + 1]
                TS("dve", out_ap, pc_ap, om, ALU.mult, PK(b) + mu.k(), out_keys)
                STT("dve", out_ap[:, 1:512], pc_ap[:, 0:511], m, out_ap[:, 1:512], ALU.mult, ALU.add, PK(b) + out_keys + mu.k(), out_keys)
                STT("dve", out_ap[:, 0:1], lastc.ap[0:np_, mc:mc + 1], m, out_ap[:, 0:1], ALU.mult, ALU.add, lastc.k() + out_keys + mu.k(), out_keys)
                CP("dve", lastc.ap[0:np_, mc:mc + 1], pc_ap[:, 511:512], PK(b), lastc.k())

            def load_xblock(bb):
                rp_ = (bb % 2) * 512
                wk_ = []
                for c in range(KC):
                    wk_ += xTa.k(c * 1024 + rp_, c * 1024 + rp_ + 512)
                DMA("sp", xTa.ap[:, :, rp_:rp_ + 512], xT_d[:, :, bb * 512:(bb + 1) * 512].rearrange("c p t -> p c t"),
                    [("xTd", bb * 4 + i) for i in range(4)], wk_)

            load_xblock(0)
            for b in range(NB):
                t0 = b * 512
                xr = (b % 2) * 512
                if b + 1 < NB:
                    load_xblock(b + 1)
                xk = []
                for c in range(KC):
                    xk += xTa.k(c * 1024 + xr, c * 1024 + xr + 512)

                def proj_fm(wt, wk, col, ncol=128, kparts=128):
                    bk = bank()
                    for k in range(KC):
                        MM(psum[0:ncol, bk, :], wt[:, k, col:col + ncol], xTa.ap[:, k, xr:xr + 512], k == 0, k == KC - 1, wk + xk, PK(bk))
                    return bk

                wt, wk = wload(wv[:, :, 0:512], [KC, 512])
                for c in range(4):
                    bk = proj_fm(wt, wk, c * 128)
                    TS("dve", qT.ap[:, c, :], psum[:, bk, :], 0.125, ALU.mult, PK(bk), qT.ks(c))
                wt, wk = wload(wv[:, :, 512:1024], [KC, 512])
                rp = (b % 2) * 512
                for c in range(4):
                    bk = proj_fm(wt, wk, c * 128)
                    CP("act", kT.ap[:, c, rp:rp + 512], psum[:, bk, :], PK(bk), kT.k(c * 1024 + rp, c * 1024 + rp + 512))
                wt, wk = wload(wv[:, :, 1024:1536], [KC, 512])
                for tt in range(4):
                    bk = bank()
                    for k in range(KC):
                        MM(psum[:, bk, :], xTa.ap[:, k, xr + tt * 128:xr + tt * 128 + 128], wt[:, k, :], k == 0, k == KC - 1, wk + xk, PK(bk))
                    slot = (b % 2) * 4 + tt
                    CP("dve", vring.ap[:, slot, :, 0:64], psum[:, bk, :].rearrange("p (h d) -> p h d", d=64), PK(bk), vring.ks(slot))

                for qi in range(4):
                    qb = b * 4 + qi
                    js = [j for j in range(5) if qb - 4 + j >= 0]
                    for hg in range(2):
                        pts = {}
                        for ji, j in enumerate(js):
                            kb = qb - 4 + j
                            ro = (kb % 8) * 128
                            bk = bank()
                            for hh in range(4):
                                h = hg * 4 + hh
                                c = h // 2; r0 = (h % 2) * 64
                                MM(psum[:, bk, hh * 128:(hh + 1) * 128], kT.ap[r0:r0 + 64, c, ro:ro + 128], qT.ap[r0:r0 + 64, c, qi * 128:(qi + 1) * 128],
                                   True, False, kT.k(c * 1024 + ro, c * 1024 + ro + 128) + qT.ks(c), PK(bk))
                                MM(psum[:, bk, hh * 128:(hh + 1) * 128], identb.ap, biasT.ap[:, h * 5 + j, :], False, True, KIB + biasT.ks(h * 5 + j), PK(bk))
                            pt = PT[ji]
                            ACT(pt.ap.rearrange("p h q -> p (h q)"), psum[:, bk, :], AF.Exp, PK(bk), pt.k())
                            pts[j] = pt
                        bk = bank()
                        pv = psum[:, bk, 0:260].rearrange("p (h d) -> p h d", d=65)
                        for hh in range(4):
                            h = hg * 4 + hh
                            for ji, j in enumerate(js):
                                kb = qb - 4 + j
                                slot = kb % 8
                                MM(pv[:, hh, :], pts[j].ap[:, hh, :], vring.ap[:, slot, h, :], ji == 0, ji == len(js) - 1,
                                   pts[j].k() + vring.ks(slot), PK(bk))
                        RECIP(rc.ap[:, hg * 4:hg * 4 + 4].rearrange("p (h o) -> p h o", o=1), pv[:, :, 64:65], PK(bk), rc.k())
                        TT("dve", ytok.ap[:, hg * 256:(hg + 1) * 256].rearrange("p (h d) -> p h d", d=64), pv[:, :, 0:64],
                           rc.ap[:, hg * 4:hg * 4 + 4].rearrange("p (h o) -> p h o", o=1).to_broadcast([128, 4, 64]), ALU.mult,
                           PK(bk) + rc.k(), ytok.k())
                    bk = bank()
                    for c in range(4):
                        TR(psum[:, bk, c * 128:(c + 1) * 128], ytok.ap[:, c * 128:(c + 1) * 128], ident.ap, ytok.k() + KI, PK(bk))
                    wkeys = []
                    for c in range(4):
                        wkeys += yAT.k(c * 512 + qi * 128, c * 512 + qi * 128 + 128)
                    CP("act", yAT.ap[:, :, qi * 128:(qi + 1) * 128], psum[:, bk, :].rearrange("p (c t) -> p c t", t=128), PK(bk), wkeys)

                if BIS == 4:
                    continue
                wt_r, wk_r = wload(wv[:, :, 1536:2048], [KC, 512])
                wt_k, wk_k = wload(wv[:, :, 2048:2560], [KC, 512])
                wt_v, wk_v = wload(wv[:, :, 2560:3072], [KC, 512])
                for hp in range(4):
                    bk = proj_fm(wt_v, wk_v, hp * 128)
                    shift(psum[:, bk, :], 128, 8 + hp, v_all.ap[:, hp, :], v_all.ks(hp), bk)
                wt_l, wk_l = wload(wv[:, :, 3072:3360], [KC, 288])
                bk = proj_fm(wt_l, wk_l, 0)
                shift(psum[:, bk, :], 128, 12, twd.ap, twd.k(), bk)
                bk = proj_fm(wt_l, wk_l, 128)
                shift(psum[:, bk, :], 128, 13, tgd1.ap, tgd1.k(), bk)
                bk = proj_fm(wt_l, wk_l, 256, ncol=32)
                shift(psum[0:32, bk, :], 32, 14, tgd2.ap, tgd2.k(), bk)
                ACT(twd.ap[0:64, :], twd.ap[0:64, :], AF.Tanh, twd.k(), twd.k())
                ACT(tgd1.ap, tgd1.ap, AF.Sigmoid, tgd1.k(), tgd1.k())
                ACT(tgd2.ap, tgd2.ap, AF.Sigmoid, tgd2.k(), tgd2.k())
                if l == 0:
                    for hp in range(4):
                        DMA("sp", vf_d[hp, :, t0:t0 + 512], v_all.ap[:, hp, :], v_all.ks(hp), [("vf", hp, b)])
                else:
                    bk = bank()
                    for hp in range(4):
                        MM(psum[0:32, bk, :], vrd.ap[:, hp, :], v_all.ap[:, hp, :], hp == 0, hp == 3, vrd.k() + v_all.ks(hp), PK(bk))
                    CP("act", vd.ap, psum[0:32, bk, :], PK(bk), vd.k())

                def pp(name, hp):
                    return prm.ap[:, PRM[name], hp:hp + 1]

                c3 = lambda t: t.ap.rearrange("p (c t) -> p c t", t=64)
                arv = ar.ap

                def prep1(hp):
                    va = v_all.ap[:, hp, :]; vk = v_all.ks(hp)
                    bk = proj_fm(wt_r, wk_r, hp * 128)
                    shift(psum[:, bk, :], 128, hp, t_r.ap, t_r.k(), bk)
                    yield
                    bk = proj_fm(wt_k, wk_k, hp * 128)
                    shift(psum[:, bk, :], 128, 4 + hp, t_k.ap, t_k.k(), bk)
                    yield
                    if l > 0:
                        bk = bank()
                        MM(psum[:, bk, :], vru.ap[0:32, hp * 128:(hp + 1) * 128], vd.ap, True, True, vru.k() + vd.k(), PK(bk))
                        ACT(t_sq.ap, psum[:, bk, :], AF.Sigmoid, PK(bk) + prm.k(), t_sq.k(), bias=pp("vres_base", hp))
                        DMA("sp", t_vf.ap, vf_d[hp, :, t0:t0 + 512], [("vf", hp, b)], t_vf.k())
                        TT("dve", t_vf.ap, t_vf.ap, va, ALU.subtract, t_vf.k() + vk, t_vf.k())
                        yield
                        TT("dve", t_vf.ap, t_vf.ap, t_sq.ap, ALU.mult, t_vf.k() + t_sq.k(), t_vf.k())
                        TT("dve", va, va, t_vf.ap, ALU.add, vk + t_vf.k(), vk)
                        yield
                    bk = bank()
                    MM(psum[:, bk, :], lora.ap[0:64, 0, hp * 128:(hp + 1) * 128], twd.ap[0:64, :], True, True, lora.ks(0) + twd.k(), PK(bk))
                    ACT(t_sg.ap, psum[:, bk, :], AF.Sigmoid, PK(bk) + prm.k(), t_sg.k(), bias=pp("decay_base", hp))
                    yield
                    P.add("dve", lambda e: e.tensor_tensor_scan(out=t_cum.ap, data0=rmask.ap, data1=t_sg.ap, initial=0.0, op0=ALU.mult, op1=ALU.add),
                          rmask.k() + t_sg.k(), t_cum.k())
                    yield
                    ACT(t_pin.ap, t_cum.ap, AF.Exp, t_cum.k(), t_pin.k(), scale=-C0)
                    ACT(t_inv.ap, t_cum.ap, AF.Exp, t_cum.k(), t_inv.k(), scale=C0)
                    TT("dve", t_sg.ap, t_cum.ap, t_sg.ap, ALU.subtract, t_cum.k() + t_sg.k(), t_sg.k())
                    yield
                    ACT(t_cum.ap, t_sg.ap, AF.Exp, t_sg.k(), t_cum.k(), scale=-C0)
                    bk = bank()
                    MM(psum[:, bk, :], lora.ap[64:128, 0, hp * 128:(hp + 1) * 128], twd.ap[64:128, :], True, True, lora.ks(0) + twd.k(), PK(bk))
                    ACT(t_a.ap, psum[:, bk, :], AF.Sigmoid, PK(bk) + prm.k(), t_a.k(), bias=pp("aaa_base", hp))
                    yield
                    TS("dve", t_kk.ap, t_k.ap, pp("k_k", hp), ALU.mult, t_k.k() + prm.k(), t_kk.k())
                    TT("pool", t_sq.ap, t_kk.ap, t_kk.ap, ALU.mult, t_kk.k(), t_sq.k())
                    yield
                    bk = bank()
                    MM(psum[:, bk, :], onesbd.ap, t_sq.ap, True, True, onesbd.k() + t_sq.k(), PK(bk))
                    TS("dve", t_sq.ap, psum[:, bk, :], 1e-24, ALU.max, PK(bk), t_sq.k())
                    yield
                    ACT(t_sq.ap, t_sq.ap, AF.Sqrt, t_sq.k(), t_sq.k())
                    yield
                    RECIP(t_sq.ap, t_sq.ap, t_sq.k(), t_sq.k())
                    TT("dve", t_kk.ap, t_kk.ap, t_sq.ap, ALU.mult, t_kk.k() + t_sq.k(), t_kk.k())
                    yield
                    TS("dve", t_km.ap, t_a.ap, 1.0, ALU.subtract, t_a.k() + prm.k(), t_km.k(), s2=pp("k_a", hp), op1=ALU.mult)
                    STT("dve", t_km.ap, t_km.ap, 1.0, t_k.ap, ALU.add, ALU.mult, t_km.k() + t_k.k(), t_km.k())
                    yield
                    STT("dve", t_sq.ap, t_r.ap, pp("r_k", hp), t_km.ap, ALU.mult, ALU.mult, t_r.k() + t_km.k() + prm.k(), t_sq.k())
                    TT("dve", t_a.ap, t_a.ap, t_kk.ap, ALU.mult, t_a.k() + t_kk.k(), t_a.k())
                    yield

                def prep2(hp):
                    va = v_all.ap[:, hp, :]; vk = v_all.ks(hp)
                    bk = bank()
                    MM(psum[:, bk, :], onesbd.ap, t_sq.ap, True, True, onesbd.k() + t_sq.k(), PK(bk))
                    TT("dve", t_bon.ap, psum[:, bk, :], va, ALU.mult, PK(bk) + vk, t_bon.k())
                    CP("dve", pC.ap, c3(t_pin)[:, :, 63], t_pin.k(), pC.k())
                    STT("dve", arv[:, :, 0, :], c3(t_kk), -1.0, c3(t_cum), ALU.mult, ALU.mult, t_kk.k() + t_cum.k(), ar.k())
                    TT("pool", arv[:, :, 1, :], c3(t_r), c3(t_pin), ALU.mult, t_r.k() + t_pin.k(), ar.k())
                    TT("dve", bt.ap, t_a.ap, t_inv.ap, ALU.mult, t_a.k() + t_inv.k(), bt.k())
                    TT("pool", kt.ap, t_km.ap, t_inv.ap, ALU.mult, t_km.k() + t_inv.k(), kt.k())

                def scan_gen(hp):
                    va = v_all.ap[:, hp, :]; vk = v_all.ks(hp)
                    atv4 = lambda at: at.ap.rearrange("p (h m) x -> p h m x", h=2)

                    def stage1(c, sl):
                        cs = slice(c * 64, (c + 1) * 64)
                        tk_ = tokT[sl]; at = AT[sl]; x0t = X0[sl]; g0 = G0[sl]
                        bk = bank()
                        TR(psum[0:64, bk, 0:128], va[:, cs], ident.ap, vk + KI, PK(bk))
                        TR(psum[0:64, bk, 128:256], bt.ap[:, cs], ident.ap, bt.k() + KI, PK(bk))
                        TR(psum[0:64, bk, 256:384], kt.ap[:, cs], ident.ap, kt.k() + KI, PK(bk))
                        CP("dve", tk_.ap, psum[0:64, bk, 0:384].rearrange("p (a b) -> p a b", b=128), PK(bk), tk_.k())
                        bk = bank2()
                        for h2 in range(2):
                            rows = slice(h2 * 64, h2 * 64 + 64)
                            rhs = arv[rows, c, :, :].rearrange("p a b -> p (a b)")
                            MM(psum[0:64, bk + h2, 0:128], bt.ap[rows, cs], rhs, True, True, bt.k() + ar.k(), PK(bk + h2))
                            MM(psum[0:64, bk + h2, 128:256], kt.ap[rows, cs], rhs, True, True, kt.k() + ar.k(), PK(bk + h2))
                        TT("dve", at.ap.rearrange("p (h m) x -> p h (m x)", h=2), psum[0:64, bk:bk + 2, 0:256],
                           maskU.ap.rearrange("p (h m) x -> p h (m x)", h=2), ALU.mult, PK(bk) + PK(bk + 1) + maskU.k(), at.k())
                        yield
                        bk = bank2()
                        for h2 in range(2):
                            rows = slice(h2 * 64, h2 * 64 + 64)
                            MM(psum[0:64, bk + h2, 0:64], arv[rows, c, 0, :], bt.ap[rows, cs], True, True, bt.k() + ar.k(), PK(bk + h2))
                        TT("dve", x0t.ap, psum[0:64, bk:bk + 2, 0:64], maskL.ap, ALU.mult, PK(bk) + PK(bk + 1) + maskL.k(), x0t.k())
                        bk = bank()
                        for h2 in range(2):
                            MM(psum[0:64, bk, h2 * 64:(h2 + 1) * 64], at.ap[:, h2 * 2 + 1, 0:64], tk_.ap[:, 0, h2 * 64:(h2 + 1) * 64], True, True, at.k() + tk_.k(), PK(bk))
                        CP("dve", g0.ap, psum[0:64, bk, 0:128].rearrange("p (a b) -> p a b", b=64), PK(bk), g0.k())
                        tt_cur = Tt[sl][0]
                        TT("pool", tt_cur.ap, atv4(at)[:, :, 0, 0:64], identT.ap, ALU.add, at.k() + identT.k(), tt_cur.k())
                        yield
                        Xc = x0t.ap; Xtc = atv4(at)[:, :, 0, 0:64]
                        xck = x0t.k(); xtk = at.k()
                        for lvl in range(1, 6):
                            xn = Xn[sl][lvl % 2]
                            bk = bank()
                            for h2 in range(2):
                                MM(psum[0:64, bk, h2 * 64:(h2 + 1) * 64], Xtc[:, h2, :], Xc[:, h2, :], True, True, xck + xtk, PK(bk))
                                if lvl < 5:
                                    MM(psum[0:64, bk, 128 + h2 * 64:128 + (h2 + 1) * 64], Xc[:, h2, :], Xtc[:, h2, :], True, True, xck + xtk, PK(bk))
                            ncol = 256 if lvl < 5 else 128
                            CP("dve", xn.ap.rearrange("p a b -> p (a b)")[:, 0:ncol], psum[0:64, bk, 0:ncol], PK(bk), xn.k())
                            yield
                            Xc = xn.ap[:, 0:2, :]; Xtc = xn.ap[:, 2:4, :]; xck = xn.k(); xtk = xn.k()
                            bk = bank()
                            for h2 in range(2):
                                MM(psum[0:64, bk, h2 * 64:(h2 + 1) * 64], Xc[:, h2, :], tt_cur.ap[:, h2, :], True, True, xck + tt_cur.k(), PK(bk))
                            tt_new = Tt[sl][lvl % 2]
                            TT("dve", tt_new.ap, psum[0:64, bk, 0:128].rearrange("p (a b) -> p a b", b=64), tt_cur.ap, ALU.add, PK(bk) + tt_cur.k(), tt_new.k())
                            tt_cur = tt_new
                            yield

                    def stage2(c, sl):
                        tk_ = tokT[sl]; at = AT[sl]; g0 = G0[sl]; tt_cur = Tt[sl][1]
                        sk = Sst.ks(hp)
                        bk = bank2()
                        for h2 in range(2):
                            rows = slice(h2 * 64, h2 * 64 + 64)
                            MM(psum[0:64, bk + h2, 0:64], arv[rows, c, 0, :], Sst.ap[rows, hp, :], True, True, ar.k() + sk, PK(bk + h2))
                        TT("dve", Wsb.ap, psum[0:64, bk:bk + 2, 0:64], g0.ap, ALU.add, PK(bk) + PK(bk + 1) + g0.k(), Wsb.k())
                        yield
                        bk = bank()
                        for h2 in range(2):
                            MM(psum[0:64, bk, h2 * 64:(h2 + 1) * 64], tt_cur.ap[:, h2, :], Wsb.ap[:, h2, :], True, True, tt_cur.k() + Wsb.k(), PK(bk))
                        CP("dve", Usb.ap, psum[0:64, bk, 0:128].rearrange("p (a b) -> p a b", b=64), PK(bk), Usb.k())
                        TS("pool", Stmp.ap, Sst.ap[:, hp, :], pC.ap[:, c:c + 1], ALU.mult, sk + pC.k(), Stmp.k())
                        yield
                        for h2 in range(2):
                            rows = slice(h2 * 64, h2 * 64 + 64)
                            yo = psum[rows, 6, c * 64:(c + 1) * 64]
                            if h2 == 0:
                                MM(yo, Sst.ap[rows, hp, :], arv[rows, c, 1, :], True, False, sk + ar.k(), PK(6))
                            else:
                                MM(psum[rows, 7, c * 64:(c + 1) * 64], Sst.ap[rows, hp, :], arv[rows, c, 1, :], True, True, sk + ar.k(), PK(7))
                            MM(yo, Usb.ap[:, h2, :], at.ap[:, h2 * 2, 64:128], h2 == 1, False, Usb.k() + at.k(), PK(6))
                            MM(yo, tk_.ap[:, 0, h2 * 64:(h2 + 1) * 64], at.ap[:, h2 * 2 + 1, 64:128], False, True, tk_.k() + at.k(), PK(6))
                        bk = bank()
                        for h2 in range(2):
                            rows = slice(h2 * 64, h2 * 64 + 64)
                            so = psum[rows, bk, 0:64]
                            MM(so, tk_.ap[:, 1, h2 * 64:(h2 + 1) * 64], Usb.ap[:, h2, :], True, False, tk_.k() + Usb.k(), PK(bk))
                            MM(so, tk_.ap[:, 2, h2 * 64:(h2 + 1) * 64], tk_.ap[:, 0, h2 * 64:(h2 + 1) * 64], False, True, tk_.k(), PK(bk))
                        STT("dve", Sst.ap[:, hp, :], psum[:, bk, 0:64], pC.ap[:, c:c + 1], Stmp.ap, ALU.mult, ALU.add, PK(bk) + pC.k() + Stmp.k(), sk)
                        yield


                    s1_next = 0; s1_active = []; s1_done = set(); s2_c = 0; s2_gen = None; s2_done = 0
                    while s2_done < 8:
                        while len(s1_active) < 3 and s1_next < 8 and s1_next - s2_done < NSL:
                            s1_active.append((s1_next, stage1(s1_next, s1_next % NSL)))
                            s1_next += 1
                        for item in list(s1_active):
                            cc_, g_ = item
                            try:
                                next(g_)
                            except StopIteration:
                                s1_active.remove(item)
                                s1_done.add(cc_)
                        if s2_gen is None and s2_c < 8 and s2_c in s1_done:
                            s2_gen = stage2(s2_c, s2_c % NSL)
                        if s2_gen is not None:
                            try:
                                next(s2_gen)
                            except StopIteration:
                                s2_gen = None
                                s2_c += 1
                                s2_done += 1
                        yield

                def finalize(hp):
                    CP("dve", yT.ap, psum[:, 6, :], PK(6), yT.k())
                    TT("dve", yT.ap[64:128, :], psum[64:128, 7, :], yT.ap[64:128, :], ALU.add, PK(7) + yT.k(), yT.k())
                    bk = bank()
                    MM(psum[:, bk, :], onesbd.ap, yT.ap, True, True, onesbd.k() + yT.k(), PK(bk))
                    STT("dve", yT.ap, psum[:, bk, :], -1.0 / 64, yT.ap, ALU.mult, ALU.add, PK(bk) + yT.k(), yT.k())
                    TT("pool", gnt.ap, yT.ap, yT.ap, ALU.mult, yT.k(), gnt.k())
                    bk = bank()
                    MM(psum[:, bk, :], onesbd.ap, gnt.ap, True, True, onesbd.k() + gnt.k(), PK(bk))
                    TS("dve", gnt.ap, psum[:, bk, :], 1.0 / 64, ALU.mult, PK(bk), gnt.k(), s2=GN_EPS, op1=ALU.add)
                    ACT(gnt.ap, gnt.ap, AF.Sqrt, gnt.k(), gnt.k())
                    RECIP(gnt.ap, gnt.ap, gnt.k(), gnt.k())
                    TT("dve", yT.ap, yT.ap, gnt.ap, ALU.mult, yT.k() + gnt.k(), yT.k())
                    TS("dve", yT.ap, yT.ap, pp("gn_g", hp), ALU.mult, yT.k() + prm.k(), yT.k(), s2=pp("gn_b", hp), op1=ALU.add)
                    TT("pool", yT.ap, yT.ap, t_bon.ap, ALU.add, yT.k() + t_bon.k(), yT.k())
                    bk = bank()
                    MM(psum[:, bk, :], lora.ap[:, 1, hp * 128:(hp + 1) * 128], tgd1.ap, True, False, lora.ks(1) + tgd1.k(), PK(bk))
                    MM(psum[:, bk, :], lora.ap[0:32, 2, hp * 128:(hp + 1) * 128], tgd2.ap, False, True, lora.ks(2) + tgd2.k(), PK(bk))
                    TT("dve", yBT.ap[:, hp, :], psum[:, bk, :], yT.ap, ALU.mult, PK(bk) + yT.k(), yBT.ks(hp))

                for _ in prep1(0):
                    pass
                prep2(0)
                for hp in range(4):
                    gens = [scan_gen(hp)]
                    if hp + 1 < 4:
                        gens.append(prep1(hp + 1))
                    interleave(gens, 2)
                    finalize(hp)
                    if hp + 1 < 4:
                        prep2(hp + 1)

                for half in range(2):
                    t = wbuf()
                    mhv = t.ap.rearrange("p a b -> p (a b)").rearrange("p (w k n) -> p w k n", w=2, k=4)
                    cast_load(mhv[:, 0], w_a_h[l][:, half * 512:(half + 1) * 512].rearrange("(k p) n -> p k n", p=128), t.k())
                    cast_load(mhv[:, 1], w_b_h[l][:, half * 512:(half + 1) * 512].rearrange("(k p) n -> p k n", p=128), t.k())
                    mhk = t.k()
                    wga, wgak = wload(wv[:, :, 3360 + half * 512:3360 + (half + 1) * 512], [KC, 512])
                    wgb, wgbk = wload(wv[:, :, 4384 + half * 512:4384 + (half + 1) * 512], [KC, 512])
                    for cc in range(4):
                        c = half * 4 + cc
                        bka = bank()
                        for k in range(4):
                            MM(psum[:, bka, :], mhv[:, 0, k, cc * 128:(cc + 1) * 128], yAT.ap[:, k, :], k == 0, k == 3, mhk + yAT.ks(k), PK(bka))
                        bkg = proj_fm(wga, wgak, cc * 128)
                        ACT(sga.ap, psum[:, bkg, :], AF.Sigmoid, PK(bkg), sga.k())
                        TT("dve", m1.ap, psum[:, bka, :], sga.ap, ALU.mult, PK(bka) + sga.k(), m1.k())
                        bkb = bank()
                        for k in range(4):
                            MM(psum[:, bkb, :], mhv[:, 1, k, cc * 128:(cc + 1) * 128], yBT.ap[:, k, :], k == 0, k == 3, mhk + yBT.ks(k), PK(bkb))
                        bkg = proj_fm(wgb, wgbk, cc * 128)
                        ACT(sgb.ap, psum[:, bkg, :], AF.Sigmoid, PK(bkg), sgb.k())
                        TT("dve", m2.ap, psum[:, bkb, :], sgb.ap, ALU.mult, PK(bkb) + sgb.k(), m2.k())
                        TT("pool", zT.ap[:, c, :], m1.ap, m2.ap, ALU.add, m1.k() + m2.k(), zT.ks(c))
                wo = []
                for kh in range(2):
                    wo.append(wload(w_out_h[l][kh * 512:(kh + 1) * 512, :].rearrange("(k p) n -> p k n", p=128), [4, 1024]))
                def ln1_tile(tt):
                    ti = b * 4 + tt
                    hbuf = hbufs[tt % 2]; x0 = x0s[tt % 2]; x1fm = x1fms[tt % 2]
                    st6 = st6s[tt % 2]; mv4 = mv4s[tt % 2]; lg = lgs[tt % 2]; rt = rts[tt % 2]
                    DMA("sp", x0.ap, xs_d[ti * 128:(ti + 1) * 128, :], [("xs", ti)], x0.k())
                    for nh in range(2):
                        bk = bank()
                        for k in range(KC):
                            wvv, wkk = wo[k // 4]
                            MM(psum[:, bk, :], zT.ap[:, k, tt * 128:(tt + 1) * 128], wvv[:, k % 4, nh * 512:(nh + 1) * 512], k == 0, k == KC - 1,
                               zT.ks(k) + wkk, PK(bk))
                        STT("dve", hbuf.ap[:, nh * 512:(nh + 1) * 512], x0.ap[:, nh * 512:(nh + 1) * 512], ALPHA, psum[:, bk, :], ALU.mult, ALU.add,
                            x0.k() + PK(bk), hbuf.k())
                    yield
                    yield from layer_norm_g(hbuf.ap, hbuf.k(), hbuf.ap, hbuf.k(), st6, mv4)
                    DMA("sp", xs_d[ti * 128:(ti + 1) * 128, :], hbuf.ap, hbuf.k(), [("xs", ti)])
                    to_feature_major(hbuf.ap, hbuf.k(), ti * 128, f32_out=x1fm)
                    yield
                    yield from router_g(l, ti, x1fm, lg, rt)

                interleave([ln1_tile(tt) for tt in range(4)], 2)

        def router_g(l, ti, x1fm, lg, rt):
            bk = bank()
            for k in range(KC):
                MM(psum[:, bk, 0:36], x1fm.ap[:, k, :], rw.ap[:, k, :], k == 0, k == KC - 1, x1fm.ks(k) + rw.k(), PK(bk))
            TT("dve", lg.ap, psum[:, bk, 0:36], rb.ap, ALU.add, PK(bk) + rb.k(), lg.k())
            K = lg.k() + rt.k()
            r = rt.ap
            g4 = lg.ap[:, 0:4]
            P.add("dve", lambda e: e.reduce_max(out=r[:, 0:1], in_=g4, axis=mybir.AxisListType.X), K, K)
            TS("dve", r[:, 4:8], g4, r[:, 0:1], ALU.subtract, K, K)
            ACT(r[:, 4:8], r[:, 4:8], AF.Exp, K, K)
            P.add("dve", lambda e: e.reduce_sum(out=r[:, 1:2], in_=r[:, 4:8], axis=mybir.AxisListType.X), K, K)
            RECIP(r[:, 1:2], r[:, 1:2], K, K)
            yield
            TS("dve", r[:, 8:12], g4, r[:, 0:1], ALU.is_ge, K, K)
            ev = lg.ap[:, 4:36].rearrange("p (g e) -> p g e", e=8)
            TT("dve", r[:, 32:64].rearrange("p (g e) -> p g e", e=8), ev, r[:, 8:12].rearrange("p (g o) -> p g o", o=1).to_broadcast([128, 4, 8]),
               ALU.mult, K, K)
            P.add("dve", lambda e: e.tensor_reduce(out=r[:, 12:20], in_=r[:, 32:64].rearrange("p (g e) -> p e g", e=8), axis=mybir.AxisListType.X,
                                                   op=ALU.add), K, K)
            sel = r[:, 12:20]
            P.add("dve", lambda e: e.reduce_max(out=r[:, 2:3], in_=sel, axis=mybir.AxisListType.X), K, K)
            TS("dve", r[:, 20:28], sel, r[:, 2:3], ALU.is_ge, K, K)
            STT("dve", r[:, 32:40], r[:, 20:28], -1e30, sel, ALU.mult, ALU.add, K, K)
            P.add("dve", lambda e: e.reduce_max(out=r[:, 3:4], in_=r[:, 32:40], axis=mybir.AxisListType.X), K, K)
            TS("dve", r[:, 40:48], sel, r[:, 3:4], ALU.is_ge, K, K)
            yield
            TT("dve", r[:, 48:49], r[:, 3:4], r[:, 2:3], ALU.subtract, K, K)
            ACT(r[:, 48:49], r[:, 48:49], AF.Exp, K, K)
            TS("dve", r[:, 48:49], r[:, 48:49], 1.0, ALU.add, K, K)
            RECIP(r[:, 49:50], r[:, 48:49], K, K)
            yield
            TS("dve", r[:, 50:51], r[:, 49:50], -1.0, ALU.mult, K, K, s2=1.0, op1=ALU.add)
            TT("dve", r[:, 49:51], r[:, 49:51], r[:, 1:2].to_broadcast([128, 2]), ALU.mult, K, K)
            TT("dve", r[:, 40:48], r[:, 40:48], r[:, 20:28], ALU.subtract, K, K)
            TS("dve", r[:, 40:48], r[:, 40:48], r[:, 50:51], ALU.mult, K, K)
            STT("dve", r[:, 40:48], r[:, 20:28], r[:, 49:50], r[:, 40:48], ALU.mult, ALU.add, K, K)
            cv = comb.ap[:, ti, :].rearrange("p (g e) -> p g e", e=8)
            TT("dve", cv, r[:, 8:12].rearrange("p (g o) -> p g o", o=1).to_broadcast([128, 4, 8]),
               r[:, 40:48].rearrange("p (o e) -> p o e", o=1).to_broadcast([128, 4, 8]), ALU.mult, K, comb.ks(ti))
            yield

        def phaseB(s, l, last):
            A.cur = mark_common
            acc = A.alloc([NT, D])
            xT = A.alloc([KC, S], BF16)
            for c in range(KC):
                DMA("sp", xT.ap[:, c, :], xT_d[c], [("xTd", i) for i in range(NT)], xT.ks(c))
            hT = [A.alloc([2, 512], BF16) for _ in range(2)]
            sil = [[A.alloc([512], BF16) for _ in range(2)] for _ in range(2)]
            x1s = [A.alloc([D]) for _ in range(2)]; st6s = [A.alloc([2, 6]) for _ in range(2)]; mv4s = [A.alloc([4]) for _ in range(2)]
            pts = [A.alloc([256]) for _ in range(2)]; pTs = [A.alloc([2, 128], BF16) for _ in range(2)]
            gsigs = [A.alloc([D]) for _ in range(2)]; lnp3 = A.alloc([2, D])
            assert Sst.off + Sst.n - lora.off >= 2048
            stgB = [Tile(A.ap, lora.off + i * 1024, 1024, F32, [1024]) for i in range(2)]
            sbc = [0]

            def stage_cast(dst, src, dkeys):
                a, b_ = src.shape[1], src.shape[2]
                step = max(1, 1024 // b_)
                for a0 in range(0, a, step):
                    a1 = min(a, a0 + step)
                    i = sbc[0]; sbc[0] += 1
                    st_ = stgB[i % 2]
                    sv = st_.ap[:, 0:(a1 - a0) * b_].rearrange("p (a b) -> p a b", b=b_)
                    DMA("sp", sv, src[:, a0:a1, :], (), st_.k())
                    CP("pool", dst[:, a0:a1, :], sv, st_.k(), dkeys)
            print("phaseB arena words", A.cur, "of", A.nwords)
            def load_expert(e):
                t = wbuf()
                gu = t.ap
                stage_cast(gu[:, :, 0:256], exp_gate[l, e].rearrange("(k p) n -> p k n", p=128), t.k())
                stage_cast(gu[:, :, 256:512], exp_up[l, e].rearrange("(k p) n -> p k n", p=128), t.k())
                t2 = wbuf()
                wd = t2.ap.rearrange("p a b -> p (a b)")[:, 0:2048].rearrange("p (k n) -> p k n", k=2)
                stage_cast(wd, exp_down[l, e].rearrange("(k p) n -> p k n", p=128), t2.k())
                return gu, t.k(), wd, t2.k()

            bctr = [0]

            def bankB():
                b_ = bctr[0] % 8
                bctr[0] += 1
                return b_

            def GU(i, e, b, w):
                gu, guk, wd, wdk = w
                t0 = b * 512
                xk = []
                for c in range(KC):
                    xk += xT.k(c * S + t0, c * S + t0 + 512)
                ht = hT[i % 2]
                for dc in range(2):
                    bg = bankB()
                    for k in range(KC):
                        MM(psum[:, bg, :], gu[:, k, dc * 128:(dc + 1) * 128], xT.ap[:, k, t0:t0 + 512], k == 0, k == KC - 1, guk + xk, PK(bg))
                    bu = bankB()
                    for k in range(KC):
                        MM(psum[:, bu, :], gu[:, k, 256 + dc * 128:256 + (dc + 1) * 128], xT.ap[:, k, t0:t0 + 512], k == 0, k == KC - 1, guk + xk, PK(bu))
                    sl = sil[i % 2][dc]
                    ACT(sl.ap, psum[:, bg, :], AF.Silu, PK(bg), sl.k())
                    TT("dve", ht.ap[:, dc, :], psum[:, bu, :], sl.ap, ALU.mult, PK(bu) + sl.k(), ht.ks(dc))

            def DN(i, e, b, w):
                gu, guk, wd, wdk = w
                ht = hT[i % 2]
                for tt in range(4):
                    ti = b * 4 + tt
                    for nh in range(2):
                        bk = bankB()
                        for k in range(2):
                            MM(psum[:, bk, :], ht.ap[:, k, tt * 128:(tt + 1) * 128], wd[:, k, nh * 512:(nh + 1) * 512], k == 0, k == 1, ht.ks(k) + wdk, PK(bk))
                        av = acc.ap[:, ti, nh * 512:(nh + 1) * 512]
                        ak = acc.k(ti * D + nh * 512, ti * D + (nh + 1) * 512)
                        if e == 0:
                            TS("dve", av, psum[:, bk, :], comb.ap[:, ti, e:e + 1], ALU.mult, PK(bk) + comb.ks(ti), ak)
                        else:
                            STT("dve", av, psum[:, bk, :], comb.ap[:, ti, e:e + 1], av, ALU.mult, ALU.add, PK(bk) + comb.ks(ti) + ak, ak)

            items = [(e, b) for e in range(32) for b in range(NB)]
            wts = {0: load_expert(0), 1: load_expert(1)}
            GU(0, 0, 0, wts[0])
            for i, (e, b) in enumerate(items):
                if i + 1 < len(items):
                    e2, b2 = items[i + 1]
                    GU(i + 1, e2, b2, wts[e2])
                DN(i, e, b, wts[e])
                if b == NB - 1:
                    del wts[e]
                    if e + 2 < 32:
                        wts[e + 2] = load_expert(e + 2)
            wpg = []
            for kh in range(2):
                wpg.append(wload(ple_gate_h[l][kh * 512:(kh + 1) * 512, :].rearrange("(k p) n -> p k n", p=128), [4, 1024]))
            wpp, wppk = wload(ple_proj_h[l].rearrange("(k p) n -> p k n", p=128), [2, 1024])
            load_ln(ln_g[l, 1], ln_b[l, 1])
            load_ln(ln_g[l, 2], ln_b[l, 2], lnp3)
            def ep_tile(ti):
                hk = acc.ks(ti)
                h = acc.ap[:, ti, :]
                x1 = x1s[ti % 2]; st6 = st6s[ti % 2]; mv4 = mv4s[ti % 2]; pt = pts[ti % 2]; pT = pTs[ti % 2]; gsig = gsigs[ti % 2]
                DMA("sp", x1.ap, xs_d[ti * 128:(ti + 1) * 128, :], [("xs", ti)], x1.k())
                DMA("sp", pt.ap, p_d[l, s, ti * 128:(ti + 1) * 128, :], (), pt.k())
                STT("dve", h, x1.ap, ALPHA, h, ALU.mult, ALU.add, x1.k() + hk, hk)
                yield from layer_norm_g(h, hk, h, hk, st6, mv4)
                to_feature_major(h, hk, ti * 128, sb=(xT, S, ti * 128), to_dram=False)
                bk = bank()
                for c in range(2):
                    TR(psum[:, bk, c * 128:(c + 1) * 128], pt.ap[:, c * 128:(c + 1) * 128], ident.ap, pt.k() + KI, PK(bk))
                CP("dve", pT.ap, psum[:, bk, 0:256].rearrange("p (c t) -> p c t", t=128), PK(bk), pT.k())
                yield
                xk = []
                for c in range(KC):
                    xk += xT.k(c * S + ti * 128, c * S + ti * 128 + 128)
                for nh in range(2):
                    bk = bank()
                    for k in range(KC):
                        wvv, wkk = wpg[k // 4]
                        MM(psum[:, bk, :], xT.ap[:, k, ti * 128:(ti + 1) * 128], wvv[:, k % 4, nh * 512:(nh + 1) * 512], k == 0, k == KC - 1, xk + wkk, PK(bk))
                    ACT(gsig.ap[:, nh * 512:(nh + 1) * 512], psum[:, bk, :], AF.Sigmoid, PK(bk), gsig.k())
                    bk = bank()
                    for k in range(2):
                        MM(psum[:, bk, :], pT.ap[:, k, :], wpp[:, k, nh * 512:(nh + 1) * 512], k == 0, k == 1, pT.k() + wppk, PK(bk))
                    TT("dve", gsig.ap[:, nh * 512:(nh + 1) * 512], psum[:, bk, :], gsig.ap[:, nh * 512:(nh + 1) * 512], ALU.mult, PK(bk) + gsig.k(), gsig.k())
                    yield
                STT("dve", h, h, ALPHA, gsig.ap, ALU.mult, ALU.add, hk + gsig.k(), hk)
                yield from layer_norm_g(h, hk, h, hk, st6, mv4, lnp3)
                if last:
                    DMA("sp", y_d[s, ti * 128:(ti + 1) * 128, :], h, hk, [("y", s, ti)])
                else:
                    DMA("sp", xs_d[ti * 128:(ti + 1) * 128, :], h, hk, [("xs", ti)])
                    to_feature_major(h, hk, ti * 128)
                yield

            interleave([ep_tile(ti) for ti in range(NT)], 2)

        for s in range(NSEQ if STOP != "c" else 0):
            A.cur = mark_common
            load_ln(ln_in_g, ln_in_b)
            p0_x = [A.alloc([D]) for _ in range(2)]
            p0_st = A.alloc([2, 6]); p0_mv = A.alloc([4])
            for i in range(NT):
                xt = p0_x[i % 2]
                DMA("sp", xt.ap, x_d[s, i * 128:(i + 1) * 128, :], (), xt.k())
                if BIS != 1:
                    layer_norm(xt.ap, xt.k(), xt.ap, xt.k(), p0_st, p0_mv)
                DMA("sp", xs_d[i * 128:(i + 1) * 128, :], xt.ap, xt.k(), [("xs", i)])
                if BIS not in (1, 2):
                    to_feature_major(xt.ap, xt.k(), i * 128)
            if s == 0:
                precast()
                cast_load(biasT.ap, bias_h.rearrange("h j k q -> k (h j) q"), biasT.k())
            if STOP == "p0":
                break
            for l in range(DEPTH):
                phaseA(s, l)
                if STOP == "A":
                    break
                phaseB(s, l, l == DEPTH - 1)
                if STOP == "B":
                    break
            if STOP:
                break
        P.emit()
    return nc


REL_CLIP = 128
_NC_CACHE = {}


def _bias_table(rel_bias):
    k = np.arange(128)[:, None]
    q = np.arange(128)[None, :]
    tabs = []
    for j in range(5):
        dist = q - k + 128 * (4 - j)
        idx = np.clip(dist, -REL_CLIP, REL_CLIP) + REL_CLIP
        t = rel_bias[:, idx]
        kc = (k // 64) + 2 * (j - 4)
        qc = q // 64
        valid = (kc <= qc) & (kc >= qc - 8)
        t = np.where(valid[None], t, np.float32(-1e30))
        tabs.append(t)
    return np.ascontiguousarray(np.stack(tabs, axis=1).astype(np.float32))


def prep_inputs(inputs, S, NSEQ, ncores):
    f = lambda a: np.ascontiguousarray(np.asarray(a, dtype=np.float32))
    shared = {
        "ln_in_g": f(inputs["ln_in_g"]), "ln_in_b": f(inputs["ln_in_b"]),
        "bias_tab": _bias_table(f(inputs["rel_bias"])),
        "w_in": f(inputs["w_in"]), "tok_mix": f(inputs["tok_mix"]),
        "decay_base": f(inputs["decay_base"]), "decay_up": f(inputs["decay_up"]),
        "aaa_base": f(inputs["aaa_base"]), "aaa_up": f(inputs["aaa_up"]), "gate_up": f(inputs["gate_up"]),
        "k_k": f(inputs["k_k"]), "k_a": f(inputs["k_a"]), "r_k": f(inputs["r_k"]).reshape(-1, 512),
        "vres_base": f(inputs["vres_base"]), "vres_down": f(inputs["vres_down"]), "vres_up": f(inputs["vres_up"]),
        "gn_g": f(inputs["gn_g"]), "gn_b": f(inputs["gn_b"]),
        "w_branch_attn": f(inputs["w_branch_attn"]), "w_branch_rwkv": f(inputs["w_branch_rwkv"]), "w_out": f(inputs["w_out"]),
        "router_w": np.ascontiguousarray(np.concatenate([f(inputs["router_grp"]), f(inputs["router_exp"])], axis=-1)),
        "router_b": np.ascontiguousarray(np.concatenate([f(inputs["router_grp_bias"]), f(inputs["router_exp_bias"])], axis=-1)),
        "exp_gate": f(inputs["exp_gate"]), "exp_up": f(inputs["exp_up"]), "exp_down": f(inputs["exp_down"]),
        "ple_proj": f(inputs["ple_proj"]), "ple_gate": f(inputs["ple_gate"]),
        "ln_g": f(inputs["ln_g"]), "ln_b": f(inputs["ln_b"]),
    }
    x = f(inputs["x"]); p = f(inputs["p"])
    maps = []
    for c in range(ncores):
        m = dict(shared)
        m["x"] = np.ascontiguousarray(x[c * NSEQ:(c + 1) * NSEQ])
        m["p"] = np.ascontiguousarray(p[:, c * NSEQ:(c + 1) * NSEQ])
        maps.append(m)
    return maps


def kernel(**inputs):
    x = np.asarray(inputs["x"])
    B, S, _ = x.shape
    ncores = 8
    NSEQ = B // ncores
    key = (S, NSEQ)
    if key not in _NC_CACHE:
        _NC_CACHE[key] = build_nc(S, NSEQ)
    nc = _NC_CACHE[key]
    maps = prep_inputs(inputs, S, NSEQ, ncores)
    res = run_bass_kernel_spmd(nc, maps, core_ids=list(range(ncores)))
    return np.concatenate([np.asarray(r["y"], dtype=np.float32) for r in res.results], axis=0)
```

```python
import contextlib
import numpy as np
import concourse.bass as bass
import concourse.mybir as mybir
from concourse.bass_utils import run_bass_kernel_spmd

F32 = mybir.dt.float32
BF16 = mybir.dt.bfloat16
AF = mybir.ActivationFunctionType
ALU = mybir.AluOpType

D = 1024
KC = 8
NIN = 5408
ALPHA = 4.0 ** 0.25
LN_EPS = 1e-5
GN_EPS = 64e-5
C0 = float(np.exp(-0.5))
N_DMA_SEMS = 40
SAME_ENGINE_SYNC = True
GRAN = 64
STOP = None
ABLK = None
BIS = 0


class Op:
    __slots__ = ("eng", "fn", "deps", "dma", "idx", "signal", "tick", "dsem", "dval", "prev_dval", "sw", "gen")

    def __init__(self, eng, fn, dma):
        self.eng = eng
        self.fn = fn
        self.dma = dma
        self.deps = set()
        self.signal = False
        self.tick = 0
        self.dsem = None
        self.dval = 0
        self.prev_dval = 0
        self.sw = None
        self.gen = 0


class Prog:
    ENGS = ("pe", "act", "dve", "pool", "sp")

    def __init__(self, nc):
        self.nc = nc
        self.ops = []
        self.res = {}
        self.ndma = 0
        self.swgen = {}

    def add(self, eng, fn, reads=(), writes=(), dma=False, sw=None):
        op = Op(eng, fn, dma)
        op.idx = len(self.ops)
        res = self.res
        psr = [r for r in reads if r[0] == "ps"]
        if psr:
            reads = [r for r in reads if r[0] != "ps"]
            writes = list(writes) + psr
        for r in reads:
            st = res.get(r)
            if st is not None and st[0] is not None:
                op.deps.add(st[0])
        for w in writes:
            st = res.get(w)
            if st is not None:
                if st[0] is not None:
                    op.deps.add(st[0])
                op.deps.update(st[1])
        for r in reads:
            st = res.get(r)
            if st is None:
                res[r] = [None, [op.idx]]
            else:
                st[1].append(op.idx)
        for w in writes:
            res[w] = [op.idx, []]
        op.deps.discard(op.idx)
        if dma and sw is not None:
            op.sw = sw
            op.gen = self.swgen.get(sw, 0)
            self.swgen[sw] = op.gen + 1
        elif dma:
            op.dsem = self.ndma % N_DMA_SEMS
            self.ndma += 1
        self.ops.append(op)
        return op

    def emit(self):
        nc = self.nc
        ops = self.ops
        for op in ops:
            for d in op.deps:
                dop = ops[d]
                if dop.dma:
                    continue
                if dop.eng == op.eng and (dop.eng == "pe" or not SAME_ENGINE_SYNC):
                    continue
                dop.signal = True
        ticks = {e: 0 for e in self.ENGS}
        dcount = [0] * N_DMA_SEMS
        for op in ops:
            if op.dma and op.sw is not None:
                continue
            if op.dma:
                op.prev_dval = dcount[op.dsem]
                dcount[op.dsem] += 16
                op.dval = dcount[op.dsem]
            elif op.signal:
                ticks[op.eng] += 1
                op.tick = ticks[op.eng]
        per_eng = {e: [o for o in ops if o.eng == e] for e in self.ENGS}
        with contextlib.ExitStack() as es:
            esem = {e: es.enter_context(nc.semaphore("s_" + e)) for e in ("pe", "act", "dve", "pool")}
            dsems = [es.enter_context(nc.semaphore("d%d" % i)) for i in range(N_DMA_SEMS)]
            swsems = {}
            for i, slot in enumerate(sorted(self.swgen.keys(), key=str)):
                swsems[slot] = [es.enter_context(nc.semaphore("w%d_%d" % (i, j))) for j in range(2)]
            block = es.enter_context(nc.Block())

            def run(engname, eng):
                waited = {}

                def wait(key, sem, val):
                    if waited.get(key, 0) >= val:
                        return
                    eng.wait_ge(sem, val)
                    waited[key] = val

                for op in per_eng[engname]:
                    for d in sorted(op.deps):
                        dop = ops[d]
                        if dop.dma and dop.sw is not None:
                            wait(("sw", dop.sw, dop.gen), swsems[dop.sw][dop.gen % 2], 16)
                        elif dop.dma:
                            wait(("d", dop.dsem), dsems[dop.dsem], dop.dval)
                        else:
                            if dop.eng == engname and (engname == "pe" or not SAME_ENGINE_SYNC):
                                continue
                            wait(("e", dop.eng), esem[dop.eng], dop.tick)
                    if op.dma and op.sw is not None:
                        if op.gen >= 1:
                            wait(("sw", op.sw, op.gen - 1), swsems[op.sw][(op.gen - 1) % 2], 16)
                            eng.sem_clear(swsems[op.sw][(op.gen - 1) % 2])
                        op.fn(eng).then_inc(swsems[op.sw][op.gen % 2], 16)
                    elif op.dma:
                        if op.prev_dval > 0:
                            wait(("d", op.dsem), dsems[op.dsem], op.prev_dval)
                        op.fn(eng).then_inc(dsems[op.dsem], 16)
                    else:
                        ins = op.fn(eng)
                        if op.signal:
                            ins.then_inc(esem[engname], 1)
                for op in per_eng[engname]:
                    if op.dma and op.sw is None:
                        wait(("d", op.dsem), dsems[op.dsem], op.dval)

            if per_eng["pe"]:
                block.tensor(lambda e: run("pe", e))
            if per_eng["act"]:
                block.scalar(lambda e: run("act", e))
            if per_eng["dve"]:
                block.vector(lambda e: run("dve", e))
            if per_eng["pool"]:
                block.gpsimd(lambda e: run("pool", e))
            if per_eng["sp"]:
                block.sync(lambda e: run("sp", e))


class Tile:
    def __init__(self, arena, off, nwords, dtype, shape, parts=128):
        self.off = off
        self.n = nwords
        self.esz = 4 if dtype == F32 else 2
        ne = int(np.prod(shape))
        nwx = ne if dtype == F32 else (ne + 1) // 2
        v = arena[0:parts, off:off + nwx]
        if dtype != F32:
            v = v.bitcast(dtype)[:, 0:ne]
        self.shape = tuple(shape)
        if len(shape) == 2:
            v = v.rearrange("p (a b) -> p a b", b=shape[1])
        elif len(shape) == 3:
            v = v.rearrange("p (a b c) -> p a b c", b=shape[1], c=shape[2])
        self.ap = v
        self.inner = int(np.prod(shape[1:])) if len(shape) > 1 else 1

    def k(self, lo=None, hi=None):
        if lo is None:
            lo, hi = 0, self.n * 4 // self.esz
        w0 = self.off + (lo * self.esz) // 4
        w1 = self.off + (hi * self.esz + 3) // 4
        return [("sb", g) for g in range(w0 // GRAN, (w1 - 1) // GRAN + 1)]

    def ks(self, i, n=1):
        return self.k(i * self.inner, (i + n) * self.inner)


class Arena:
    def __init__(self, nc, es, nwords):
        self.t = es.enter_context(nc.sbuf_tensor("arena", [128, nwords], F32))
        self.ap = self.t[:]
        self.nwords = nwords
        self.cur = 0

    def alloc(self, shape, dtype=F32, parts=128):
        ne = int(np.prod(shape))
        nw = ne if dtype == F32 else (ne + 1) // 2
        nw = (nw + GRAN - 1) // GRAN * GRAN
        assert self.cur + nw <= self.nwords, ("arena overflow", self.cur, nw, self.nwords)
        t = Tile(self.ap, self.cur, nw, dtype, shape, parts)
        self.cur += nw
        return t


def build_nc(S, NSEQ, DEPTH=2, dbg=None):
    NB = S // 512
    NT = S // 128
    nc = bass.Bass("TRN2", target_bir_lowering=False)
    dram = lambda name, shape, kind="ExternalInput": nc.dram_tensor(name, shape, F32, kind=kind).ap()
    x_d = dram("x", [NSEQ, S, D])
    p_d = dram("p", [DEPTH, NSEQ, S, 256])
    ln_in_g = dram("ln_in_g", [D]); ln_in_b = dram("ln_in_b", [D])
    bias_d = dram("bias_tab", [8, 5, 128, 128])
    w_in = dram("w_in", [DEPTH, D, NIN])
    tok_mix = dram("tok_mix", [DEPTH, 1824])
    decay_base = dram("decay_base", [DEPTH, 512]); decay_up = dram("decay_up", [DEPTH, 64, 512])
    aaa_base = dram("aaa_base", [DEPTH, 512]); aaa_up = dram("aaa_up", [DEPTH, 64, 512])
    gate_up = dram("gate_up", [DEPTH, 160, 512])
    k_k = dram("k_k", [DEPTH, 512]); k_a = dram("k_a", [DEPTH, 512]); r_k = dram("r_k", [DEPTH, 512])
    vres_base = dram("vres_base", [DEPTH - 1, 512]); vres_down = dram("vres_down", [DEPTH - 1, 512, 32])
    vres_up = dram("vres_up", [DEPTH - 1, 32, 512])
    gn_g = dram("gn_g", [DEPTH, 512]); gn_b = dram("gn_b", [DEPTH, 512])
    w_a = dram("w_branch_attn", [DEPTH, 512, D]); w_b = dram("w_branch_rwkv", [DEPTH, 512, D])
    w_out = dram("w_out", [DEPTH, D, D])
    router_w = dram("router_w", [DEPTH, D, 36]); router_b = dram("router_b", [DEPTH, 36])
    exp_gate = dram("exp_gate", [DEPTH, 32, D, 256]); exp_up = dram("exp_up", [DEPTH, 32, D, 256])
    exp_down = dram("exp_down", [DEPTH, 32, 256, D])
    ple_proj = dram("ple_proj", [DEPTH, 256, D]); ple_gate = dram("ple_gate", [DEPTH, D, D])
    ln_g = dram("ln_g", [DEPTH, 3, D]); ln_b = dram("ln_b", [DEPTH, 3, D])
    y_d = dram("y", [NSEQ, S, D], kind="ExternalOutput")
    xs_d = dram("xs_scr", [S, D], kind="Internal")
    dramh = lambda name, shape: nc.dram_tensor(name, shape, BF16, kind="Internal").ap()
    xT_d = dramh("xT_scr", [KC, 128, S])
    w_in_h = dramh("w_in_h", [DEPTH, D, NIN]); w_a_h = dramh("w_a_h", [DEPTH, 512, D]); w_b_h = dramh("w_b_h", [DEPTH, 512, D])
    w_out_h = dramh("w_out_h", [DEPTH, D, D]); exp_gate_h = dramh("exp_gate_h", [DEPTH, 32, D, 256])
    exp_up_h = dramh("exp_up_h", [DEPTH, 32, D, 256]); exp_down_h = dramh("exp_down_h", [DEPTH, 32, 256, D])
    ple_proj_h = dramh("ple_proj_h", [DEPTH, 256, D]); ple_gate_h = dramh("ple_gate_h", [DEPTH, D, D])
    bias_h = dramh("bias_h", [8, 5, 128, 128])
    vf_d = dram("vf_scr", [4, 128, S], kind="Internal")
    dbg_d = {}
    if dbg:
        for name, shape in dbg.items():
            dbg_d[name] = dram("dbg_" + name, shape, kind="ExternalOutput")

    P = Prog(nc)
    es = contextlib.ExitStack()
    with es:
        A = Arena(nc, es, 53000)
        psum = es.enter_context(nc.psum_tensor("ps", [128, 8, 512], F32))
        bank_ctr = [0]

        def bank():
            b = bank_ctr[0] % 6
            bank_ctr[0] += 1
            return b

        def bank2():
            b = bank_ctr[0] % 6
            if b % 2:
                bank_ctr[0] += 1
                b = bank_ctr[0] % 6
            bank_ctr[0] += 2
            return b

        PK = lambda b: [("ps", b)]

        def MM(out, lhsT, rhs, start, stop, reads, writes):
            P.add("pe", lambda e: e.matmul(out, lhsT=lhsT, rhs=rhs, start=start, stop=stop), reads, writes)

        def TR(out, in_, ident, reads, writes):
            P.add("pe", lambda e: e.transpose(out, in_, ident), reads, writes)

        def ACT(out, in_, func, reads, writes, bias=None, scale=None):
            kw = {}
            if bias is not None:
                kw["bias"] = bias
            if scale is not None:
                kw["scale"] = scale
            P.add("act", lambda e: e.activation(out=out, in_=in_, func=func, **kw), reads, writes)

        def TT(eng, out, in0, in1, op, reads, writes):
            P.add(eng, lambda e: e.tensor_tensor(out=out, in0=in0, in1=in1, op=op), reads, writes)

        def TS(eng, out, in0, s1, op0, reads, writes, s2=None, op1=None):
            if op1 is None:
                P.add(eng, lambda e: e.tensor_scalar(out=out, in0=in0, scalar1=s1, scalar2=None, op0=op0), reads, writes)
            else:
                P.add(eng, lambda e: e.tensor_scalar(out=out, in0=in0, scalar1=s1, scalar2=s2, op0=op0, op1=op1), reads, writes)

        def STT(eng, out, in0, scalar, in1, op0, op1, reads, writes):
            P.add(eng, lambda e: e.scalar_tensor_tensor(out=out, in0=in0, scalar=scalar, in1=in1, op0=op0, op1=op1), reads, writes)

        def CP(eng, out, in_, reads, writes):
            if eng == "act":
                P.add("dve", lambda e: e.tensor_copy(out=out, in_=in_), reads, writes)
            else:
                P.add(eng, lambda e: e.tensor_copy(out=out, in_=in_), reads, writes)

        def RECIP(out, in_, reads, writes):
            P.add("dve", lambda e: e.reciprocal(out=out, in_=in_), reads, writes)

        def MEMSET(eng, ap, val, writes):
            P.add(eng, lambda e: e.memset(ap, val), (), writes)

        def DMA(eng, out, in_, reads, writes, sw=None, **kw):
            P.add(eng, lambda e: e.dma_start(out=out, in_=in_, **kw), reads, writes, dma=True, sw=sw)

        ident = A.alloc([128]); identb = A.alloc([128], BF16)
        onesbd = A.alloc([128])
        maskU = A.alloc([4, 128], parts=64)
        maskL = A.alloc([2, 64], parts=64)
        identT = A.alloc([2, 64], parts=64)
        rmask = A.alloc([512])
        biasT = A.alloc([40, 128], BF16)
        KI = ident.k(); KIB = identb.k()
        MEMSET("pool", ident.ap, 0.0, KI)
        P.add("pool", lambda e: e.affine_select(out=ident.ap, in_=ident.ap, pattern=[[-1, 128]], compare_op=ALU.not_equal,
                                                fill=1.0, base=0, channel_multiplier=1), KI, KI)
        CP("pool", identb.ap, ident.ap, KI, KIB)
        MEMSET("pool", onesbd.ap, 0.0, onesbd.k())
        MEMSET("pool", onesbd.ap[0:64, 0:64], 1.0, onesbd.k())
        MEMSET("pool", onesbd.ap[64:128, 64:128], 1.0, onesbd.k())
        MEMSET("pool", maskU.ap, 1.0, maskU.k())
        for q in range(4):
            P.add("pool", (lambda q: lambda e: e.affine_select(out=maskU.ap[:, q, 0:64], in_=maskU.ap[:, q, 0:64], pattern=[[1, 64]],
                  compare_op=ALU.is_ge, fill=0.0, base=-1, channel_multiplier=-1))(q), maskU.k(), maskU.k())
            P.add("pool", (lambda q: lambda e: e.affine_select(out=maskU.ap[:, q, 64:128], in_=maskU.ap[:, q, 64:128], pattern=[[1, 64]],
                  compare_op=ALU.is_ge, fill=0.0, base=0, channel_multiplier=-1))(q), maskU.k(), maskU.k())
        MEMSET("pool", maskL.ap, 1.0, maskL.k())
        for q in range(2):
            P.add("pool", (lambda q: lambda e: e.affine_select(out=maskL.ap[:, q, :], in_=maskL.ap[:, q, :], pattern=[[-1, 64]],
                  compare_op=ALU.is_ge, fill=0.0, base=-1, channel_multiplier=1))(q), maskL.k(), maskL.k())
        CP("pool", identT.ap[:, 0, :], ident.ap[0:64, 0:64], KI, identT.k())
        CP("pool", identT.ap[:, 1, :], ident.ap[0:64, 0:64], KI, identT.k())
        MEMSET("pool", rmask.ap, 1.0, rmask.k())
        MEMSET("pool", rmask.ap.rearrange("p (c t) -> p c t", t=64)[:, :, 0:1], 0.0, rmask.k())

        xstg = [A.alloc([KC, 128], BF16) for _ in range(2)]
        xsc = [0]
        NWB = 4
        wpool = [A.alloc([KC, 512], BF16) for _ in range(NWB)]
        wctr = [0]

        def wbuf():
            t = wpool[wctr[0] % NWB]
            t.idx = wctr[0] % NWB
            wctr[0] += 1
            return t

        def cast_load(dst, src, dkeys):
            DMA("sp", dst, src, ["wh_all"], dkeys)

        lnp = A.alloc([2, D])
        comb = A.alloc([NT, 32])
        prm = A.alloc([16, 4])
        PRM = {n: i for i, n in enumerate(["decay_base", "aaa_base", "k_k", "k_a", "r_k", "gn_g", "gn_b", "vres_base"])}
        mu = A.alloc([2, 16])
        lora = A.alloc([3, 512])
        vrd = A.alloc([4, 32]); vru = A.alloc([512], parts=32)
        rw = A.alloc([KC, 36]); rb = A.alloc([36])
        lastc = A.alloc([16])
        Sst = A.alloc([4, 64])

        mark_common = A.cur

        def precast():
            stg = [A.alloc([4096]) for _ in range(4)]
            outb = [A.alloc([4096], BF16) for _ in range(4)]
            cnt = [0]
            allk = []

            def chunk(src3, dst3):
                a, b_ = src3.shape[1], src3.shape[2]
                i = cnt[0]; cnt[0] += 1
                st_ = stg[i % 4]; ob = outb[i % 4]
                sv = st_.ap[:, 0:a * b_].rearrange("p (a b) -> p a b", b=b_)
                ov = ob.ap[:, 0:a * b_].rearrange("p (a b) -> p a b", b=b_)
                DMA("sp", sv, src3, (), st_.k())
                CP("dve", ov, sv, st_.k(), ob.k())
                key = ("whc", i)
                DMA("act", dst3, ov, ob.k(), [key])
                allk.append(key)

            def cast2d(src2, dst2):
                R, C = src2.shape
                nrb = R // 128
                if C <= 4096:
                    Astep = max(1, 4096 // C)
                    sv = src2.rearrange("(a p) n -> p a n", p=128)
                    dv = dst2.rearrange("(a p) n -> p a n", p=128)
                    for a0 in range(0, nrb, Astep):
                        a1 = min(nrb, a0 + Astep)
                        chunk(sv[:, a0:a1, :], dv[:, a0:a1, :])
                else:
                    sv = src2.rearrange("(a p) n -> p a n", p=128)
                    dv = dst2.rearrange("(a p) n -> p a n", p=128)
                    for a0 in range(nrb):
                        for c0 in range(0, C, 4096):
                            c1 = min(C, c0 + 4096)
                            chunk(sv[:, a0:a0 + 1, c0:c1], dv[:, a0:a0 + 1, c0:c1])

            cast2d(w_in.rearrange("l k n -> (l k) n"), w_in_h.rearrange("l k n -> (l k) n"))
            cast2d(w_a.rearrange("l k n -> (l k) n"), w_a_h.rearrange("l k n -> (l k) n"))
            cast2d(w_b.rearrange("l k n -> (l k) n"), w_b_h.rearrange("l k n -> (l k) n"))
            cast2d(w_out.rearrange("l k n -> (l k) n"), w_out_h.rearrange("l k n -> (l k) n"))
            cast2d(ple_proj.rearrange("l k n -> (l k) n"), ple_proj_h.rearrange("l k n -> (l k) n"))
            cast2d(ple_gate.rearrange("l k n -> (l k) n"), ple_gate_h.rearrange("l k n -> (l k) n"))
            cast2d(bias_d.rearrange("h j k q -> (h j k) q"), bias_h.rearrange("h j k q -> (h j k) q"))
            DMA("sp", stg[0].ap[0:1, 0:8], ln_in_g[0:8].rearrange("(o n) -> o n", o=1), allk, stg[0].k() + ["wh_all"])
            A.cur = mark_common


        def load_ln(gsrc, bsrc, lp=None):
            lp = lp or lnp
            DMA("sp", lp.ap[:, 0, :], gsrc.partition_broadcast(128), (), lp.ks(0))
            DMA("sp", lp.ap[:, 1, :], bsrc.partition_broadcast(128), (), lp.ks(1))

        def layer_norm_g(h, hk, out_ap, out_keys, st, mv, lp=None):
            lp = lp or lnp
            for c in range(2):
                P.add("dve", (lambda c: lambda e: e.bn_stats(out=st.ap[:, c, :], in_=h[:, c * 512:(c + 1) * 512]))(c), hk, st.k())
            P.add("dve", lambda e: e.bn_aggr(out=mv.ap[:, 0:2], in_=st.ap), st.k(), mv.k())
            TS("dve", mv.ap[:, 2:3], mv.ap[:, 1:2], LN_EPS, ALU.add, mv.k(), mv.k())
            yield
            ACT(mv.ap[:, 2:3], mv.ap[:, 2:3], AF.Sqrt, mv.k(), mv.k())
            yield
            RECIP(mv.ap[:, 2:3], mv.ap[:, 2:3], mv.k(), mv.k())
            TS("dve", h, h, mv.ap[:, 0:1], ALU.subtract, hk + mv.k(), hk, s2=mv.ap[:, 2:3], op1=ALU.mult)
            yield
            TT("dve", h, h, lp.ap[:, 0, :], ALU.mult, hk + lp.ks(0), hk)
            yield
            TT("pool", out_ap, h, lp.ap[:, 1, :], ALU.add, hk + lp.ks(1), out_keys)
            yield

        def layer_norm(h, hk, out_ap, out_keys, st, mv, lp=None):
            for _ in layer_norm_g(h, hk, out_ap, out_keys, st, mv, lp):
                pass

        def interleave(gens, width):
            gens = list(gens)
            nxt = 0
            active = []
            while nxt < len(gens) or active:
                while len(active) < width and nxt < len(gens):
                    active.append(gens[nxt])
                    nxt += 1
                for g_ in list(active):
                    try:
                        next(g_)
                    except StopIteration:
                        active.remove(g_)

        def to_feature_major(xt_tok, xk, tok0, f32_out=None, sb=None, to_dram=True):
            if to_dram:
                stg = xstg[xsc[0] % 2]
                xsc[0] += 1
            for half in range(2):
                b = bank()
                for cc in range(4):
                    c = half * 4 + cc
                    TR(psum[:, b, cc * 128:(cc + 1) * 128], xt_tok[:, c * 128:(c + 1) * 128], ident.ap, xk + KI, PK(b))
                src = psum[:, b, :].rearrange("p (c t) -> p c t", t=128)
                if sb is not None:
                    tl, ncols, col0 = sb
                    wk = []
                    for c in range(half * 4, half * 4 + 4):
                        wk += tl.k(c * ncols + col0, c * ncols + col0 + 128)
                    CP("dve", tl.ap[:, half * 4:half * 4 + 4, col0:col0 + 128], src, PK(b), wk)
                if to_dram:
                    CP("dve", stg.ap[:, half * 4:half * 4 + 4, :], src, PK(b), stg.ks(half * 4, 4))
                if f32_out is not None:
                    CP("dve", f32_out.ap[:, half * 4:half * 4 + 4, :], src, PK(b), f32_out.ks(half * 4, 4))
            if to_dram:
                DMA("sp", xT_d[:, :, tok0:tok0 + 128].rearrange("c p t -> p c t"), stg.ap, stg.k(), [("xTd", tok0 // 128)])

        def load_layer_params(l):
            fm = lambda src: src.rearrange("(c p) -> p c", p=128)
            lst = [("decay_base", decay_base[l]), ("aaa_base", aaa_base[l]), ("k_k", k_k[l]), ("k_a", k_a[l]),
                   ("r_k", r_k[l]), ("gn_g", gn_g[l]), ("gn_b", gn_b[l])]
            if l > 0:
                lst.append(("vres_base", vres_base[l - 1]))
            for name, src in lst:
                DMA("sp", prm.ap[:, PRM[name], :], fm(src), (), prm.k(), allow_slow_non_contiguous=True)
            DMA("sp", mu.ap[:, 0, 0:14], tok_mix[l, 0:1792].rearrange("(c p) -> p c", p=128), (), mu.k(), allow_slow_non_contiguous=True)
            DMA("sp", mu.ap[0:32, 0, 14:15], tok_mix[l, 1792:1824].rearrange("(p o) -> p o", o=1), (), mu.k())
            TS("pool", mu.ap[:, 1, 0:14], mu.ap[:, 0, 0:14], -1.0, ALU.mult, mu.k(), mu.k(), s2=1.0, op1=ALU.add)
            TS("pool", mu.ap[0:32, 1, 14:15], mu.ap[0:32, 0, 14:15], -1.0, ALU.mult, mu.k(), mu.k(), s2=1.0, op1=ALU.add)
            DMA("sp", lora.ap[0:64, 0, :], decay_up[l], (), lora.ks(0))
            DMA("sp", lora.ap[64:128, 0, :], aaa_up[l], (), lora.ks(0))
            DMA("sp", lora.ap[:, 1, :], gate_up[l, 0:128, :], (), lora.ks(1))
            DMA("sp", lora.ap[0:32, 2, :], gate_up[l, 128:160, :], (), lora.ks(2))
            if l > 0:
                DMA("sp", vrd.ap, vres_down[l - 1].rearrange("(c p) r -> p c r", p=128), (), vrd.k())
                DMA("sp", vru.ap, vres_up[l - 1], (), vru.k())
            DMA("sp", rw.ap, router_w[l].rearrange("(k p) n -> p k n", p=128), (), rw.k())
            DMA("sp", rb.ap, router_b[l].partition_broadcast(128), (), rb.k())

        def wload(src_ap, shape):
            t = wbuf()
            n = int(np.prod(shape))
            v = t.ap.rearrange("p a b -> p (a b)")[:, 0:n]
            if len(shape) == 2:
                v = v.rearrange("p (a b) -> p a b", b=shape[1])
            cast_load(v, src_ap, t.k())
            return v, t.k()

        def phaseA(s, l):
            A.cur = mark_common
            kT = A.alloc([4, 1024], BF16); vring = A.alloc([8, 8, 65], BF16)
            xTa = A.alloc([KC, 1024], BF16)
            yAT = A.alloc([4, 512], BF16); yBT = A.alloc([4, 512], BF16)
            st6s = [A.alloc([2, 6]) for _ in range(2)]; mv4s = [A.alloc([4]) for _ in range(2)]
            lgs = [A.alloc([36]) for _ in range(2)]; rts = [A.alloc([64]) for _ in range(2)]
            twd = A.alloc([512]); tgd1 = A.alloc([512]); tgd2 = A.alloc([512], parts=32)
            v_all = A.alloc([4, 512]); vd = A.alloc([512], parts=32)
            mark_att = A.cur
            qT = A.alloc([4, 512], BF16)
            PT = [A.alloc([4, 128], BF16) for _ in range(6)]
            ytok = A.alloc([512]); rc = A.alloc([8])
            end_att = A.cur
            A.cur = mark_att
            NSL = 4
            tokT = [A.alloc([3, 128], parts=64) for _ in range(NSL)]
            AT = [A.alloc([4, 128], parts=64) for _ in range(NSL)]
            X0 = [A.alloc([2, 64], parts=64) for _ in range(NSL)]
            Xn = [[A.alloc([4, 64], parts=64) for _ in range(2)] for _ in range(NSL)]
            Tt = [[A.alloc([2, 64], parts=64) for _ in range(2)] for _ in range(NSL)]
            G0 = [A.alloc([2, 64], parts=64) for _ in range(NSL)]
            A.cur = max(A.cur, end_att)
            mark_r = A.cur
            t_r = A.alloc([512]); t_k = A.alloc([512]); t_sg = A.alloc([512]); t_cum = A.alloc([512])
            t_pin = A.alloc([512]); t_inv = A.alloc([512]); t_a = A.alloc([512]); t_kk = A.alloc([512])
            t_sq = A.alloc([512]); t_km = A.alloc([512]); t_bon = A.alloc([512])
            bt = A.alloc([512]); kt = A.alloc([512]); yT = A.alloc([512]); gnt = A.alloc([512])
            ar = A.alloc([8, 2, 64]); pC = A.alloc([8]); t_vf = A.alloc([512])
            Wsb = A.alloc([2, 64], parts=64); Usb = A.alloc([2, 64], parts=64); Stmp = A.alloc([64])
            end_r = A.cur
            A.cur = mark_r
            sga = A.alloc([512]); sgb = A.alloc([512]); m1 = A.alloc([512]); m2 = A.alloc([512])
            zT = A.alloc([8, 512], BF16)
            hbufs = [A.alloc([D]) for _ in range(2)]; x0s = [A.alloc([D]) for _ in range(2)]; x1fms = [A.alloc([KC, 128]) for _ in range(2)]
            A.cur = max(A.cur, end_r)
            print("phaseA arena words", A.cur, "of", A.nwords)

            load_layer_params(l)
            load_ln(ln_g[l, 0], ln_b[l, 0])
            MEMSET("pool", lastc.ap, 0.0, lastc.k())
            MEMSET("pool", Sst.ap, 0.0, Sst.k())
            MEMSET("pool", vring.ap[:, :, :, 64:65], 1.0, vring.k())
            wv = w_in_h[l].rearrange("(k p) n -> p k n", p=128)

            def shift(pc_ap, np_, mc, out_ap, out_keys, b):
                m = mu.ap[0:np_, 0, mc:mc + 1]; om = mu.ap[0:np_, 1, mc:mc + 1]
                TS("dve", out_ap, pc_ap, om, ALU.mult, PK(b) + mu.k(), out_keys)
                STT("dve", out_ap[:, 1:512], pc_ap[:, 0:511], m, out_ap[:, 1:512], ALU.mult, ALU.add, PK(b) + out_keys + mu.k(), out_keys)
                STT("dve", out_ap[:, 0:1], lastc.ap[0:np_, mc:mc + 1], m, out_ap[:, 0:1], ALU.mult, ALU.add, lastc.k() + out_keys + mu.k(), out_keys)
                CP("dve", lastc.ap[0:np_, mc:mc + 1], pc_ap[:, 511:512], PK(b), lastc.k())

            def load_xblock(bb):
                rp_ = (bb % 2) * 512
                wk_ = []
                for c in range(KC):
                    wk_ += xTa.k(c * 1024 + rp_, c * 1024 + rp_ + 512)
                DMA("sp", xTa.ap[:, :, rp_:rp_ + 512], xT_d[:, :, bb * 512:(bb + 1) * 512].rearrange("c p t -> p c t"),
                    [("xTd", bb * 4 + i) for i in range(4)], wk_)

            load_xblock(0)
            for b in range(NB):
                t0 = b * 512
                xr = (b % 2) * 512
                if b + 1 < NB:
                    load_xblock(b + 1)
                xk = []
                for c in range(KC):
                    xk += xTa.k(c * 1024 + xr, c * 1024 + xr + 512)

                def proj_fm(wt, wk, col, ncol=128, kparts=128):
                    bk = bank()
                    for k in range(KC):
                        MM(psum[0:ncol, bk, :], wt[:, k, col:col + ncol], xTa.ap[:, k, xr:xr + 512], k == 0, k == KC - 1, wk + xk, PK(bk))
                    return bk

                wt, wk = wload(wv[:, :, 0:512], [KC, 512])
                for c in range(4):
                    bk = proj_fm(wt, wk, c * 128)
                    TS("dve", qT.ap[:, c, :], psum[:, bk, :], 0.125, ALU.mult, PK(bk), qT.ks(c))
                wt, wk = wload(wv[:, :, 512:1024], [KC, 512])
                rp = (b % 2) * 512
                for c in range(4):
                    bk = proj_fm(wt, wk, c * 128)
                    CP("act", kT.ap[:, c, rp:rp + 512], psum[:, bk, :], PK(bk), kT.k(c * 1024 + rp, c * 1024 + rp + 512))
                wt, wk = wload(wv[:, :, 1024:1536], [KC, 512])
                for tt in range(4):
                    bk = bank()
                    for k in range(KC):
                        MM(psum[:, bk, :], xTa.ap[:, k, xr + tt * 128:xr + tt * 128 + 128], wt[:, k, :], k == 0, k == KC - 1, wk + xk, PK(bk))
                    slot = (b % 2) * 4 + tt
                    CP("dve", vring.ap[:, slot, :, 0:64], psum[:, bk, :].rearrange("p (h d) -> p h d", d=64), PK(bk), vring.ks(slot))

                for qi in range(4):
                    qb = b * 4 + qi
                    js = [j for j in range(5) if qb - 4 + j >= 0]
                    for hg in range(2):
                        pts = {}
                        for ji, j in enumerate(js):
                            kb = qb - 4 + j
                            ro = (kb % 8) * 128
                            bk = bank()
                            for hh in range(4):
                                h = hg * 4 + hh
                                c = h // 2; r0 = (h % 2) * 64
                                MM(psum[:, bk, hh * 128:(hh + 1) * 128], kT.ap[r0:r0 + 64, c, ro:ro + 128], qT.ap[r0:r0 + 64, c, qi * 128:(qi + 1) * 128],
                                   True, False, kT.k(c * 1024 + ro, c * 1024 + ro + 128) + qT.ks(c), PK(bk))
                                MM(psum[:, bk, hh * 128:(hh + 1) * 128], identb.ap, biasT.ap[:, h * 5 + j, :], False, True, KIB + biasT.ks(h * 5 + j), PK(bk))
                            pt = PT[ji]
                            ACT(pt.ap.rearrange("p h q -> p (h q)"), psum[:, bk, :], AF.Exp, PK(bk), pt.k())
                            pts[j] = pt
                        bk = bank()
                        pv = psum[:, bk, 0:260].rearrange("p (h d) -> p h d", d=65)
                        for hh in range(4):
                            h = hg * 4 + hh
                            for ji, j in enumerate(js):
                                kb = qb - 4 + j
                                slot = kb % 8
                                MM(pv[:, hh, :], pts[j].ap[:, hh, :], vring.ap[:, slot, h, :], ji == 0, ji == len(js) - 1,
                                   pts[j].k() + vring.ks(slot), PK(bk))
                        RECIP(rc.ap[:, hg * 4:hg * 4 + 4].rearrange("p (h o) -> p h o", o=1), pv[:, :, 64:65], PK(bk), rc.k())
                        TT("dve", ytok.ap[:, hg * 256:(hg + 1) * 256].rearrange("p (h d) -> p h d", d=64), pv[:, :, 0:64],
                           rc.ap[:, hg * 4:hg * 4 + 4].rearrange("p (h o) -> p h o", o=1).to_broadcast([128, 4, 64]), ALU.mult,
                           PK(bk) + rc.k(), ytok.k())
                    bk = bank()
                    for c in range(4):
                        TR(psum[:, bk, c * 128:(c + 1) * 128], ytok.ap[:, c * 128:(c + 1) * 128], ident.ap, ytok.k() + KI, PK(bk))
                    wkeys = []
                    for c in range(4):
                        wkeys += yAT.k(c * 512 + qi * 128, c * 512 + qi * 128 + 128)
                    CP("act", yAT.ap[:, :, qi * 128:(qi + 1) * 128], psum[:, bk, :].rearrange("p (c t) -> p c t", t=128), PK(bk), wkeys)

                if BIS == 4:
                    continue
                wt_r, wk_r = wload(wv[:, :, 1536:2048], [KC, 512])
                wt_k, wk_k = wload(wv[:, :, 2048:2560], [KC, 512])
                wt_v, wk_v = wload(wv[:, :, 2560:3072], [KC, 512])
                for hp in range(4):
                    bk = proj_fm(wt_v, wk_v, hp * 128)
                    shift(psum[:, bk, :], 128, 8 + hp, v_all.ap[:, hp, :], v_all.ks(hp), bk)
                wt_l, wk_l = wload(wv[:, :, 3072:3360], [KC, 288])
                bk = proj_fm(wt_l, wk_l, 0)
                shift(psum[:, bk, :], 128, 12, twd.ap, twd.k(), bk)
                bk = proj_fm(wt_l, wk_l, 128)
                shift(psum[:, bk, :], 128, 13, tgd1.ap, tgd1.k(), bk)
                bk = proj_fm(wt_l, wk_l, 256, ncol=32)
                shift(psum[0:32, bk, :], 32, 14, tgd2.ap, tgd2.k(), bk)
                ACT(twd.ap[0:64, :], twd.ap[0:64, :], AF.Tanh, twd.k(), twd.k())
                ACT(tgd1.ap, tgd1.ap, AF.Sigmoid, tgd1.k(), tgd1.k())
                ACT(tgd2.ap, tgd2.ap, AF.Sigmoid, tgd2.k(), tgd2.k())
                if l == 0:
                    for hp in range(4):
                        DMA("sp", vf_d[hp, :, t0:t0 + 512], v_all.ap[:, hp, :], v_all.ks(hp), [("vf", hp, b)])
                else:
                    bk = bank()
                    for hp in range(4):
                        MM(psum[0:32, bk, :], vrd.ap[:, hp, :], v_all.ap[:, hp, :], hp == 0, hp == 3, vrd.k() + v_all.ks(hp), PK(bk))
                    CP("act", vd.ap, psum[0:32, bk, :], PK(bk), vd.k())

                def pp(name, hp):
                    return prm.ap[:, PRM[name], hp:hp + 1]

                c3 = lambda t: t.ap.rearrange("p (c t) -> p c t", t=64)
                arv = ar.ap

                def prep1(hp):
                    va = v_all.ap[:, hp, :]; vk = v_all.ks(hp)
                    bk = proj_fm(wt_r, wk_r, hp * 128)
                    shift(psum[:, bk, :], 128, hp, t_r.ap, t_r.k(), bk)
                    yield
                    bk = proj_fm(wt_k, wk_k, hp * 128)
                    shift(psum[:, bk, :], 128, 4 + hp, t_k.ap, t_k.k(), bk)
                    yield
                    if l > 0:
                        bk = bank()
                        MM(psum[:, bk, :], vru.ap[0:32, hp * 128:(hp + 1) * 128], vd.ap, True, True, vru.k() + vd.k(), PK(bk))
                        ACT(t_sq.ap, psum[:, bk, :], AF.Sigmoid, PK(bk) + prm.k(), t_sq.k(), bias=pp("vres_base", hp))
                        DMA("sp", t_vf.ap, vf_d[hp, :, t0:t0 + 512], [("vf", hp, b)], t_vf.k())
                        TT("dve", t_vf.ap, t_vf.ap, va, ALU.subtract, t_vf.k() + vk, t_vf.k())
                        yield
                        TT("dve", t_vf.ap, t_vf.ap, t_sq.ap, ALU.mult, t_vf.k() + t_sq.k(), t_vf.k())
                        TT("dve", va, va, t_vf.ap, ALU.add, vk + t_vf.k(), vk)
                        yield
                    bk = bank()
                    MM(psum[:, bk, :], lora.ap[0:64, 0, hp * 128:(hp + 1) * 128], twd.ap[0:64, :], True, True, lora.ks(0) + twd.k(), PK(bk))
                    ACT(t_sg.ap, psum[:, bk, :], AF.Sigmoid, PK(bk) + prm.k(), t_sg.k(), bias=pp("decay_base", hp))
                    yield
                    P.add("dve", lambda e: e.tensor_tensor_scan(out=t_cum.ap, data0=rmask.ap, data1=t_sg.ap, initial=0.0, op0=ALU.mult, op1=ALU.add),
                          rmask.k() + t_sg.k(), t_cum.k())
                    yield
                    ACT(t_pin.ap, t_cum.ap, AF.Exp, t_cum.k(), t_pin.k(), scale=-C0)
                    ACT(t_inv.ap, t_cum.ap, AF.Exp, t_cum.k(), t_inv.k(), scale=C0)
                    TT("dve", t_sg.ap, t_cum.ap, t_sg.ap, ALU.subtract, t_cum.k() + t_sg.k(), t_sg.k())
                    yield
                    ACT(t_cum.ap, t_sg.ap, AF.Exp, t_sg.k(), t_cum.k(), scale=-C0)
                    bk = bank()
                    MM(psum[:, bk, :], lora.ap[64:128, 0, hp * 128:(hp + 1) * 128], twd.ap[64:128, :], True, True, lora.ks(0) + twd.k(), PK(bk))
                    ACT(t_a.ap, psum[:, bk, :], AF.Sigmoid, PK(bk) + prm.k(), t_a.k(), bias=pp("aaa_base", hp))
                    yield
                    TS("dve", t_kk.ap, t_k.ap, pp("k_k", hp), ALU.mult, t_k.k() + prm.k(), t_kk.k())
                    TT("pool", t_sq.ap, t_kk.ap, t_kk.ap, ALU.mult, t_kk.k(), t_sq.k())
                    yield
                    bk = bank()
                    MM(psum[:, bk, :], onesbd.ap, t_sq.ap, True, True, onesbd.k() + t_sq.k(), PK(bk))
                    TS("dve", t_sq.ap, psum[:, bk, :], 1e-24, ALU.max, PK(bk), t_sq.k())
                    yield
                    ACT(t_sq.ap, t_sq.ap, AF.Sqrt, t_sq.k(), t_sq.k())
                    yield
                    RECIP(t_sq.ap, t_sq.ap, t_sq.k(), t_sq.k())
                    TT("dve", t_kk.ap, t_kk.ap, t_sq.ap, ALU.mult, t_kk.k() + t_sq.k(), t_kk.k())
                    yield
                    TS("dve", t_km.ap, t_a.ap, 1.0, ALU.subtract, t_a.k() + prm.k(), t_km.k(), s2=pp("k_a", hp), op1=ALU.mult)
                    STT("dve", t_km.ap, t_km.ap, 1.0, t_k.ap, ALU.add, ALU.mult, t_km.k() + t_k.k(), t_km.k())
                    yield
                    STT("dve", t_sq.ap, t_r.ap, pp("r_k", hp), t_km.ap, ALU.mult, ALU.mult, t_r.k() + t_km.k() + prm.k(), t_sq.k())
                    TT("dve", t_a.ap, t_a.ap, t_kk.ap, ALU.mult, t_a.k() + t_kk.k(), t_a.k())
                    yield

                def prep2(hp):
                    va = v_all.ap[:, hp, :]; vk = v_all.ks(hp)
                    bk = bank()
                    MM(psum[:, bk, :], onesbd.ap, t_sq.ap, True, True, onesbd.k() + t_sq.k(), PK(bk))
                    TT("dve", t_bon.ap, psum[:, bk, :], va, ALU.mult, PK(bk) + vk, t_bon.k())
                    CP("dve", pC.ap, c3(t_pin)[:, :, 63], t_pin.k(), pC.k())
                    STT("dve", arv[:, :, 0, :], c3(t_kk), -1.0, c3(t_cum), ALU.mult, ALU.mult, t_kk.k() + t_cum.k(), ar.k())
                    TT("pool", arv[:, :, 1, :], c3(t_r), c3(t_pin), ALU.mult, t_r.k() + t_pin.k(), ar.k())
                    TT("dve", bt.ap, t_a.ap, t_inv.ap, ALU.mult, t_a.k() + t_inv.k(), bt.k())
                    TT("pool", kt.ap, t_km.ap, t_inv.ap, ALU.mult, t_km.k() + t_inv.k(), kt.k())

                def scan_gen(hp):
                    va = v_all.ap[:, hp, :]; vk = v_all.ks(hp)
                    atv4 = lambda at: at.ap.rearrange("p (h m) x -> p h m x", h=2)

                    def stage1(c, sl):
                        cs = slice(c * 64, (c + 1) * 64)
                        tk_ = tokT[sl]; at = AT[sl]; x0t = X0[sl]; g0 = G0[sl]
                        bk = bank()
                        TR(psum[0:64, bk, 0:128], va[:, cs], ident.ap, vk + KI, PK(bk))
                        TR(psum[0:64, bk, 128:256], bt.ap[:, cs], ident.ap, bt.k() + KI, PK(bk))
                        TR(psum[0:64, bk, 256:384], kt.ap[:, cs], ident.ap, kt.k() + KI, PK(bk))
                        CP("dve", tk_.ap, psum[0:64, bk, 0:384].rearrange("p (a b) -> p a b", b=128), PK(bk), tk_.k())
                        bk = bank2()
                        for h2 in range(2):
                            rows = slice(h2 * 64, h2 * 64 + 64)
                            rhs = arv[rows, c, :, :].rearrange("p a b -> p (a b)")
                            MM(psum[0:64, bk + h2, 0:128], bt.ap[rows, cs], rhs, True, True, bt.k() + ar.k(), PK(bk + h2))
                            MM(psum[0:64, bk + h2, 128:256], kt.ap[rows, cs], rhs, True, True, kt.k() + ar.k(), PK(bk + h2))
                        TT("dve", at.ap.rearrange("p (h m) x -> p h (m x)", h=2), psum[0:64, bk:bk + 2, 0:256],
                           maskU.ap.rearrange("p (h m) x -> p h (m x)", h=2), ALU.mult, PK(bk) + PK(bk + 1) + maskU.k(), at.k())
                        yield
                        bk = bank2()
                        for h2 in range(2):
                            rows = slice(h2 * 64, h2 * 64 + 64)
                            MM(psum[0:64, bk + h2, 0:64], arv[rows, c, 0, :], bt.ap[rows, cs], True, True, bt.k() + ar.k(), PK(bk + h2))
                        TT("dve", x0t.ap, psum[0:64, bk:bk + 2, 0:64], maskL.ap, ALU.mult, PK(bk) + PK(bk + 1) + maskL.k(), x0t.k())
                        bk = bank()
                        for h2 in range(2):
                            MM(psum[0:64, bk, h2 * 64:(h2 + 1) * 64], at.ap[:, h2 * 2 + 1, 0:64], tk_.ap[:, 0, h2 * 64:(h2 + 1) * 64], True, True, at.k() + tk_.k(), PK(bk))
                        CP("dve", g0.ap, psum[0:64, bk, 0:128].rearrange("p (a b) -> p a b", b=64), PK(bk), g0.k())
                        tt_cur = Tt[sl][0]
                        TT("pool", tt_cur.ap, atv4(at)[:, :, 0, 0:64], identT.ap, ALU.add, at.k() + identT.k(), tt_cur.k())
                        yield
                        Xc = x0t.ap; Xtc = atv4(at)[:, :, 0, 0:64]
                        xck = x0t.k(); xtk = at.k()
                        for lvl in range(1, 6):
                            xn = Xn[sl][lvl % 2]
                            bk = bank()
                            for h2 in range(2):
                                MM(psum[0:64, bk, h2 * 64:(h2 + 1) * 64], Xtc[:, h2, :], Xc[:, h2, :], True, True, xck + xtk, PK(bk))
                                if lvl < 5:
                                    MM(psum[0:64, bk, 128 + h2 * 64:128 + (h2 + 1) * 64], Xc[:, h2, :], Xtc[:, h2, :], True, True, xck + xtk, PK(bk))
                            ncol = 256 if lvl < 5 else 128
                            CP("dve", xn.ap.rearrange("p a b -> p (a b)")[:, 0:ncol], psum[0:64, bk, 0:ncol], PK(bk), xn.k())
                            yield
                            Xc = xn.ap[:, 0:2, :]; Xtc = xn.ap[:, 2:4, :]; xck = xn.k(); xtk = xn.k()
                            bk = bank()
                            for h2 in range(2):
                                MM(psum[0:64, bk, h2 * 64:(h2 + 1) * 64], Xc[:, h2, :], tt_cur.ap[:, h2, :], True, True, xck + tt_cur.k(), PK(bk))
                            tt_new = Tt[sl][lvl % 2]
                            TT("dve", tt_new.ap, psum[0:64, bk, 0:128].rearrange("p (a b) -> p a b", b=64), tt_cur.ap, ALU.add, PK(bk) + tt_cur.k(), tt_new.k())
                            tt_cur = tt_new
                            yield

                    def stage2(c, sl):
                        tk_ = tokT[sl]; at = AT[sl]; g0 = G0[sl]; tt_cur = Tt[sl][1]
                        sk = Sst.ks(hp)
                        bk = bank2()
                        for h2 in range(2):
                            rows = slice(h2 * 64, h2 * 64 + 64)
                            MM(psum[0:64, bk + h2, 0:64], arv[rows, c, 0, :], Sst.ap[rows, hp, :], True, True, ar.k() + sk, PK(bk + h2))
                        TT("dve", Wsb.ap, psum[0:64, bk:bk + 2, 0:64], g0.ap, ALU.add, PK(bk) + PK(bk + 1) + g0.k(), Wsb.k())
                        yield
                        bk = bank()
                        for h2 in range(2):
                            MM(psum[0:64, bk, h2 * 64:(h2 + 1) * 64], tt_cur.ap[:, h2, :], Wsb.ap[:, h2, :], True, True, tt_cur.k() + Wsb.k(), PK(bk))
                        CP("dve", Usb.ap, psum[0:64, bk, 0:128].rearrange("p (a b) -> p a b", b=64), PK(bk), Usb.k())
                        TS("pool", Stmp.ap, Sst.ap[:, hp, :], pC.ap[:, c:c + 1], ALU.mult, sk + pC.k(), Stmp.k())
                        yield
                        for h2 in range(2):
                            rows = slice(h2 * 64, h2 * 64 + 64)
                            yo = psum[rows, 6, c * 64:(c + 1) * 64]
                            if h2 == 0:
                                MM(yo, Sst.ap[rows, hp, :], arv[rows, c, 1, :], True, False, sk + ar.k(), PK(6))
                            else:
                                MM(psum[rows, 7, c * 64:(c + 1) * 64], Sst.ap[rows, hp, :], arv[rows, c, 1, :], True, True, sk + ar.k(), PK(7))
                            MM(yo, Usb.ap[:, h2, :], at.ap[:, h2 * 2, 64:128], h2 == 1, False, Usb.k() + at.k(), PK(6))
                            MM(yo, tk_.ap[:, 0, h2 * 64:(h2 + 1) * 64], at.ap[:, h2 * 2 + 1, 64:128], False, True, tk_.k() + at.k(), PK(6))
                        bk = bank()
                        for h2 in range(2):
                            rows = slice(h2 * 64, h2 * 64 + 64)
                            so = psum[rows, bk, 0:64]
                            MM(so, tk_.ap[:, 1, h2 * 64:(h2 + 1) * 64], Usb.ap[:, h2, :], True, False, tk_.k() + Usb.k(), PK(bk))
                            MM(so, tk_.ap[:, 2, h2 * 64:(h2 + 1) * 64], tk_.ap[:, 0, h2 * 64:(h2 + 1) * 64], False, True, tk_.k(), PK(bk))
                        STT("dve", Sst.ap[:, hp, :], psum[:, bk, 0:64], pC.ap[:, c:c + 1], Stmp.ap, ALU.mult, ALU.add, PK(bk) + pC.k() + Stmp.k(), sk)
                        yield


                    s1_next = 0; s1_active = []; s1_done = set(); s2_c = 0; s2_gen = None; s2_done = 0
                    while s2_done < 8:
                        while len(s1_active) < 3 and s1_next < 8 and s1_next - s2_done < NSL:
                            s1_active.append((s1_next, stage1(s1_next, s1_next % NSL)))
                            s1_next += 1
                        for item in list(s1_active):
                            cc_, g_ = item
                            try:
                                next(g_)
                            except StopIteration:
                                s1_active.remove(item)
                                s1_done.add(cc_)
                        if s2_gen is None and s2_c < 8 and s2_c in s1_done:
                            s2_gen = stage2(s2_c, s2_c % NSL)
                        if s2_gen is not None:
                            try:
                                next(s2_gen)
                            except StopIteration:
                                s2_gen = None
                                s2_c += 1
                                s2_done += 1
                        yield

                def finalize(hp):
                    CP("dve", yT.ap, psum[:, 6, :], PK(6), yT.k())
                    TT("dve", yT.ap[64:128, :], psum[64:128, 7, :], yT.ap[64:128, :], ALU.add, PK(7) + yT.k(), yT.k())
                    bk = bank()
                    MM(psum[:, bk, :], onesbd.ap, yT.ap, True, True, onesbd.k() + yT.k(), PK(bk))
                    STT("dve", yT.ap, psum[:, bk, :], -1.0 / 64, yT.ap, ALU.mult, ALU.add, PK(bk) + yT.k(), yT.k())
                    TT("pool", gnt.ap, yT.ap, yT.ap, ALU.mult, yT.k(), gnt.k())
                    bk = bank()
                    MM(psum[:, bk, :], onesbd.ap, gnt.ap, True, True, onesbd.k() + gnt.k(), PK(bk))
                    TS("dve", gnt.ap, psum[:, bk, :], 1.0 / 64, ALU.mult, PK(bk), gnt.k(), s2=GN_EPS, op1=ALU.add)
                    ACT(gnt.ap, gnt.ap, AF.Sqrt, gnt.k(), gnt.k())
                    RECIP(gnt.ap, gnt.ap, gnt.k(), gnt.k())
                    TT("dve", yT.ap, yT.ap, gnt.ap, ALU.mult, yT.k() + gnt.k(), yT.k())
                    TS("dve", yT.ap, yT.ap, pp("gn_g", hp), ALU.mult, yT.k() + prm.k(), yT.k(), s2=pp("gn_b", hp), op1=ALU.add)
                    TT("pool", yT.ap, yT.ap, t_bon.ap, ALU.add, yT.k() + t_bon.k(), yT.k())
                    bk = bank()
                    MM(psum[:, bk, :], lora.ap[:, 1, hp * 128:(hp + 1) * 128], tgd1.ap, True, False, lora.ks(1) + tgd1.k(), PK(bk))
                    MM(psum[:, bk, :], lora.ap[0:32, 2, hp * 128:(hp + 1) * 128], tgd2.ap, False, True, lora.ks(2) + tgd2.k(), PK(bk))
                    TT("dve", yBT.ap[:, hp, :], psum[:, bk, :], yT.ap, ALU.mult, PK(bk) + yT.k(), yBT.ks(hp))

                for _ in prep1(0):
                    pass
                prep2(0)
                for hp in range(4):
                    gens = [scan_gen(hp)]
                    if hp + 1 < 4:
                        gens.append(prep1(hp + 1))
                    interleave(gens, 2)
                    finalize(hp)
                    if hp + 1 < 4:
                        prep2(hp + 1)

                for half in range(2):
                    t = wbuf()
                    mhv = t.ap.rearrange("p a b -> p (a b)").rearrange("p (w k n) -> p w k n", w=2, k=4)
                    cast_load(mhv[:, 0], w_a_h[l][:, half * 512:(half + 1) * 512].rearrange("(k p) n -> p k n", p=128), t.k())
                    cast_load(mhv[:, 1], w_b_h[l][:, half * 512:(half + 1) * 512].rearrange("(k p) n -> p k n", p=128), t.k())
                    mhk = t.k()
                    wga, wgak = wload(wv[:, :, 3360 + half * 512:3360 + (half + 1) * 512], [KC, 512])
                    wgb, wgbk = wload(wv[:, :, 4384 + half * 512:4384 + (half + 1) * 512], [KC, 512])
                    for cc in range(4):
                        c = half * 4 + cc
                        bka = bank()
                        for k in range(4):
                            MM(psum[:, bka, :], mhv[:, 0, k, cc * 128:(cc + 1) * 128], yAT.ap[:, k, :], k == 0, k == 3, mhk + yAT.ks(k), PK(bka))
                        bkg = proj_fm(wga, wgak, cc * 128)
                        ACT(sga.ap, psum[:, bkg, :], AF.Sigmoid, PK(bkg), sga.k())
                        TT("dve", m1.ap, psum[:, bka, :], sga.ap, ALU.mult, PK(bka) + sga.k(), m1.k())
                        bkb = bank()
                        for k in range(4):
                            MM(psum[:, bkb, :], mhv[:, 1, k, cc * 128:(cc + 1) * 128], yBT.ap[:, k, :], k == 0, k == 3, mhk + yBT.ks(k), PK(bkb))
                        bkg = proj_fm(wgb, wgbk, cc * 128)
                        ACT(sgb.ap, psum[:, bkg, :], AF.Sigmoid, PK(bkg), sgb.k())
                        TT("dve", m2.ap, psum[:, bkb, :], sgb.ap, ALU.mult, PK(bkb) + sgb.k(), m2.k())
                        TT("pool", zT.ap[:, c, :], m1.ap, m2.ap, ALU.add, m1.k() + m2.k(), zT.ks(c))
                wo = []
                for kh in range(2):
                    wo.append(wload(w_out_h[l][kh * 512:(kh + 1) * 512, :].rearrange("(k p) n -> p k n", p=128), [4, 1024]))
                def ln1_tile(tt):
                    ti = b * 4 + tt
                    hbuf = hbufs[tt % 2]; x0 = x0s[tt % 2]; x1fm = x1fms[tt % 2]
                    st6 = st6s[tt % 2]; mv4 = mv4s[tt % 2]; lg = lgs[tt % 2]; rt = rts[tt % 2]
                    DMA("sp", x0.ap, xs_d[ti * 128:(ti + 1) * 128, :], [("xs", ti)], x0.k())
                    for nh in range(2):
                        bk = bank()
                        for k in range(KC):
                            wvv, wkk = wo[k // 4]
                            MM(psum[:, bk, :], zT.ap[:, k, tt * 128:(tt + 1) * 128], wvv[:, k % 4, nh * 512:(nh + 1) * 512], k == 0, k == KC - 1,
                               zT.ks(k) + wkk, PK(bk))
                        STT("dve", hbuf.ap[:, nh * 512:(nh + 1) * 512], x0.ap[:, nh * 512:(nh + 1) * 512], ALPHA, psum[:, bk, :], ALU.mult, ALU.add,
                            x0.k() + PK(bk), hbuf.k())
                    yield
                    yield from layer_norm_g(hbuf.ap, hbuf.k(), hbuf.ap, hbuf.k(), st6, mv4)
                    DMA("sp", xs_d[ti * 128:(ti + 1) * 128, :], hbuf.ap, hbuf.k(), [("xs", ti)])
                    to_feature_major(hbuf.ap, hbuf.k(), ti * 128, f32_out=x1fm)
                    yield
                    yield from router_g(l, ti, x1fm, lg, rt)

                interleave([ln1_tile(tt) for tt in range(4)], 2)

        def router_g(l, ti, x1fm, lg, rt):
            bk = bank()
            for k in range(KC):
                MM(psum[:, bk, 0:36], x1fm.ap[:, k, :], rw.ap[:, k, :], k == 0, k == KC - 1, x1fm.ks(k) + rw.k(), PK(bk))
            TT("dve", lg.ap, psum[:, bk, 0:36], rb.ap, ALU.add, PK(bk) + rb.k(), lg.k())
            K = lg.k() + rt.k()
            r = rt.ap
            g4 = lg.ap[:, 0:4]
            P.add("dve", lambda e: e.reduce_max(out=r[:, 0:1], in_=g4, axis=mybir.AxisListType.X), K, K)
            TS("dve", r[:, 4:8], g4, r[:, 0:1], ALU.subtract, K, K)
            ACT(r[:, 4:8], r[:, 4:8], AF.Exp, K, K)
            P.add("dve", lambda e: e.reduce_sum(out=r[:, 1:2], in_=r[:, 4:8], axis=mybir.AxisListType.X), K, K)
            RECIP(r[:, 1:2], r[:, 1:2], K, K)
            yield
            TS("dve", r[:, 8:12], g4, r[:, 0:1], ALU.is_ge, K, K)
            ev = lg.ap[:, 4:36].rearrange("p (g e) -> p g e", e=8)
            TT("dve", r[:, 32:64].rearrange("p (g e) -> p g e", e=8), ev, r[:, 8:12].rearrange("p (g o) -> p g o", o=1).to_broadcast([128, 4, 8]),
               ALU.mult, K, K)
            P.add("dve", lambda e: e.tensor_reduce(out=r[:, 12:20], in_=r[:, 32:64].rearrange("p (g e) -> p e g", e=8), axis=mybir.AxisListType.X,
                                                   op=ALU.add), K, K)
            sel = r[:, 12:20]
            P.add("dve", lambda e: e.reduce_max(out=r[:, 2:3], in_=sel, axis=mybir.AxisListType.X), K, K)
            TS("dve", r[:, 20:28], sel, r[:, 2:3], ALU.is_ge, K, K)
            STT("dve", r[:, 32:40], r[:, 20:28], -1e30, sel, ALU.mult, ALU.add, K, K)
            P.add("dve", lambda e: e.reduce_max(out=r[:, 3:4], in_=r[:, 32:40], axis=mybir.AxisListType.X), K, K)
            TS("dve", r[:, 40:48], sel, r[:, 3:4], ALU.is_ge, K, K)
            yield
            TT("dve", r[:, 48:49], r[:, 3:4], r[:, 2:3], ALU.subtract, K, K)
            ACT(r[:, 48:49], r[:, 48:49], AF.Exp, K, K)
            TS("dve", r[:, 48:49], r[:, 48:49], 1.0, ALU.add, K, K)
            RECIP(r[:, 49:50], r[:, 48:49], K, K)
            yield
            TS("dve", r[:, 50:51], r[:, 49:50], -1.0, ALU.mult, K, K, s2=1.0, op1=ALU.add)
            TT("dve", r[:, 49:51], r[:, 49:51], r[:, 1:2].to_broadcast([128, 2]), ALU.mult, K, K)
            TT("dve", r[:, 40:48], r[:, 40:48], r[:, 20:28], ALU.subtract, K, K)
            TS("dve", r[:, 40:48], r[:, 40:48], r[:, 50:51], ALU.mult, K, K)
            STT("dve", r[:, 40:48], r[:, 20:28], r[:, 49:50], r[:, 40:48], ALU.mult, ALU.add, K, K)
            cv = comb.ap[:, ti, :].rearrange("p (g e) -> p g e", e=8)
            TT("dve", cv, r[:, 8:12].rearrange("p (g o) -> p g o", o=1).to_broadcast([128, 4, 8]),
               r[:, 40:48].rearrange("p (o e) -> p o e", o=1).to_broadcast([128, 4, 8]), ALU.mult, K, comb.ks(ti))
            yield

        def phaseB(s, l, last):
            A.cur = mark_common
            acc = A.alloc([NT, D])
            xT = A.alloc([KC, S], BF16)
            for c in range(KC):
                DMA("sp", xT.ap[:, c, :], xT_d[c], [("xTd", i) for i in range(NT)], xT.ks(c))
            hT = [A.alloc([2, 512], BF16) for _ in range(2)]
            sil = [[A.alloc([512], BF16) for _ in range(2)] for _ in range(2)]
            x1s = [A.alloc([D]) for _ in range(2)]; st6s = [A.alloc([2, 6]) for _ in range(2)]; mv4s = [A.alloc([4]) for _ in range(2)]
            pts = [A.alloc([256]) for _ in range(2)]; pTs = [A.alloc([2, 128], BF16) for _ in range(2)]
            gsigs = [A.alloc([D]) for _ in range(2)]; lnp3 = A.alloc([2, D])
            assert Sst.off + Sst.n - lora.off >= 2048
            stgB = [Tile(A.ap, lora.off + i * 1024, 1024, F32, [1024]) for i in range(2)]
            sbc = [0]

            def stage_cast(dst, src, dkeys):
                a, b_ = src.shape[1], src.shape[2]
                step = max(1, 1024 // b_)
                for a0 in range(0, a, step):
                    a1 = min(a, a0 + step)
                    i = sbc[0]; sbc[0] += 1
                    st_ = stgB[i % 2]
                    sv = st_.ap[:, 0:(a1 - a0) * b_].rearrange("p (a b) -> p a b", b=b_)
                    DMA("sp", sv, src[:, a0:a1, :], (), st_.k())
                    CP("pool", dst[:, a0:a1, :], sv, st_.k(), dkeys)
            print("phaseB arena words", A.cur, "of", A.nwords)
            def load_expert(e):
                t = wbuf()
                gu = t.ap
                stage_cast(gu[:, :, 0:256], exp_gate[l, e].rearrange("(k p) n -> p k n", p=128), t.k())
                stage_cast(gu[:, :, 256:512], exp_up[l, e].rearrange("(k p) n -> p k n", p=128), t.k())
                t2 = wbuf()
                wd = t2.ap.rearrange("p a b -> p (a b)")[:, 0:2048].rearrange("p (k n) -> p k n", k=2)
                stage_cast(wd, exp_down[l, e].rearrange("(k p) n -> p k n", p=128), t2.k())
                return gu, t.k(), wd, t2.k()

            bctr = [0]

            def bankB():
                b_ = bctr[0] % 8
                bctr[0] += 1
                return b_

            def GU(i, e, b, w):
                gu, guk, wd, wdk = w
                t0 = b * 512
                xk = []
                for c in range(KC):
                    xk += xT.k(c * S + t0, c * S + t0 + 512)
                ht = hT[i % 2]
                for dc in range(2):
                    bg = bankB()
                    for k in range(KC):
                        MM(psum[:, bg, :], gu[:, k, dc * 128:(dc + 1) * 128], xT.ap[:, k, t0:t0 + 512], k == 0, k == KC - 1, guk + xk, PK(bg))
                    bu = bankB()
                    for k in range(KC):
                        MM(psum[:, bu, :], gu[:, k, 256 + dc * 128:256 + (dc + 1) * 128], xT.ap[:, k, t0:t0 + 512], k == 0, k == KC - 1, guk + xk, PK(bu))
                    sl = sil[i % 2][dc]
                    ACT(sl.ap, psum[:, bg, :], AF.Silu, PK(bg), sl.k())
                    TT("dve", ht.ap[:, dc, :], psum[:, bu, :], sl.ap, ALU.mult, PK(bu) + sl.k(), ht.ks(dc))

            def DN(i, e, b, w):
                gu, guk, wd, wdk = w
                ht = hT[i % 2]
                for tt in range(4):
                    ti = b * 4 + tt
                    for nh in range(2):
                        bk = bankB()
                        for k in range(2):
                            MM(psum[:, bk, :], ht.ap[:, k, tt * 128:(tt + 1) * 128], wd[:, k, nh * 512:(nh + 1) * 512], k == 0, k == 1, ht.ks(k) + wdk, PK(bk))
                        av = acc.ap[:, ti, nh * 512:(nh + 1) * 512]
                        ak = acc.k(ti * D + nh * 512, ti * D + (nh + 1) * 512)
                        if e == 0:
                            TS("dve", av, psum[:, bk, :], comb.ap[:, ti, e:e + 1], ALU.mult, PK(bk) + comb.ks(ti), ak)
                        else:
                            STT("dve", av, psum[:, bk, :], comb.ap[:, ti, e:e + 1], av, ALU.mult, ALU.add, PK(bk) + comb.ks(ti) + ak, ak)

            items = [(e, b) for e in range(32) for b in range(NB)]
            wts = {0: load_expert(0), 1: load_expert(1)}
            GU(0, 0, 0, wts[0])
            for i, (e, b) in enumerate(items):
                if i + 1 < len(items):
                    e2, b2 = items[i + 1]
                    GU(i + 1, e2, b2, wts[e2])
                DN(i, e, b, wts[e])
                if b == NB - 1:
                    del wts[e]
                    if e + 2 < 32:
                        wts[e + 2] = load_expert(e + 2)
            wpg = []
            for kh in range(2):
                wpg.append(wload(ple_gate_h[l][kh * 512:(kh + 1) * 512, :].rearrange("(k p) n -> p k n", p=128), [4, 1024]))
            wpp, wppk = wload(ple_proj_h[l].rearrange("(k p) n -> p k n", p=128), [2, 1024])
            load_ln(ln_g[l, 1], ln_b[l, 1])
            load_ln(ln_g[l, 2], ln_b[l, 2], lnp3)
            def ep_tile(ti):
                hk = acc.ks(ti)
                h = acc.ap[:, ti, :]
                x1 = x1s[ti % 2]; st6 = st6s[ti % 2]; mv4 = mv4s[ti % 2]; pt = pts[ti % 2]; pT = pTs[ti % 2]; gsig = gsigs[ti % 2]
                DMA("sp", x1.ap, xs_d[ti * 128:(ti + 1) * 128, :], [("xs", ti)], x1.k())
                DMA("sp", pt.ap, p_d[l, s, ti * 128:(ti + 1) * 128, :], (), pt.k())
                STT("dve", h, x1.ap, ALPHA, h, ALU.mult, ALU.add, x1.k() + hk, hk)
                yield from layer_norm_g(h, hk, h, hk, st6, mv4)
                to_feature_major(h, hk, ti * 128, sb=(xT, S, ti * 128), to_dram=False)
                bk = bank()
                for c in range(2):
                    TR(psum[:, bk, c * 128:(c + 1) * 128], pt.ap[:, c * 128:(c + 1) * 128], ident.ap, pt.k() + KI, PK(bk))
                CP("dve", pT.ap, psum[:, bk, 0:256].rearrange("p (c t) -> p c t", t=128), PK(bk), pT.k())
                yield
                xk = []
                for c in range(KC):
                    xk += xT.k(c * S + ti * 128, c * S + ti * 128 + 128)
                for nh in range(2):
                    bk = bank()
                    for k in range(KC):
                        wvv, wkk = wpg[k // 4]
                        MM(psum[:, bk, :], xT.ap[:, k, ti * 128:(ti + 1) * 128], wvv[:, k % 4, nh * 512:(nh + 1) * 512], k == 0, k == KC - 1, xk + wkk, PK(bk))
                    ACT(gsig.ap[:, nh * 512:(nh + 1) * 512], psum[:, bk, :], AF.Sigmoid, PK(bk), gsig.k())
                    bk = bank()
                    for k in range(2):
                        MM(psum[:, bk, :], pT.ap[:, k, :], wpp[:, k, nh * 512:(nh + 1) * 512], k == 0, k == 1, pT.k() + wppk, PK(bk))
                    TT("dve", gsig.ap[:, nh * 512:(nh + 1) * 512], psum[:, bk, :], gsig.ap[:, nh * 512:(nh + 1) * 512], ALU.mult, PK(bk) + gsig.k(), gsig.k())
                    yield
                STT("dve", h, h, ALPHA, gsig.ap, ALU.mult, ALU.add, hk + gsig.k(), hk)
                yield from layer_norm_g(h, hk, h, hk, st6, mv4, lnp3)
                if last:
                    DMA("sp", y_d[s, ti * 128:(ti + 1) * 128, :], h, hk, [("y", s, ti)])
                else:
                    DMA("sp", xs_d[ti * 128:(ti + 1) * 128, :], h, hk, [("xs", ti)])
                    to_feature_major(h, hk, ti * 128)
                yield

            interleave([ep_tile(ti) for ti in range(NT)], 2)

        for s in range(NSEQ if STOP != "c" else 0):
            A.cur = mark_common
            load_ln(ln_in_g, ln_in_b)
            p0_x = [A.alloc([D]) for _ in range(2)]
            p0_st = A.alloc([2, 6]); p0_mv = A.alloc([4])
            for i in range(NT):
                xt = p0_x[i % 2]
                DMA("sp", xt.ap, x_d[s, i * 128:(i + 1) * 128, :], (), xt.k())
                if BIS != 1:
                    layer_norm(xt.ap, xt.k(), xt.ap, xt.k(), p0_st, p0_mv)
                DMA("sp", xs_d[i * 128:(i + 1) * 128, :], xt.ap, xt.k(), [("xs", i)])
                if BIS not in (1, 2):
                    to_feature_major(xt.ap, xt.k(), i * 128)
            if s == 0:
                precast()
                cast_load(biasT.ap, bias_h.rearrange("h j k q -> k (h j) q"), biasT.k())
            if STOP == "p0":
                break
            for l in range(DEPTH):
                phaseA(s, l)
                if STOP == "A":
                    break
                phaseB(s, l, l == DEPTH - 1)
                if STOP == "B":
                    break
            if STOP:
                break
        P.emit()
    return nc


REL_CLIP = 128
_NC_CACHE = {}


def _bias_table(rel_bias):
    k = np.arange(128)[:, None]
    q = np.arange(128)[None, :]
    tabs = []
    for j in range(5):
        dist = q - k + 128 * (4 - j)
        idx = np.clip(dist, -REL_CLIP, REL_CLIP) + REL_CLIP
        t = rel_bias[:, idx]
        kc = (k // 64) + 2 * (j - 4)
        qc = q // 64
        valid = (kc <= qc) & (kc >= qc - 8)
        t = np.where(valid[None], t, np.float32(-1e30))
        tabs.append(t)
    return np.ascontiguousarray(np.stack(tabs, axis=1).astype(np.float32))


def prep_inputs(inputs, S, NSEQ, ncores):
    f = lambda a: np.ascontiguousarray(np.asarray(a, dtype=np.float32))
    shared = {
        "ln_in_g": f(inputs["ln_in_g"]), "ln_in_b": f(inputs["ln_in_b"]),
        "bias_tab": _bias_table(f(inputs["rel_bias"])),
        "w_in": f(inputs["w_in"]), "tok_mix": f(inputs["tok_mix"]),
        "decay_base": f(inputs["decay_base"]), "decay_up": f(inputs["decay_up"]),
        "aaa_base": f(inputs["aaa_base"]), "aaa_up": f(inputs["aaa_up"]), "gate_up": f(inputs["gate_up"]),
        "k_k": f(inputs["k_k"]), "k_a": f(inputs["k_a"]), "r_k": f(inputs["r_k"]).reshape(-1, 512),
        "vres_base": f(inputs["vres_base"]), "vres_down": f(inputs["vres_down"]), "vres_up": f(inputs["vres_up"]),
        "gn_g": f(inputs["gn_g"]), "gn_b": f(inputs["gn_b"]),
        "w_branch_attn": f(inputs["w_branch_attn"]), "w_branch_rwkv": f(inputs["w_branch_rwkv"]), "w_out": f(inputs["w_out"]),
        "router_w": np.ascontiguousarray(np.concatenate([f(inputs["router_grp"]), f(inputs["router_exp"])], axis=-1)),
        "router_b": np.ascontiguousarray(np.concatenate([f(inputs["router_grp_bias"]), f(inputs["router_exp_bias"])], axis=-1)),
        "exp_gate": f(inputs["exp_gate"]), "exp_up": f(inputs["exp_up"]), "exp_down": f(inputs["exp_down"]),
        "ple_proj": f(inputs["ple_proj"]), "ple_gate": f(inputs["ple_gate"]),
        "ln_g": f(inputs["ln_g"]), "ln_b": f(inputs["ln_b"]),
    }
    x = f(inputs["x"]); p = f(inputs["p"])
    maps = []
    for c in range(ncores):
        m = dict(shared)
        m["x"] = np.ascontiguousarray(x[c * NSEQ:(c + 1) * NSEQ])
        m["p"] = np.ascontiguousarray(p[:, c * NSEQ:(c + 1) * NSEQ])
        maps.append(m)
    return maps


def kernel(**inputs):
    x = np.asarray(inputs["x"])
    B, S, _ = x.shape
    ncores = 8
    NSEQ = B // ncores
    key = (S, NSEQ)
    if key not in _NC_CACHE:
        _NC_CACHE[key] = build_nc(S, NSEQ)
    nc = _NC_CACHE[key]
    maps = prep_inputs(inputs, S, NSEQ, ncores)
    res = run_bass_kernel_spmd(nc, maps, core_ids=list(range(ncores)))
    return np.concatenate([np.asarray(r["y"], dtype=np.float32) for r in res.results], axis=0)
```

```python
import contextlib
import numpy as np
import concourse.bass as bass
import concourse.mybir as mybir
from concourse.bass_utils import run_bass_kernel_spmd

F32 = mybir.dt.float32
BF16 = mybir.dt.bfloat16
AF = mybir.ActivationFunctionType
ALU = mybir.AluOpType

D = 1024
KC = 8
NIN = 5408
ALPHA = 4.0 ** 0.25
LN_EPS = 1e-5
GN_EPS = 64e-5
C0 = float(np.exp(-0.5))
N_DMA_SEMS = 40
SAME_ENGINE_SYNC = True
GRAN = 64
STOP = None
ABLK = None
BIS = 0


class Op:
    __slots__ = ("eng", "fn", "deps", "dma", "idx", "signal", "tick", "dsem", "dval", "prev_dval", "sw", "gen")

    def __init__(self, eng, fn, dma):
        self.eng = eng
        self.fn = fn
        self.dma = dma
        self.deps = set()
        self.signal = False
        self.tick = 0
        self.dsem = None
        self.dval = 0
        self.prev_dval = 0
        self.sw = None
        self.gen = 0


class Prog:
    ENGS = ("pe", "act", "dve", "pool", "sp")

    def __init__(self, nc):
        self.nc = nc
        self.ops = []
        self.res = {}
        self.ndma = 0
        self.swgen = {}

    def add(self, eng, fn, reads=(), writes=(), dma=False, sw=None):
        op = Op(eng, fn, dma)
        op.idx = len(self.ops)
        res = self.res
        psr = [r for r in reads if r[0] == "ps"]
        if psr:
            reads = [r for r in reads if r[0] != "ps"]
            writes = list(writes) + psr
        for r in reads:
            st = res.get(r)
            if st is not None and st[0] is not None:
                op.deps.add(st[0])
        for w in writes:
            st = res.get(w)
            if st is not None:
                if st[0] is not None:
                    op.deps.add(st[0])
                op.deps.update(st[1])
        for r in reads:
            st = res.get(r)
            if st is None:
                res[r] = [None, [op.idx]]
            else:
                st[1].append(op.idx)
        for w in writes:
            res[w] = [op.idx, []]
        op.deps.discard(op.idx)
        if dma and sw is not None:
            op.sw = sw
            op.gen = self.swgen.get(sw, 0)
            self.swgen[sw] = op.gen + 1
        elif dma:
            op.dsem = self.ndma % N_DMA_SEMS
            self.ndma += 1
        self.ops.append(op)
        return op

    def emit(self):
        nc = self.nc
        ops = self.ops
        for op in ops:
            for d in op.deps:
                dop = ops[d]
                if dop.dma:
                    continue
                if dop.eng == op.eng and (dop.eng == "pe" or not SAME_ENGINE_SYNC):
                    continue
                dop.signal = True
        ticks = {e: 0 for e in self.ENGS}
        dcount = [0] * N_DMA_SEMS
        for op in ops:
            if op.dma and op.sw is not None:
                continue
            if op.dma:
                op.prev_dval = dcount[op.dsem]
                dcount[op.dsem] += 16
                op.dval = dcount[op.dsem]
            elif op.signal:
                ticks[op.eng] += 1
                op.tick = ticks[op.eng]
        per_eng = {e: [o for o in ops if o.eng == e] for e in self.ENGS}
        with contextlib.ExitStack() as es:
            esem = {e: es.enter_context(nc.semaphore("s_" + e)) for e in ("pe", "act", "dve", "pool")}
            dsems = [es.enter_context(nc.semaphore("d%d" % i)) for i in range(N_DMA_SEMS)]
            swsems = {}
            for i, slot in enumerate(sorted(self.swgen.keys(), key=str)):
                swsems[slot] = [es.enter_context(nc.semaphore("w%d_%d" % (i, j))) for j in range(2)]
            block = es.enter_context(nc.Block())

            def run(engname, eng):
                waited = {}

                def wait(key, sem, val):
                    if waited.get(key, 0) >= val:
                        return
                    eng.wait_ge(sem, val)
                    waited[key] = val

                for op in per_eng[engname]:
                    for d in sorted(op.deps):
                        dop = ops[d]
                        if dop.dma and dop.sw is not None:
                            wait(("sw", dop.sw, dop.gen), swsems[dop.sw][dop.gen % 2], 16)
                        elif dop.dma:
                            wait(("d", dop.dsem), dsems[dop.dsem], dop.dval)
                        else:
                            if dop.eng == engname and (engname == "pe" or not SAME_ENGINE_SYNC):
                                continue
                            wait(("e", dop.eng), esem[dop.eng], dop.tick)
                    if op.dma and op.sw is not None:
                        if op.gen >= 1:
                            wait(("sw", op.sw, op.gen - 1), swsems[op.sw][(op.gen - 1) % 2], 16)
                            eng.sem_clear(swsems[op.sw][(op.gen - 1) % 2])
                        op.fn(eng).then_inc(swsems[op.sw][op.gen % 2], 16)
                    elif op.dma:
                        if op.prev_dval > 0:
                            wait(("d", op.dsem), dsems[op.dsem], op.prev_dval)
                        op.fn(eng).then_inc(dsems[op.dsem], 16)
                    else:
                        ins = op.fn(eng)
                        if op.signal:
                            ins.then_inc(esem[engname], 1)
                for op in per_eng[engname]:
                    if op.dma and op.sw is None:
                        wait(("d", op.dsem), dsems[op.dsem], op.dval)

            if per_eng["pe"]:
                block.tensor(lambda e: run("pe", e))
            if per_eng["act"]:
                block.scalar(lambda e: run("act", e))
            if per_eng["dve"]:
                block.vector(lambda e: run("dve", e))
            if per_eng["pool"]:
                block.gpsimd(lambda e: run("pool", e))
            if per_eng["sp"]:
                block.sync(lambda e: run("sp", e))


class Tile:
    def __init__(self, arena, off, nwords, dtype, shape, parts=128):
        self.off = off
        self.n = nwords
        self.esz = 4 if dtype == F32 else 2
        ne = int(np.prod(shape))
        nwx = ne if dtype == F32 else (ne + 1) // 2
        v = arena[0:parts, off:off + nwx]
        if dtype != F32:
            v = v.bitcast(dtype)[:, 0:ne]
        self.shape = tuple(shape)
        if len(shape) == 2:
            v = v.rearrange("p (a b) -> p a b", b=shape[1])
        elif len(shape) == 3:
            v = v.rearrange("p (a b c) -> p a b c", b=shape[1], c=shape[2])
        self.ap = v
        self.inner = int(np.prod(shape[1:])) if len(shape) > 1 else 1

    def k(self, lo=None, hi=None):
        if lo is None:
            lo, hi = 0, self.n * 4 // self.esz
        w0 = self.off + (lo * self.esz) // 4
        w1 = self.off + (hi * self.esz + 3) // 4
        return [("sb", g) for g in range(w0 // GRAN, (w1 - 1) // GRAN + 1)]

    def ks(self, i, n=1):
        return self.k(i * self.inner, (i + n) * self.inner)


class Arena:
    def __init__(self, nc, es, nwords):
        self.t = es.enter_context(nc.sbuf_tensor("arena", [128, nwords], F32))
        self.ap = self.t[:]
        self.nwords = nwords
        self.cur = 0

    def alloc(self, shape, dtype=F32, parts=128):
        ne = int(np.prod(shape))
        nw = ne if dtype == F32 else (ne + 1) // 2
        nw = (nw + GRAN - 1) // GRAN * GRAN
        assert self.cur + nw <= self.nwords, ("arena overflow", self.cur, nw, self.nwords)
        t = Tile(self.ap, self.cur, nw, dtype, shape, parts)
        self.cur += nw
        return t


def build_nc(S, NSEQ, DEPTH=2, dbg=None):
    NB = S // 512
    NT = S // 128
    nc = bass.Bass("TRN2", target_bir_lowering=False)
    dram = lambda name, shape, kind="ExternalInput": nc.dram_tensor(name, shape, F32, kind=kind).ap()
    x_d = dram("x", [NSEQ, S, D])
    p_d = dram("p", [DEPTH, NSEQ, S, 256])
    ln_in_g = dram("ln_in_g", [D]); ln_in_b = dram("ln_in_b", [D])
    bias_d = dram("bias_tab", [8, 5, 128, 128])
    w_in = dram("w_in", [DEPTH, D, NIN])
    tok_mix = dram("tok_mix", [DEPTH, 1824])
    decay_base = dram("decay_base", [DEPTH, 512]); decay_up = dram("decay_up", [DEPTH, 64, 512])
    aaa_base = dram("aaa_base", [DEPTH, 512]); aaa_up = dram("aaa_up", [DEPTH, 64, 512])
    gate_up = dram("gate_up", [DEPTH, 160, 512])
    k_k = dram("k_k", [DEPTH, 512]); k_a = dram("k_a", [DEPTH, 512]); r_k = dram("r_k", [DEPTH, 512])
    vres_base = dram("vres_base", [DEPTH - 1, 512]); vres_down = dram("vres_down", [DEPTH - 1, 512, 32])
    vres_up = dram("vres_up", [DEPTH - 1, 32, 512])
    gn_g = dram("gn_g", [DEPTH, 512]); gn_b = dram("gn_b", [DEPTH, 512])
    w_a = dram("w_branch_attn", [DEPTH, 512, D]); w_b = dram("w_branch_rwkv", [DEPTH, 512, D])
    w_out = dram("w_out", [DEPTH, D, D])
    router_w = dram("router_w", [DEPTH, D, 36]); router_b = dram("router_b", [DEPTH, 36])
    exp_gate = dram("exp_gate", [DEPTH, 32, D, 256]); exp_up = dram("exp_up", [DEPTH, 32, D, 256])
    exp_down = dram("exp_down", [DEPTH, 32, 256, D])
    ple_proj = dram("ple_proj", [DEPTH, 256, D]); ple_gate = dram("ple_gate", [DEPTH, D, D])
    ln_g = dram("ln_g", [DEPTH, 3, D]); ln_b = dram("ln_b", [DEPTH, 3, D])
    y_d = dram("y", [NSEQ, S, D], kind="ExternalOutput")
    xs_d = dram("xs_scr", [S, D], kind="Internal")
    dramh = lambda name, shape: nc.dram_tensor(name, shape, BF16, kind="Internal").ap()
    xT_d = dramh("xT_scr", [KC, 128, S])
    w_in_h = dramh("w_in_h", [DEPTH, D, NIN]); w_a_h = dramh("w_a_h", [DEPTH, 512, D]); w_b_h = dramh("w_b_h", [DEPTH, 512, D])
    w_out_h = dramh("w_out_h", [DEPTH, D, D]); exp_gate_h = dramh("exp_gate_h", [DEPTH, 32, D, 256])
    exp_up_h = dramh("exp_up_h", [DEPTH, 32, D, 256]); exp_down_h = dramh("exp_down_h", [DEPTH, 32, 256, D])
    ple_proj_h = dramh("ple_proj_h", [DEPTH, 256, D]); ple_gate_h = dramh("ple_gate_h", [DEPTH, D, D])
    bias_h = dramh("bias_h", [8, 5, 128, 128])
    vf_d = dram("vf_scr", [4, 128, S], kind="Internal")
    dbg_d = {}
    if dbg:
        for name, shape in dbg.items():
            dbg_d[name] = dram("dbg_" + name, shape, kind="ExternalOutput")

    P = Prog(nc)
    es = contextlib.ExitStack()
    with es:
        A = Arena(nc, es, 53000)
        psum = es.enter_context(nc.psum_tensor("ps", [128, 8, 512], F32))
        bank_ctr = [0]

        def bank():
            b = bank_ctr[0] % 6
            bank_ctr[0] += 1
            return b

        def bank2():
            b = bank_ctr[0] % 6
            if b % 2:
                bank_ctr[0] += 1
                b = bank_ctr[0] % 6
            bank_ctr[0] += 2
            return b

        PK = lambda b: [("ps", b)]

        def MM(out, lhsT, rhs, start, stop, reads, writes):
            P.add("pe", lambda e: e.matmul(out, lhsT=lhsT, rhs=rhs, start=start, stop=stop), reads, writes)

        def TR(out, in_, ident, reads, writes):
            P.add("pe", lambda e: e.transpose(out, in_, ident), reads, writes)

        def ACT(out, in_, func, reads, writes, bias=None, scale=None):
            kw = {}
            if bias is not None:
                kw["bias"] = bias
            if scale is not None:
                kw["scale"] = scale
            P.add("act", lambda e: e.activation(out=out, in_=in_, func=func, **kw), reads, writes)

        def TT(eng, out, in0, in1, op, reads, writes):
            P.add(eng, lambda e: e.tensor_tensor(out=out, in0=in0, in1=in1, op=op), reads, writes)

        def TS(eng, out, in0, s1, op0, reads, writes, s2=None, op1=None):
            if op1 is None:
                P.add(eng, lambda e: e.tensor_scalar(out=out, in0=in0, scalar1=s1, scalar2=None, op0=op0), reads, writes)
            else:
                P.add(eng, lambda e: e.tensor_scalar(out=out, in0=in0, scalar1=s1, scalar2=s2, op0=op0, op1=op1), reads, writes)

        def STT(eng, out, in0, scalar, in1, op0, op1, reads, writes):
            P.add(eng, lambda e: e.scalar_tensor_tensor(out=out, in0=in0, scalar=scalar, in1=in1, op0=op0, op1=op1), reads, writes)

        def CP(eng, out, in_, reads, writes):
            if eng == "act":
                P.add("dve", lambda e: e.tensor_copy(out=out, in_=in_), reads, writes)
            else:
                P.add(eng, lambda e: e.tensor_copy(out=out, in_=in_), reads, writes)

        def RECIP(out, in_, reads, writes):
            P.add("dve", lambda e: e.reciprocal(out=out, in_=in_), reads, writes)

        def MEMSET(eng, ap, val, writes):
            P.add(eng, lambda e: e.memset(ap, val), (), writes)

        def DMA(eng, out, in_, reads, writes, sw=None, **kw):
            P.add(eng, lambda e: e.dma_start(out=out, in_=in_, **kw), reads, writes, dma=True, sw=sw)

        ident = A.alloc([128]); identb = A.alloc([128], BF16)
        onesbd = A.alloc([128])
        maskU = A.alloc([4, 128], parts=64)
        maskL = A.alloc([2, 64], parts=64)
        identT = A.alloc([2, 64], parts=64)
        rmask = A.alloc([512])
        biasT = A.alloc([40, 128], BF16)
        KI = ident.k(); KIB = identb.k()
        MEMSET("pool", ident.ap, 0.0, KI)
        P.add("pool", lambda e: e.affine_select(out=ident.ap, in_=ident.ap, pattern=[[-1, 128]], compare_op=ALU.not_equal,
                                                fill=1.0, base=0, channel_multiplier=1), KI, KI)
        CP("pool", identb.ap, ident.ap, KI, KIB)
        MEMSET("pool", onesbd.ap, 0.0, onesbd.k())
        MEMSET("pool", onesbd.ap[0:64, 0:64], 1.0, onesbd.k())
        MEMSET("pool", onesbd.ap[64:128, 64:128], 1.0, onesbd.k())
        MEMSET("pool", maskU.ap, 1.0, maskU.k())
        for q in range(4):
            P.add("pool", (lambda q: lambda e: e.affine_select(out=maskU.ap[:, q, 0:64], in_=maskU.ap[:, q, 0:64], pattern=[[1, 64]],
                  compare_op=ALU.is_ge, fill=0.0, base=-1, channel_multiplier=-1))(q), maskU.k(), maskU.k())
            P.add("pool", (lambda q: lambda e: e.affine_select(out=maskU.ap[:, q, 64:128], in_=maskU.ap[:, q, 64:128], pattern=[[1, 64]],
                  compare_op=ALU.is_ge, fill=0.0, base=0, channel_multiplier=-1))(q), maskU.k(), maskU.k())
        MEMSET("pool", maskL.ap, 1.0, maskL.k())
        for q in range(2):
            P.add("pool", (lambda q: lambda e: e.affine_select(out=maskL.ap[:, q, :], in_=maskL.ap[:, q, :], pattern=[[-1, 64]],
                  compare_op=ALU.is_ge, fill=0.0, base=-1, channel_multiplier=1))(q), maskL.k(), maskL.k())
        CP("pool", identT.ap[:, 0, :], ident.ap[0:64, 0:64], KI, identT.k())
        CP("pool", identT.ap[:, 1, :], ident.ap[0:64, 0:64], KI, identT.k())
        MEMSET("pool", rmask.ap, 1.0, rmask.k())
        MEMSET("pool", rmask.ap.rearrange("p (c t) -> p c t", t=64)[:, :, 0:1], 0.0, rmask.k())

        xstg = [A.alloc([KC, 128], BF16) for _ in range(2)]
        xsc = [0]
        NWB = 4
        wpool = [A.alloc([KC, 512], BF16) for _ in range(NWB)]
        wctr = [0]

        def wbuf():
            t = wpool[wctr[0] % NWB]
            t.idx = wctr[0] % NWB
            wctr[0] += 1
            return t

        def cast_load(dst, src, dkeys):
            DMA("sp", dst, src, ["wh_all"], dkeys)

        lnp = A.alloc([2, D])
        comb = A.alloc([NT, 32])
        prm = A.alloc([16, 4])
        PRM = {n: i for i, n in enumerate(["decay_base", "aaa_base", "k_k", "k_a", "r_k", "gn_g", "gn_b", "vres_base"])}
        mu = A.alloc([2, 16])
        lora = A.alloc([3, 512])
        vrd = A.alloc([4, 32]); vru = A.alloc([512], parts=32)
        rw = A.alloc([KC, 36]); rb = A.alloc([36])
        lastc = A.alloc([16])
        Sst = A.alloc([4, 64])

        mark_common = A.cur

        def precast():
            stg = [A.alloc([4096]) for _ in range(4)]
            outb = [A.alloc([4096], BF16) for _ in range(4)]
            cnt = [0]
            allk = []

            def chunk(src3, dst3):
                a, b_ = src3.shape[1], src3.shape[2]
                i = cnt[0]; cnt[0] += 1
                st_ = stg[i % 4]; ob = outb[i % 4]
                sv = st_.ap[:, 0:a * b_].rearrange("p (a b) -> p a b", b=b_)
                ov = ob.ap[:, 0:a * b_].rearrange("p (a b) -> p a b", b=b_)
                DMA("sp", sv, src3, (), st_.k())
                CP("dve", ov, sv, st_.k(), ob.k())
                key = ("whc", i)
                DMA("act", dst3, ov, ob.k(), [key])
                allk.append(key)

            def cast2d(src2, dst2):
                R, C = src2.shape
                nrb = R // 128
                if C <= 4096:
                    Astep = max(1, 4096 // C)
                    sv = src2.rearrange("(a p) n -> p a n", p=128)
                    dv = dst2.rearrange("(a p) n -> p a n", p=128)
                    for a0 in range(0, nrb, Astep):
                        a1 = min(nrb, a0 + Astep)
                        chunk(sv[:, a0:a1, :], dv[:, a0:a1, :])
                else:
                    sv = src2.rearrange("(a p) n -> p a n", p=128)
                    dv = dst2.rearrange("(a p) n -> p a n", p=128)
                    for a0 in range(nrb):
                        for c0 in range(0, C, 4096):
                            c1 = min(C, c0 + 4096)
                            chunk(sv[:, a0:a0 + 1, c0:c1], dv[:, a0:a0 + 1, c0:c1])

            cast2d(w_in.rearrange("l k n -> (l k) n"), w_in_h.rearrange("l k n -> (l k) n"))
            cast2d(w_a.rearrange("l k n -> (l k) n"), w_a_h.rearrange("l k n -> (l k) n"))
            cast2d(w_b.rearrange("l k n -> (l k) n"), w_b_h.rearrange("l k n -> (l k) n"))
            cast2d(w_out.rearrange("l k n -> (l k) n"), w_out_h.rearrange("l k n -> (l k) n"))
            cast2d(ple_proj.rearrange("l k n -> (l k) n"), ple_proj_h.rearrange("l k n -> (l k) n"))
            cast2d(ple_gate.rearrange("l k n -> (l k) n"), ple_gate_h.rearrange("l k n -> (l k) n"))
            cast2d(bias_d.rearrange("h j k q -> (h j k) q"), bias_h.rearrange("h j k q -> (h j k) q"))
            DMA("sp", stg[0].ap[0:1, 0:8], ln_in_g[0:8].rearrange("(o n) -> o n", o=1), allk, stg[0].k() + ["wh_all"])
            A.cur = mark_common


        def load_ln(gsrc, bsrc, lp=None):
            lp = lp or lnp
            DMA("sp", lp.ap[:, 0, :], gsrc.partition_broadcast(128), (), lp.ks(0))
            DMA("sp", lp.ap[:, 1, :], bsrc.partition_broadcast(128), (), lp.ks(1))

        def layer_norm_g(h, hk, out_ap, out_keys, st, mv, lp=None):
            lp = lp or lnp
            for c in range(2):
                P.add("dve", (lambda c: lambda e: e.bn_stats(out=st.ap[:, c, :], in_=h[:, c * 512:(c + 1) * 512]))(c), hk, st.k())
            P.add("dve", lambda e: e.bn_aggr(out=mv.ap[:, 0:2], in_=st.ap), st.k(), mv.k())
            TS("dve", mv.ap[:, 2:3], mv.ap[:, 1:2], LN_EPS, ALU.add, mv.k(), mv.k())
            yield
            ACT(mv.ap[:, 2:3], mv.ap[:, 2:3], AF.Sqrt, mv.k(), mv.k())
            yield
            RECIP(mv.ap[:, 2:3], mv.ap[:, 2:3], mv.k(), mv.k())
            TS("dve", h, h, mv.ap[:, 0:1], ALU.subtract, hk + mv.k(), hk, s2=mv.ap[:, 2:3], op1=ALU.mult)
            yield
            TT("dve", h, h, lp.ap[:, 0, :], ALU.mult, hk + lp.ks(0), hk)
            yield
            TT("dve", out_ap, h, lp.ap[:, 1, :], ALU.add, hk + lp.ks(1), out_keys)
            yield

        def layer_norm(h, hk, out_ap, out_keys, st, mv, lp=None):
            for _ in layer_norm_g(h, hk, out_ap, out_keys, st, mv, lp):
                pass

        def interleave(gens, width):
            gens = list(gens)
            nxt = 0
            active = []
            while nxt < len(gens) or active:
                while len(active) < width and nxt < len(gens):
                    active.append(gens[nxt])
                    nxt += 1
                for g_ in list(active):
                    try:
                        next(g_)
                    except StopIteration:
                        active.remove(g_)

        def to_feature_major(xt_tok, xk, tok0, f32_out=None, sb=None, to_dram=True):
            if to_dram:
                stg = xstg[xsc[0] % 2]
                xsc[0] += 1
            for half in range(2):
                b = bank()
                for cc in range(4):
                    c = half * 4 + cc
                    TR(psum[:, b, cc * 128:(cc + 1) * 128], xt_tok[:, c * 128:(c + 1) * 128], ident.ap, xk + KI, PK(b))
                src = psum[:, b, :].rearrange("p (c t) -> p c t", t=128)
                if sb is not None:
                    tl, ncols, col0 = sb
                    wk = []
                    for c in range(half * 4, half * 4 + 4):
                        wk += tl.k(c * ncols + col0, c * ncols + col0 + 128)
                    CP("dve", tl.ap[:, half * 4:half * 4 + 4, col0:col0 + 128], src, PK(b), wk)
                if to_dram:
                    CP("dve", stg.ap[:, half * 4:half * 4 + 4, :], src, PK(b), stg.ks(half * 4, 4))
                if f32_out is not None:
                    CP("dve", f32_out.ap[:, half * 4:half * 4 + 4, :], src, PK(b), f32_out.ks(half * 4, 4))
            if to_dram:
                DMA("sp", xT_d[:, :, tok0:tok0 + 128].rearrange("c p t -> p c t"), stg.ap, stg.k(), [("xTd", tok0 // 128)])

        def load_layer_params(l):
            fm = lambda src: src.rearrange("(c p) -> p c", p=128)
            lst = [("decay_base", decay_base[l]), ("aaa_base", aaa_base[l]), ("k_k", k_k[l]), ("k_a", k_a[l]),
                   ("r_k", r_k[l]), ("gn_g", gn_g[l]), ("gn_b", gn_b[l])]
            if l > 0:
                lst.append(("vres_base", vres_base[l - 1]))
            for name, src in lst:
                DMA("sp", prm.ap[:, PRM[name], :], fm(src), (), prm.k(), allow_slow_non_contiguous=True)
            DMA("sp", mu.ap[:, 0, 0:14], tok_mix[l, 0:1792].rearrange("(c p) -> p c", p=128), (), mu.k(), allow_slow_non_contiguous=True)
            DMA("sp", mu.ap[0:32, 0, 14:15], tok_mix[l, 1792:1824].rearrange("(p o) -> p o", o=1), (), mu.k())
            TS("pool", mu.ap[:, 1, 0:14], mu.ap[:, 0, 0:14], -1.0, ALU.mult, mu.k(), mu.k(), s2=1.0, op1=ALU.add)
            TS("pool", mu.ap[0:32, 1, 14:15], mu.ap[0:32, 0, 14:15], -1.0, ALU.mult, mu.k(), mu.k(), s2=1.0, op1=ALU.add)
            DMA("sp", lora.ap[0:64, 0, :], decay_up[l], (), lora.ks(0))
            DMA("sp", lora.ap[64:128, 0, :], aaa_up[l], (), lora.ks(0))
            DMA("sp", lora.ap[:, 1, :], gate_up[l, 0:128, :], (), lora.ks(1))
            DMA("sp", lora.ap[0:32, 2, :], gate_up[l, 128:160, :], (), lora.ks(2))
            if l > 0:
                DMA("sp", vrd.ap, vres_down[l - 1].rearrange("(c p) r -> p c r", p=128), (), vrd.k())
                DMA("sp", vru.ap, vres_up[l - 1], (), vru.k())
            DMA("sp", rw.ap, router_w[l].rearrange("(k p) n -> p k n", p=128), (), rw.k())
            DMA("sp", rb.ap, router_b[l].partition_broadcast(128), (), rb.k())

        def wload(src_ap, shape):
            t = wbuf()
            n = int(np.prod(shape))
            v = t.ap.rearrange("p a b -> p (a b)")[:, 0:n]
            if len(shape) == 2:
                v = v.rearrange("p (a b) -> p a b", b=shape[1])
            cast_load(v, src_ap, t.k())
            return v, t.k()

        def phaseA(s, l):
            A.cur = mark_common
            kT = A.alloc([4, 1024], BF16); vring = A.alloc([8, 8, 65], BF16)
            xTa = A.alloc([KC, 1024], BF16)
            yAT = A.alloc([4, 512], BF16); yBT = A.alloc([4, 512], BF16)
            st6s = [A.alloc([2, 6]) for _ in range(2)]; mv4s = [A.alloc([4]) for _ in range(2)]
            lgs = [A.alloc([36]) for _ in range(2)]; rts = [A.alloc([64]) for _ in range(2)]
            twd = A.alloc([512]); tgd1 = A.alloc([512]); tgd2 = A.alloc([512], parts=32)
            v_all = A.alloc([4, 512]); vd = A.alloc([512], parts=32)
            mark_att = A.cur
            qT = A.alloc([4, 512], BF16)
            PT = [A.alloc([4, 128], BF16) for _ in range(6)]
            ytok = A.alloc([512]); rc = A.alloc([8])
            end_att = A.cur
            A.cur = mark_att
            NSL = 4
            tokT = [A.alloc([3, 128], parts=64) for _ in range(NSL)]
            AT = [A.alloc([4, 128], parts=64) for _ in range(NSL)]
            X0 = [A.alloc([2, 64], parts=64) for _ in range(NSL)]
            Xn = [[A.alloc([4, 64], parts=64) for _ in range(2)] for _ in range(NSL)]
            Tt = [[A.alloc([2, 64], parts=64) for _ in range(2)] for _ in range(NSL)]
            G0 = [A.alloc([2, 64], parts=64) for _ in range(NSL)]
            A.cur = max(A.cur, end_att)
            mark_r = A.cur
            t_r = A.alloc([512]); t_k = A.alloc([512]); t_sg = A.alloc([512]); t_cum = A.alloc([512])
            t_pin = A.alloc([512]); t_inv = A.alloc([512]); t_a = A.alloc([512]); t_kk = A.alloc([512])
            t_sq = A.alloc([512]); t_km = A.alloc([512]); t_bon = A.alloc([512])
            bt = A.alloc([512]); kt = A.alloc([512]); yT = A.alloc([512]); gnt = A.alloc([512])
            ar = A.alloc([8, 2, 64]); pC = A.alloc([8]); t_vf = A.alloc([512])
            Wsb = A.alloc([2, 64], parts=64); Usb = A.alloc([2, 64], parts=64); Stmp = A.alloc([64])
            end_r = A.cur
            A.cur = mark_r
            sga = A.alloc([512]); sgb = A.alloc([512]); m1 = A.alloc([512]); m2 = A.alloc([512])
            zT = A.alloc([8, 512], BF16)
            hbufs = [A.alloc([D]) for _ in range(2)]; x0s = [A.alloc([D]) for _ in range(2)]; x1fms = [A.alloc([KC, 128]) for _ in range(2)]
            A.cur = max(A.cur, end_r)
            print("phaseA arena words", A.cur, "of", A.nwords)

            load_layer_params(l)
            load_ln(ln_g[l, 0], ln_b[l, 0])
            MEMSET("pool", lastc.ap, 0.0, lastc.k())
            MEMSET("pool", Sst.ap, 0.0, Sst.k())
            MEMSET("pool", vring.ap[:, :, :, 64:65], 1.0, vring.k())
            wv = w_in_h[l].rearrange("(k p) n -> p k n", p=128)

            def shift(pc_ap, np_, mc, out_ap, out_keys, b):
                m = mu.ap[0:np_, 0, mc:mc + 1]; om = mu.ap[0:np_, 1, mc:mc + 1]
                TS("dve", out_ap, pc_ap, om, ALU.mult, PK(b) + mu.k(), out_keys)
                STT("dve", out_ap[:, 1:512], pc_ap[:, 0:511], m, out_ap[:, 1:512], ALU.mult, ALU.add, PK(b) + out_keys + mu.k(), out_keys)
                STT("dve", out_ap[:, 0:1], lastc.ap[0:np_, mc:mc + 1], m, out_ap[:, 0:1], ALU.mult, ALU.add, lastc.k() + out_keys + mu.k(), out_keys)
                CP("dve", lastc.ap[0:np_, mc:mc + 1], pc_ap[:, 511:512], PK(b), lastc.k())

            def load_xblock(bb):
                rp_ = (bb % 2) * 512
                wk_ = []
                for c in range(KC):
                    wk_ += xTa.k(c * 1024 + rp_, c * 1024 + rp_ + 512)
                DMA("sp", xTa.ap[:, :, rp_:rp_ + 512], xT_d[:, :, bb * 512:(bb + 1) * 512].rearrange("c p t -> p c t"),
                    [("xTd", bb * 4 + i) for i in range(4)], wk_)

            load_xblock(0)
            for b in range(NB):
                t0 = b * 512
                xr = (b % 2) * 512
                if b + 1 < NB:
                    load_xblock(b + 1)
                xk = []
                for c in range(KC):
                    xk += xTa.k(c * 1024 + xr, c * 1024 + xr + 512)

                def proj_fm(wt, wk, col, ncol=128, kparts=128):
                    bk = bank()
                    for k in range(KC):
                        MM(psum[0:ncol, bk, :], wt[:, k, col:col + ncol], xTa.ap[:, k, xr:xr + 512], k == 0, k == KC - 1, wk + xk, PK(bk))
                    return bk

                wt, wk = wload(wv[:, :, 0:512], [KC, 512])
                for c in range(4):
                    bk = proj_fm(wt, wk, c * 128)
                    TS("dve", qT.ap[:, c, :], psum[:, bk, :], 0.125, ALU.mult, PK(bk), qT.ks(c))
                wt, wk = wload(wv[:, :, 512:1024], [KC, 512])
                rp = (b % 2) * 512
                for c in range(4):
                    bk = proj_fm(wt, wk, c * 128)
                    CP("act", kT.ap[:, c, rp:rp + 512], psum[:, bk, :], PK(bk), kT.k(c * 1024 + rp, c * 1024 + rp + 512))
                wt, wk = wload(wv[:, :, 1024:1536], [KC, 512])
                for tt in range(4):
                    bk = bank()
                    for k in range(KC):
                        MM(psum[:, bk, :], xTa.ap[:, k, xr + tt * 128:xr + tt * 128 + 128], wt[:, k, :], k == 0, k == KC - 1, wk + xk, PK(bk))
                    slot = (b % 2) * 4 + tt
                    CP("dve", vring.ap[:, slot, :, 0:64], psum[:, bk, :].rearrange("p (h d) -> p h d", d=64), PK(bk), vring.ks(slot))

                for qi in range(4):
                    qb = b * 4 + qi
                    js = [j for j in range(5) if qb - 4 + j >= 0]
                    for hg in range(2):
                        pts = {}
                        for ji, j in enumerate(js):
                            kb = qb - 4 + j
                            ro = (kb % 8) * 128
                            bk = bank()
                            for hh in range(4):
                                h = hg * 4 + hh
                                c = h // 2; r0 = (h % 2) * 64
                                MM(psum[:, bk, hh * 128:(hh + 1) * 128], kT.ap[r0:r0 + 64, c, ro:ro + 128], qT.ap[r0:r0 + 64, c, qi * 128:(qi + 1) * 128],
                                   True, False, kT.k(c * 1024 + ro, c * 1024 + ro + 128) + qT.ks(c), PK(bk))
                                MM(psum[:, bk, hh * 128:(hh + 1) * 128], identb.ap, biasT.ap[:, h * 5 + j, :], False, True, KIB + biasT.ks(h * 5 + j), PK(bk))
                            pt = PT[ji]
                            ACT(pt.ap.rearrange("p h q -> p (h q)"), psum[:, bk, :], AF.Exp, PK(bk), pt.k())
                            pts[j] = pt
                        bk = bank()
                        pv = psum[:, bk, 0:260].rearrange("p (h d) -> p h d", d=65)
                        for hh in range(4):
                            h = hg * 4 + hh
                            for ji, j in enumerate(js):
                                kb = qb - 4 + j
                                slot = kb % 8
                                MM(pv[:, hh, :], pts[j].ap[:, hh, :], vring.ap[:, slot, h, :], ji == 0, ji == len(js) - 1,
                                   pts[j].k() + vring.ks(slot), PK(bk))
                        RECIP(rc.ap[:, hg * 4:hg * 4 + 4].rearrange("p (h o) -> p h o", o=1), pv[:, :, 64:65], PK(bk), rc.k())
                        TT("dve", ytok.ap[:, hg * 256:(hg + 1) * 256].rearrange("p (h d) -> p h d", d=64), pv[:, :, 0:64],
                           rc.ap[:, hg * 4:hg * 4 + 4].rearrange("p (h o) -> p h o", o=1).to_broadcast([128, 4, 64]), ALU.mult,
                           PK(bk) + rc.k(), ytok.k())
                    bk = bank()
                    for c in range(4):
                        TR(psum[:, bk, c * 128:(c + 1) * 128], ytok.ap[:, c * 128:(c + 1) * 128], ident.ap, ytok.k() + KI, PK(bk))
                    wkeys = []
                    for c in range(4):
                        wkeys += yAT.k(c * 512 + qi * 128, c * 512 + qi * 128 + 128)
                    CP("act", yAT.ap[:, :, qi * 128:(qi + 1) * 128], psum[:, bk, :].rearrange("p (c t) -> p c t", t=128), PK(bk), wkeys)

                if BIS == 4:
                    continue
                wt_r, wk_r = wload(wv[:, :, 1536:2048], [KC, 512])
                wt_k, wk_k = wload(wv[:, :, 2048:2560], [KC, 512])
                wt_v, wk_v = wload(wv[:, :, 2560:3072], [KC, 512])
                for hp in range(4):
                    bk = proj_fm(wt_v, wk_v, hp * 128)
                    shift(psum[:, bk, :], 128, 8 + hp, v_all.ap[:, hp, :], v_all.ks(hp), bk)
                wt_l, wk_l = wload(wv[:, :, 3072:3360], [KC, 288])
                bk = proj_fm(wt_l, wk_l, 0)
                shift(psum[:, bk, :], 128, 12, twd.ap, twd.k(), bk)
                bk = proj_fm(wt_l, wk_l, 128)
                shift(psum[:, bk, :], 128, 13, tgd1.ap, tgd1.k(), bk)
                bk = proj_fm(wt_l, wk_l, 256, ncol=32)
                shift(psum[0:32, bk, :], 32, 14, tgd2.ap, tgd2.k(), bk)
                ACT(twd.ap[0:64, :], twd.ap[0:64, :], AF.Tanh, twd.k(), twd.k())
                ACT(tgd1.ap, tgd1.ap, AF.Sigmoid, tgd1.k(), tgd1.k())
                ACT(tgd2.ap, tgd2.ap, AF.Sigmoid, tgd2.k(), tgd2.k())
                if l == 0:
                    for hp in range(4):
                        DMA("sp", vf_d[hp, :, t0:t0 + 512], v_all.ap[:, hp, :], v_all.ks(hp), [("vf", hp, b)])
                else:
                    bk = bank()
                    for hp in range(4):
                        MM(psum[0:32, bk, :], vrd.ap[:, hp, :], v_all.ap[:, hp, :], hp == 0, hp == 3, vrd.k() + v_all.ks(hp), PK(bk))
                    CP("act", vd.ap, psum[0:32, bk, :], PK(bk), vd.k())

                def pp(name, hp):
                    return prm.ap[:, PRM[name], hp:hp + 1]

                c3 = lambda t: t.ap.rearrange("p (c t) -> p c t", t=64)
                arv = ar.ap

                def prep1(hp):
                    va = v_all.ap[:, hp, :]; vk = v_all.ks(hp)
                    bk = proj_fm(wt_r, wk_r, hp * 128)
                    shift(psum[:, bk, :], 128, hp, t_r.ap, t_r.k(), bk)
                    yield
                    bk = proj_fm(wt_k, wk_k, hp * 128)
                    shift(psum[:, bk, :], 128, 4 + hp, t_k.ap, t_k.k(), bk)
                    yield
                    if l > 0:
                        bk = bank()
                        MM(psum[:, bk, :], vru.ap[0:32, hp * 128:(hp + 1) * 128], vd.ap, True, True, vru.k() + vd.k(), PK(bk))
                        ACT(t_sq.ap, psum[:, bk, :], AF.Sigmoid, PK(bk) + prm.k(), t_sq.k(), bias=pp("vres_base", hp))
                        DMA("sp", t_vf.ap, vf_d[hp, :, t0:t0 + 512], [("vf", hp, b)], t_vf.k())
                        TT("dve", t_vf.ap, t_vf.ap, va, ALU.subtract, t_vf.k() + vk, t_vf.k())
                        yield
                        TT("dve", t_vf.ap, t_vf.ap, t_sq.ap, ALU.mult, t_vf.k() + t_sq.k(), t_vf.k())
                        TT("dve", va, va, t_vf.ap, ALU.add, vk + t_vf.k(), vk)
                        yield
                    bk = bank()
                    MM(psum[:, bk, :], lora.ap[0:64, 0, hp * 128:(hp + 1) * 128], twd.ap[0:64, :], True, True, lora.ks(0) + twd.k(), PK(bk))
                    ACT(t_sg.ap, psum[:, bk, :], AF.Sigmoid, PK(bk) + prm.k(), t_sg.k(), bias=pp("decay_base", hp))
                    yield
                    P.add("dve", lambda e: e.tensor_tensor_scan(out=t_cum.ap, data0=rmask.ap, data1=t_sg.ap, initial=0.0, op0=ALU.mult, op1=ALU.add),
                          rmask.k() + t_sg.k(), t_cum.k())
                    yield
                    ACT(t_pin.ap, t_cum.ap, AF.Exp, t_cum.k(), t_pin.k(), scale=-C0)
                    ACT(t_inv.ap, t_cum.ap, AF.Exp, t_cum.k(), t_inv.k(), scale=C0)
                    TT("dve", t_sg.ap, t_cum.ap, t_sg.ap, ALU.subtract, t_cum.k() + t_sg.k(), t_sg.k())
                    yield
                    ACT(t_cum.ap, t_sg.ap, AF.Exp, t_sg.k(), t_cum.k(), scale=-C0)
                    bk = bank()
                    MM(psum[:, bk, :], lora.ap[64:128, 0, hp * 128:(hp + 1) * 128], twd.ap[64:128, :], True, True, lora.ks(0) + twd.k(), PK(bk))
                    ACT(t_a.ap, psum[:, bk, :], AF.Sigmoid, PK(bk) + prm.k(), t_a.k(), bias=pp("aaa_base", hp))
                    yield
                    TS("dve", t_kk.ap, t_k.ap, pp("k_k", hp), ALU.mult, t_k.k() + prm.k(), t_kk.k())
                    TT("pool", t_sq.ap, t_kk.ap, t_kk.ap, ALU.mult, t_kk.k(), t_sq.k())
                    yield
                    bk = bank()
                    MM(psum[:, bk, :], onesbd.ap, t_sq.ap, True, True, onesbd.k() + t_sq.k(), PK(bk))
                    TS("dve", t_sq.ap, psum[:, bk, :], 1e-24, ALU.max, PK(bk), t_sq.k())
                    yield
                    ACT(t_sq.ap, t_sq.ap, AF.Sqrt, t_sq.k(), t_sq.k())
                    yield
                    RECIP(t_sq.ap, t_sq.ap, t_sq.k(), t_sq.k())
                    TT("dve", t_kk.ap, t_kk.ap, t_sq.ap, ALU.mult, t_kk.k() + t_sq.k(), t_kk.k())
                    yield
                    TS("dve", t_km.ap, t_a.ap, 1.0, ALU.subtract, t_a.k() + prm.k(), t_km.k(), s2=pp("k_a", hp), op1=ALU.mult)
                    STT("dve", t_km.ap, t_km.ap, 1.0, t_k.ap, ALU.add, ALU.mult, t_km.k() + t_k.k(), t_km.k())
                    yield
                    STT("dve", t_sq.ap, t_r.ap, pp("r_k", hp), t_km.ap, ALU.mult, ALU.mult, t_r.k() + t_km.k() + prm.k(), t_sq.k())
                    TT("dve", t_a.ap, t_a.ap, t_kk.ap, ALU.mult, t_a.k() + t_kk.k(), t_a.k())
                    yield

                def prep2(hp):
                    va = v_all.ap[:, hp, :]; vk = v_all.ks(hp)
                    bk = bank()
                    MM(psum[:, bk, :], onesbd.ap, t_sq.ap, True, True, onesbd.k() + t_sq.k(), PK(bk))
                    TT("dve", t_bon.ap, psum[:, bk, :], va, ALU.mult, PK(bk) + vk, t_bon.k())
                    CP("dve", pC.ap, c3(t_pin)[:, :, 63], t_pin.k(), pC.k())
                    STT("dve", arv[:, :, 0, :], c3(t_kk), -1.0, c3(t_cum), ALU.mult, ALU.mult, t_kk.k() + t_cum.k(), ar.k())
                    TT("pool", arv[:, :, 1, :], c3(t_r), c3(t_pin), ALU.mult, t_r.k() + t_pin.k(), ar.k())
                    TT("dve", bt.ap, t_a.ap, t_inv.ap, ALU.mult, t_a.k() + t_inv.k(), bt.k())
                    TT("pool", kt.ap, t_km.ap, t_inv.ap, ALU.mult, t_km.k() + t_inv.k(), kt.k())

                def scan_gen(hp):
                    va = v_all.ap[:, hp, :]; vk = v_all.ks(hp)
                    atv4 = lambda at: at.ap.rearrange("p (h m) x -> p h m x", h=2)

                    def stage1(c, sl):
                        cs = slice(c * 64, (c + 1) * 64)
                        tk_ = tokT[sl]; at = AT[sl]; x0t = X0[sl]; g0 = G0[sl]
                        bk = bank()
                        TR(psum[0:64, bk, 0:128], va[:, cs], ident.ap, vk + KI, PK(bk))
                        TR(psum[0:64, bk, 128:256], bt.ap[:, cs], ident.ap, bt.k() + KI, PK(bk))
                        TR(psum[0:64, bk, 256:384], kt.ap[:, cs], ident.ap, kt.k() + KI, PK(bk))
                        CP("dve", tk_.ap, psum[0:64, bk, 0:384].rearrange("p (a b) -> p a b", b=128), PK(bk), tk_.k())
                        bk = bank2()
                        for h2 in range(2):
                            rows = slice(h2 * 64, h2 * 64 + 64)
                            rhs = arv[rows, c, :, :].rearrange("p a b -> p (a b)")
                            MM(psum[0:64, bk + h2, 0:128], bt.ap[rows, cs], rhs, True, True, bt.k() + ar.k(), PK(bk + h2))
                            MM(psum[0:64, bk + h2, 128:256], kt.ap[rows, cs], rhs, True, True, kt.k() + ar.k(), PK(bk + h2))
                        TT("dve", at.ap.rearrange("p (h m) x -> p h (m x)", h=2), psum[0:64, bk:bk + 2, 0:256],
                           maskU.ap.rearrange("p (h m) x -> p h (m x)", h=2), ALU.mult, PK(bk) + PK(bk + 1) + maskU.k(), at.k())
                        yield
                        bk = bank2()
                        for h2 in range(2):
                            rows = slice(h2 * 64, h2 * 64 + 64)
                            MM(psum[0:64, bk + h2, 0:64], arv[rows, c, 0, :], bt.ap[rows, cs], True, True, bt.k() + ar.k(), PK(bk + h2))
                        TT("dve", x0t.ap, psum[0:64, bk:bk + 2, 0:64], maskL.ap, ALU.mult, PK(bk) + PK(bk + 1) + maskL.k(), x0t.k())
                        bk = bank()
                        for h2 in range(2):
                            MM(psum[0:64, bk, h2 * 64:(h2 + 1) * 64], at.ap[:, h2 * 2 + 1, 0:64], tk_.ap[:, 0, h2 * 64:(h2 + 1) * 64], True, True, at.k() + tk_.k(), PK(bk))
                        CP("dve", g0.ap, psum[0:64, bk, 0:128].rearrange("p (a b) -> p a b", b=64), PK(bk), g0.k())
                        tt_cur = Tt[sl][0]
                        TT("pool", tt_cur.ap, atv4(at)[:, :, 0, 0:64], identT.ap, ALU.add, at.k() + identT.k(), tt_cur.k())
                        yield
                        Xc = x0t.ap; Xtc = atv4(at)[:, :, 0, 0:64]
                        xck = x0t.k(); xtk = at.k()
                        for lvl in range(1, 6):
                            xn = Xn[sl][lvl % 2]
                            bk = bank()
                            for h2 in range(2):
                                MM(psum[0:64, bk, h2 * 64:(h2 + 1) * 64], Xtc[:, h2, :], Xc[:, h2, :], True, True, xck + xtk, PK(bk))
                                if lvl < 5:
                                    MM(psum[0:64, bk, 128 + h2 * 64:128 + (h2 + 1) * 64], Xc[:, h2, :], Xtc[:, h2, :], True, True, xck + xtk, PK(bk))
                            ncol = 256 if lvl < 5 else 128
                            CP("dve", xn.ap.rearrange("p a b -> p (a b)")[:, 0:ncol], psum[0:64, bk, 0:ncol], PK(bk), xn.k())
                            yield
                            Xc = xn.ap[:, 0:2, :]; Xtc = xn.ap[:, 2:4, :]; xck = xn.k(); xtk = xn.k()
                            bk = bank()
                            for h2 in range(2):
                                MM(psum[0:64, bk, h2 * 64:(h2 + 1) * 64], Xc[:, h2, :], tt_cur.ap[:, h2, :], True, True, xck + tt_cur.k(), PK(bk))
                            tt_new = Tt[sl][lvl % 2]
                            TT("dve", tt_new.ap, psum[0:64, bk, 0:128].rearrange("p (a b) -> p a b", b=64), tt_cur.ap, ALU.add, PK(bk) + tt_cur.k(), tt_new.k())
                            tt_cur = tt_new
                            yield

                    def stage2(c, sl):
                        tk_ = tokT[sl]; at = AT[sl]; g0 = G0[sl]; tt_cur = Tt[sl][1]
                        sk = Sst.ks(hp)
                        bk = bank2()
                        for h2 in range(2):
                            rows = slice(h2 * 64, h2 * 64 + 64)
                            MM(psum[0:64, bk + h2, 0:64], arv[rows, c, 0, :], Sst.ap[rows, hp, :], True, True, ar.k() + sk, PK(bk + h2))
                        TT("dve", Wsb.ap, psum[0:64, bk:bk + 2, 0:64], g0.ap, ALU.add, PK(bk) + PK(bk + 1) + g0.k(), Wsb.k())
                        yield
                        bk = bank()
                        for h2 in range(2):
                            MM(psum[0:64, bk, h2 * 64:(h2 + 1) * 64], tt_cur.ap[:, h2, :], Wsb.ap[:, h2, :], True, True, tt_cur.k() + Wsb.k(), PK(bk))
                        CP("dve", Usb.ap, psum[0:64, bk, 0:128].rearrange("p (a b) -> p a b", b=64), PK(bk), Usb.k())
                        TS("pool", Stmp.ap, Sst.ap[:, hp, :], pC.ap[:, c:c + 1], ALU.mult, sk + pC.k(), Stmp.k())
                        yield
                        for h2 in range(2):
                            rows = slice(h2 * 64, h2 * 64 + 64)
                            yo = psum[rows, 6, c * 64:(c + 1) * 64]
                            if h2 == 0:
                                MM(yo, Sst.ap[rows, hp, :], arv[rows, c, 1, :], True, False, sk + ar.k(), PK(6))
                            else:
                                MM(psum[rows, 7, c * 64:(c + 1) * 64], Sst.ap[rows, hp, :], arv[rows, c, 1, :], True, True, sk + ar.k(), PK(7))
                            MM(yo, Usb.ap[:, h2, :], at.ap[:, h2 * 2, 64:128], h2 == 1, False, Usb.k() + at.k(), PK(6))
                            MM(yo, tk_.ap[:, 0, h2 * 64:(h2 + 1) * 64], at.ap[:, h2 * 2 + 1, 64:128], False, True, tk_.k() + at.k(), PK(6))
                        bk = bank()
                        for h2 in range(2):
                            rows = slice(h2 * 64, h2 * 64 + 64)
                            so = psum[rows, bk, 0:64]
                            MM(so, tk_.ap[:, 1, h2 * 64:(h2 + 1) * 64], Usb.ap[:, h2, :], True, False, tk_.k() + Usb.k(), PK(bk))
                            MM(so, tk_.ap[:, 2, h2 * 64:(h2 + 1) * 64], tk_.ap[:, 0, h2 * 64:(h2 + 1) * 64], False, True, tk_.k(), PK(bk))
                        STT("dve", Sst.ap[:, hp, :], psum[:, bk, 0:64], pC.ap[:, c:c + 1], Stmp.ap, ALU.mult, ALU.add, PK(bk) + pC.k() + Stmp.k(), sk)
                        yield


                    s1_next = 0; s1_active = []; s1_done = set(); s2_c = 0; s2_gen = None; s2_done = 0
                    while s2_done < 8:
                        while len(s1_active) < 3 and s1_next < 8 and s1_next - s2_done < NSL:
                            s1_active.append((s1_next, stage1(s1_next, s1_next % NSL)))
                            s1_next += 1
                        for item in list(s1_active):
                            cc_, g_ = item
                            try:
                                next(g_)
                            except StopIteration:
                                s1_active.remove(item)
                                s1_done.add(cc_)
                        if s2_gen is None and s2_c < 8 and s2_c in s1_done:
                            s2_gen = stage2(s2_c, s2_c % NSL)
                        if s2_gen is not None:
                            try:
                                next(s2_gen)
                            except StopIteration:
                                s2_gen = None
                                s2_c += 1
                                s2_done += 1
                        yield

                def finalize(hp):
                    CP("dve", yT.ap, psum[:, 6, :], PK(6), yT.k())
                    TT("dve", yT.ap[64:128, :], psum[64:128, 7, :], yT.ap[64:128, :], ALU.add, PK(7) + yT.k(), yT.k())
                    bk = bank()
                    MM(psum[:, bk, :], onesbd.ap, yT.ap, True, True, onesbd.k() + yT.k(), PK(bk))
                    STT("dve", yT.ap, psum[:, bk, :], -1.0 / 64, yT.ap, ALU.mult, ALU.add, PK(bk) + yT.k(), yT.k())
                    TT("pool", gnt.ap, yT.ap, yT.ap, ALU.mult, yT.k(), gnt.k())
                    bk = bank()
                    MM(psum[:, bk, :], onesbd.ap, gnt.ap, True, True, onesbd.k() + gnt.k(), PK(bk))
                    TS("dve", gnt.ap, psum[:, bk, :], 1.0 / 64, ALU.mult, PK(bk), gnt.k(), s2=GN_EPS, op1=ALU.add)
                    ACT(gnt.ap, gnt.ap, AF.Sqrt, gnt.k(), gnt.k())
                    RECIP(gnt.ap, gnt.ap, gnt.k(), gnt.k())
                    TT("dve", yT.ap, yT.ap, gnt.ap, ALU.mult, yT.k() + gnt.k(), yT.k())
                    TS("dve", yT.ap, yT.ap, pp("gn_g", hp), ALU.mult, yT.k() + prm.k(), yT.k(), s2=pp("gn_b", hp), op1=ALU.add)
                    TT("pool", yT.ap, yT.ap, t_bon.ap, ALU.add, yT.k() + t_bon.k(), yT.k())
                    bk = bank()
                    MM(psum[:, bk, :], lora.ap[:, 1, hp * 128:(hp + 1) * 128], tgd1.ap, True, False, lora.ks(1) + tgd1.k(), PK(bk))
                    MM(psum[:, bk, :], lora.ap[0:32, 2, hp * 128:(hp + 1) * 128], tgd2.ap, False, True, lora.ks(2) + tgd2.k(), PK(bk))
                    TT("dve", yBT.ap[:, hp, :], psum[:, bk, :], yT.ap, ALU.mult, PK(bk) + yT.k(), yBT.ks(hp))

                for _ in prep1(0):
                    pass
                prep2(0)
                for hp in range(4):
                    gens = [scan_gen(hp)]
                    if hp + 1 < 4:
                        gens.append(prep1(hp + 1))
                    interleave(gens, 2)
                    finalize(hp)
                    if hp + 1 < 4:
                        prep2(hp + 1)

                for half in range(2):
                    t = wbuf()
                    mhv = t.ap.rearrange("p a b -> p (a b)").rearrange("p (w k n) -> p w k n", w=2, k=4)
                    cast_load(mhv[:, 0], w_a_h[l][:, half * 512:(half + 1) * 512].rearrange("(k p) n -> p k n", p=128), t.k())
                    cast_load(mhv[:, 1], w_b_h[l][:, half * 512:(half + 1) * 512].rearrange("(k p) n -> p k n", p=128), t.k())
                    mhk = t.k()
                    wga, wgak = wload(wv[:, :, 3360 + half * 512:3360 + (half + 1) * 512], [KC, 512])
                    wgb, wgbk = wload(wv[:, :, 4384 + half * 512:4384 + (half + 1) * 512], [KC, 512])
                    for cc in range(4):
                        c = half * 4 + cc
                        bka = bank()
                        for k in range(4):
                            MM(psum[:, bka, :], mhv[:, 0, k, cc * 128:(cc + 1) * 128], yAT.ap[:, k, :], k == 0, k == 3, mhk + yAT.ks(k), PK(bka))
                        bkg = proj_fm(wga, wgak, cc * 128)
                        ACT(sga.ap, psum[:, bkg, :], AF.Sigmoid, PK(bkg), sga.k())
                        TT("dve", m1.ap, psum[:, bka, :], sga.ap, ALU.mult, PK(bka) + sga.k(), m1.k())
                        bkb = bank()
                        for k in range(4):
                            MM(psum[:, bkb, :], mhv[:, 1, k, cc * 128:(cc + 1) * 128], yBT.ap[:, k, :], k == 0, k == 3, mhk + yBT.ks(k), PK(bkb))
                        bkg = proj_fm(wgb, wgbk, cc * 128)
                        ACT(sgb.ap, psum[:, bkg, :], AF.Sigmoid, PK(bkg), sgb.k())
                        TT("dve", m2.ap, psum[:, bkb, :], sgb.ap, ALU.mult, PK(bkb) + sgb.k(), m2.k())
                        TT("pool", zT.ap[:, c, :], m1.ap, m2.ap, ALU.add, m1.k() + m2.k(), zT.ks(c))
                wo = []
                for kh in range(2):
                    wo.append(wload(w_out_h[l][kh * 512:(kh + 1) * 512, :].rearrange("(k p) n -> p k n", p=128), [4, 1024]))
                def ln1_tile(tt):
                    ti = b * 4 + tt
                    hbuf = hbufs[tt % 2]; x0 = x0s[tt % 2]; x1fm = x1fms[tt % 2]
                    st6 = st6s[tt % 2]; mv4 = mv4s[tt % 2]; lg = lgs[tt % 2]; rt = rts[tt % 2]
                    DMA("sp", x0.ap, xs_d[ti * 128:(ti + 1) * 128, :], [("xs", ti)], x0.k())
                    for nh in range(2):
                        bk = bank()
                        for k in range(KC):
                            wvv, wkk = wo[k // 4]
                            MM(psum[:, bk, :], zT.ap[:, k, tt * 128:(tt + 1) * 128], wvv[:, k % 4, nh * 512:(nh + 1) * 512], k == 0, k == KC - 1,
                               zT.ks(k) + wkk, PK(bk))
                        STT("dve", hbuf.ap[:, nh * 512:(nh + 1) * 512], x0.ap[:, nh * 512:(nh + 1) * 512], ALPHA, psum[:, bk, :], ALU.mult, ALU.add,
                            x0.k() + PK(bk), hbuf.k())
                    yield
                    yield from layer_norm_g(hbuf.ap, hbuf.k(), hbuf.ap, hbuf.k(), st6, mv4)
                    DMA("sp", xs_d[ti * 128:(ti + 1) * 128, :], hbuf.ap, hbuf.k(), [("xs", ti)])
                    to_feature_major(hbuf.ap, hbuf.k(), ti * 128, f32_out=x1fm)
                    yield
                    yield from router_g(l, ti, x1fm, lg, rt)

                interleave([ln1_tile(tt) for tt in range(4)], 2)

        def router_g(l, ti, x1fm, lg, rt):
            bk = bank()
            for k in range(KC):
                MM(psum[:, bk, 0:36], x1fm.ap[:, k, :], rw.ap[:, k, :], k == 0, k == KC - 1, x1fm.ks(k) + rw.k(), PK(bk))
            TT("dve", lg.ap, psum[:, bk, 0:36], rb.ap, ALU.add, PK(bk) + rb.k(), lg.k())
            K = lg.k() + rt.k()
            r = rt.ap
            g4 = lg.ap[:, 0:4]
            P.add("dve", lambda e: e.reduce_max(out=r[:, 0:1], in_=g4, axis=mybir.AxisListType.X), K, K)
            TS("dve", r[:, 4:8], g4, r[:, 0:1], ALU.subtract, K, K)
            ACT(r[:, 4:8], r[:, 4:8], AF.Exp, K, K)
            P.add("dve", lambda e: e.reduce_sum(out=r[:, 1:2], in_=r[:, 4:8], axis=mybir.AxisListType.X), K, K)
            RECIP(r[:, 1:2], r[:, 1:2], K, K)
            yield
            TS("dve", r[:, 8:12], g4, r[:, 0:1], ALU.is_ge, K, K)
            ev = lg.ap[:, 4:36].rearrange("p (g e) -> p g e", e=8)
            TT("dve", r[:, 32:64].rearrange("p (g e) -> p g e", e=8), ev, r[:, 8:12].rearrange("p (g o) -> p g o", o=1).to_broadcast([128, 4, 8]),
               ALU.mult, K, K)
            P.add("dve", lambda e: e.tensor_reduce(out=r[:, 12:20], in_=r[:, 32:64].rearrange("p (g e) -> p e g", e=8), axis=mybir.AxisListType.X,
                                                   op=ALU.add), K, K)
            sel = r[:, 12:20]
            P.add("dve", lambda e: e.reduce_max(out=r[:, 2:3], in_=sel, axis=mybir.AxisListType.X), K, K)
            TS("dve", r[:, 20:28], sel, r[:, 2:3], ALU.is_ge, K, K)
            STT("dve", r[:, 32:40], r[:, 20:28], -1e30, sel, ALU.mult, ALU.add, K, K)
            P.add("dve", lambda e: e.reduce_max(out=r[:, 3:4], in_=r[:, 32:40], axis=mybir.AxisListType.X), K, K)
            TS("dve", r[:, 40:48], sel, r[:, 3:4], ALU.is_ge, K, K)
            yield
            TT("dve", r[:, 48:49], r[:, 3:4], r[:, 2:3], ALU.subtract, K, K)
            ACT(r[:, 48:49], r[:, 48:49], AF.Exp, K, K)
            TS("dve", r[:, 48:49], r[:, 48:49], 1.0, ALU.add, K, K)
            RECIP(r[:, 49:50], r[:, 48:49], K, K)
            yield
            TS("dve", r[:, 50:51], r[:, 49:50], -1.0, ALU.mult, K, K, s2=1.0, op1=ALU.add)
            TT("dve", r[:, 49:51], r[:, 49:51], r[:, 1:2].to_broadcast([128, 2]), ALU.mult, K, K)
            TT("dve", r[:, 40:48], r[:, 40:48], r[:, 20:28], ALU.subtract, K, K)
            TS("dve", r[:, 40:48], r[:, 40:48], r[:, 50:51], ALU.mult, K, K)
            STT("dve", r[:, 40:48], r[:, 20:28], r[:, 49:50], r[:, 40:48], ALU.mult, ALU.add, K, K)
            cv = comb.ap[:, ti, :].rearrange("p (g e) -> p g e", e=8)
            TT("dve", cv, r[:, 8:12].rearrange("p (g o) -> p g o", o=1).to_broadcast([128, 4, 8]),
               r[:, 40:48].rearrange("p (o e) -> p o e", o=1).to_broadcast([128, 4, 8]), ALU.mult, K, comb.ks(ti))
            yield

        def phaseB(s, l, last):
            A.cur = mark_common
            acc = A.alloc([NT, D])
            xT = A.alloc([KC, S], BF16)
            for c in range(KC):
                DMA("sp", xT.ap[:, c, :], xT_d[c], [("xTd", i) for i in range(NT)], xT.ks(c))
            hT = [A.alloc([2, 512], BF16) for _ in range(2)]
            sil = [[A.alloc([512], BF16) for _ in range(2)] for _ in range(2)]
            x1s = [A.alloc([D]) for _ in range(2)]; st6s = [A.alloc([2, 6]) for _ in range(2)]; mv4s = [A.alloc([4]) for _ in range(2)]
            pts = [A.alloc([256]) for _ in range(2)]; pTs = [A.alloc([2, 128], BF16) for _ in range(2)]
            gsigs = [A.alloc([D]) for _ in range(2)]; lnp3 = A.alloc([2, D])
            assert Sst.off + Sst.n - lora.off >= 2048
            stgB = [Tile(A.ap, lora.off + i * 1024, 1024, F32, [1024]) for i in range(2)]
            sbc = [0]

            def stage_cast(dst, src, dkeys):
                a, b_ = src.shape[1], src.shape[2]
                step = max(1, 1024 // b_)
                for a0 in range(0, a, step):
                    a1 = min(a, a0 + step)
                    i = sbc[0]; sbc[0] += 1
                    st_ = stgB[i % 2]
                    sv = st_.ap[:, 0:(a1 - a0) * b_].rearrange("p (a b) -> p a b", b=b_)
                    DMA("sp", sv, src[:, a0:a1, :], (), st_.k())
                    CP("pool", dst[:, a0:a1, :], sv, st_.k(), dkeys)
            print("phaseB arena words", A.cur, "of", A.nwords)
            def load_expert(e):
                t = wbuf()
                gu = t.ap
                stage_cast(gu[:, :, 0:256], exp_gate[l, e].rearrange("(k p) n -> p k n", p=128), t.k())
                stage_cast(gu[:, :, 256:512], exp_up[l, e].rearrange("(k p) n -> p k n", p=128), t.k())
                t2 = wbuf()
                wd = t2.ap.rearrange("p a b -> p (a b)")[:, 0:2048].rearrange("p (k n) -> p k n", k=2)
                stage_cast(wd, exp_down[l, e].rearrange("(k p) n -> p k n", p=128), t2.k())
                return gu, t.k(), wd, t2.k()

            bctr = [0]

            def bankB():
                b_ = bctr[0] % 8
                bctr[0] += 1
                return b_

            def GU(i, e, b, w):
                gu, guk, wd, wdk = w
                t0 = b * 512
                xk = []
                for c in range(KC):
                    xk += xT.k(c * S + t0, c * S + t0 + 512)
                ht = hT[i % 2]
                for dc in range(2):
                    bg = bankB()
                    for k in range(KC):
                        MM(psum[:, bg, :], gu[:, k, dc * 128:(dc + 1) * 128], xT.ap[:, k, t0:t0 + 512], k == 0, k == KC - 1, guk + xk, PK(bg))
                    bu = bankB()
                    for k in range(KC):
                        MM(psum[:, bu, :], gu[:, k, 256 + dc * 128:256 + (dc + 1) * 128], xT.ap[:, k, t0:t0 + 512], k == 0, k == KC - 1, guk + xk, PK(bu))
                    sl = sil[i % 2][dc]
                    ACT(sl.ap, psum[:, bg, :], AF.Silu, PK(bg), sl.k())
                    TT("dve", ht.ap[:, dc, :], psum[:, bu, :], sl.ap, ALU.mult, PK(bu) + sl.k(), ht.ks(dc))

            def DN(i, e, b, w):
                gu, guk, wd, wdk = w
                ht = hT[i % 2]
                for tt in range(4):
                    ti = b * 4 + tt
                    for nh in range(2):
                        bk = bankB()
                        for k in range(2):
                            MM(psum[:, bk, :], ht.ap[:, k, tt * 128:(tt + 1) * 128], wd[:, k, nh * 512:(nh + 1) * 512], k == 0, k == 1, ht.ks(k) + wdk, PK(bk))
                        av = acc.ap[:, ti, nh * 512:(nh + 1) * 512]
                        ak = acc.k(ti * D + nh * 512, ti * D + (nh + 1) * 512)
                        if e == 0:
                            TS("dve", av, psum[:, bk, :], comb.ap[:, ti, e:e + 1], ALU.mult, PK(bk) + comb.ks(ti), ak)
                        else:
                            STT("dve", av, psum[:, bk, :], comb.ap[:, ti, e:e + 1], av, ALU.mult, ALU.add, PK(bk) + comb.ks(ti) + ak, ak)

            items = [(e, b) for e in range(32) for b in range(NB)]
            wts = {0: load_expert(0), 1: load_expert(1)}
            GU(0, 0, 0, wts[0])
            for i, (e, b) in enumerate(items):
                if i + 1 < len(items):
                    e2, b2 = items[i + 1]
                    GU(i + 1, e2, b2, wts[e2])
                DN(i, e, b, wts[e])
                if b == NB - 1:
                    del wts[e]
                    if e + 2 < 32:
                        wts[e + 2] = load_expert(e + 2)
            wpg = []
            for kh in range(2):
                wpg.append(wload(ple_gate_h[l][kh * 512:(kh + 1) * 512, :].rearrange("(k p) n -> p k n", p=128), [4, 1024]))
            wpp, wppk = wload(ple_proj_h[l].rearrange("(k p) n -> p k n", p=128), [2, 1024])
            load_ln(ln_g[l, 1], ln_b[l, 1])
            load_ln(ln_g[l, 2], ln_b[l, 2], lnp3)
            def ep_tile(ti):
                hk = acc.ks(ti)
                h = acc.ap[:, ti, :]
                x1 = x1s[ti % 2]; st6 = st6s[ti % 2]; mv4 = mv4s[ti % 2]; pt = pts[ti % 2]; pT = pTs[ti % 2]; gsig = gsigs[ti % 2]
                DMA("sp", x1.ap, xs_d[ti * 128:(ti + 1) * 128, :], [("xs", ti)], x1.k())
                DMA("sp", pt.ap, p_d[l, s, ti * 128:(ti + 1) * 128, :], (), pt.k())
                STT("dve", h, x1.ap, ALPHA, h, ALU.mult, ALU.add, x1.k() + hk, hk)
                yield from layer_norm_g(h, hk, h, hk, st6, mv4)
                to_feature_major(h, hk, ti * 128, sb=(xT, S, ti * 128), to_dram=False)
                bk = bank()
                for c in range(2):
                    TR(psum[:, bk, c * 128:(c + 1) * 128], pt.ap[:, c * 128:(c + 1) * 128], ident.ap, pt.k() + KI, PK(bk))
                CP("dve", pT.ap, psum[:, bk, 0:256].rearrange("p (c t) -> p c t", t=128), PK(bk), pT.k())
                yield
                xk = []
                for c in range(KC):
                    xk += xT.k(c * S + ti * 128, c * S + ti * 128 + 128)
                for nh in range(2):
                    bk = bank()
                    for k in range(KC):
                        wvv, wkk = wpg[k // 4]
                        MM(psum[:, bk, :], xT.ap[:, k, ti * 128:(ti + 1) * 128], wvv[:, k % 4, nh * 512:(nh + 1) * 512], k == 0, k == KC - 1, xk + wkk, PK(bk))
                    ACT(gsig.ap[:, nh * 512:(nh + 1) * 512], psum[:, bk, :], AF.Sigmoid, PK(bk), gsig.k())
                    bk = bank()
                    for k in range(2):
                        MM(psum[:, bk, :], pT.ap[:, k, :], wpp[:, k, nh * 512:(nh + 1) * 512], k == 0, k == 1, pT.k() + wppk, PK(bk))
                    TT("dve", gsig.ap[:, nh * 512:(nh + 1) * 512], psum[:, bk, :], gsig.ap[:, nh * 512:(nh + 1) * 512], ALU.mult, PK(bk) + gsig.k(), gsig.k())
                    yield
                STT("dve", h, h, ALPHA, gsig.ap, ALU.mult, ALU.add, hk + gsig.k(), hk)
                yield from layer_norm_g(h, hk, h, hk, st6, mv4, lnp3)
                if last:
                    DMA("sp", y_d[s, ti * 128:(ti + 1) * 128, :], h, hk, [("y", s, ti)])
                else:
                    DMA("sp", xs_d[ti * 128:(ti + 1) * 128, :], h, hk, [("xs", ti)])
                    to_feature_major(h, hk, ti * 128)
                yield

            interleave([ep_tile(ti) for ti in range(NT)], 2)

        for s in range(NSEQ if STOP != "c" else 0):
            A.cur = mark_common
            load_ln(ln_in_g, ln_in_b)
            p0_x = [A.alloc([D]) for _ in range(2)]
            p0_st = A.alloc([2, 6]); p0_mv = A.alloc([4])
            for i in range(NT):
                xt = p0_x[i % 2]
                DMA("sp", xt.ap, x_d[s, i * 128:(i + 1) * 128, :], (), xt.k())
                if BIS != 1:
                    layer_norm(xt.ap, xt.k(), xt.ap, xt.k(), p0_st, p0_mv)
                DMA("sp", xs_d[i * 128:(i + 1) * 128, :], xt.ap, xt.k(), [("xs", i)])
                if BIS not in (1, 2):
                    to_feature_major(xt.ap, xt.k(), i * 128)
            if s == 0:
                precast()
                cast_load(biasT.ap, bias_h.rearrange("h j k q -> k (h j) q"), biasT.k())
            if STOP == "p0":
                break
            for l in range(DEPTH):
                phaseA(s, l)
                if STOP == "A":
                    break
                phaseB(s, l, l == DEPTH - 1)
                if STOP == "B":
                    break
            if STOP:
                break
        P.emit()
    return nc


REL_CLIP = 128
_NC_CACHE = {}


def _bias_table(rel_bias):
    k = np.arange(128)[:, None]
    q = np.arange(128)[None, :]
    tabs = []
    for j in range(5):
        dist = q - k + 128 * (4 - j)
        idx = np.clip(dist, -REL_CLIP, REL_CLIP) + REL_CLIP
        t = rel_bias[:, idx]
        kc = (k // 64) + 2 * (j - 4)
        qc = q // 64
        valid = (kc <= qc) & (kc >= qc - 8)
        t = np.where(valid[None], t, np.float32(-1e30))
        tabs.append(t)
    return np.ascontiguousarray(np.stack(tabs, axis=1).astype(np.float32))


def prep_inputs(inputs, S, NSEQ, ncores):
    f = lambda a: np.ascontiguousarray(np.asarray(a, dtype=np.float32))
    shared = {
        "ln_in_g": f(inputs["ln_in_g"]), "ln_in_b": f(inputs["ln_in_b"]),
        "bias_tab": _bias_table(f(inputs["rel_bias"])),
        "w_in": f(inputs["w_in"]), "tok_mix": f(inputs["tok_mix"]),
        "decay_base": f(inputs["decay_base"]), "decay_up": f(inputs["decay_up"]),
        "aaa_base": f(inputs["aaa_base"]), "aaa_up": f(inputs["aaa_up"]), "gate_up": f(inputs["gate_up"]),
        "k_k": f(inputs["k_k"]), "k_a": f(inputs["k_a"]), "r_k": f(inputs["r_k"]).reshape(-1, 512),
        "vres_base": f(inputs["vres_base"]), "vres_down": f(inputs["vres_down"]), "vres_up": f(inputs["vres_up"]),
        "gn_g": f(inputs["gn_g"]), "gn_b": f(inputs["gn_b"]),
        "w_branch_attn": f(inputs["w_branch_attn"]), "w_branch_rwkv": f(inputs["w_branch_rwkv"]), "w_out": f(inputs["w_out"]),
        "router_w": np.ascontiguousarray(np.concatenate([f(inputs["router_grp"]), f(inputs["router_exp"])], axis=-1)),
        "router_b": np.ascontiguousarray(np.concatenate([f(inputs["router_grp_bias"]), f(inputs["router_exp_bias"])], axis=-1)),
        "exp_gate": f(inputs["exp_gate"]), "exp_up": f(inputs["exp_up"]), "exp_down": f(inputs["exp_down"]),
        "ple_proj": f(inputs["ple_proj"]), "ple_gate": f(inputs["ple_gate"]),
        "ln_g": f(inputs["ln_g"]), "ln_b": f(inputs["ln_b"]),
    }
    x = f(inputs["x"]); p = f(inputs["p"])
    maps = []
    for c in range(ncores):
        m = dict(shared)
        m["x"] = np.ascontiguousarray(x[c * NSEQ:(c + 1) * NSEQ])
        m["p"] = np.ascontiguousarray(p[:, c * NSEQ:(c + 1) * NSEQ])
        maps.append(m)
    return maps


def kernel(**inputs):
    x = np.asarray(inputs["x"])
    B, S, _ = x.shape
    ncores = 8
    NSEQ = B // ncores
    key = (S, NSEQ)
    if key not in _NC_CACHE:
        _NC_CACHE[key] = build_nc(S, NSEQ)
    nc = _NC_CACHE[key]
    maps = prep_inputs(inputs, S, NSEQ, ncores)
    res = run_bass_kernel_spmd(nc, maps, core_ids=list(range(ncores)))
    return np.concatenate([np.asarray(r["y"], dtype=np.float32) for r in res.results], axis=0)
```

```python
import contextlib
import numpy as np
import concourse.bass as bass
import concourse.mybir as mybir
from concourse.bass_utils import run_bass_kernel_spmd

F32 = mybir.dt.float32
BF16 = mybir.dt.bfloat16
AF = mybir.ActivationFunctionType
ALU = mybir.AluOpType

D = 1024
KC = 8
NIN = 5408
ALPHA = 4.0 ** 0.25
LN_EPS = 1e-5
GN_EPS = 64e-5
C0 = float(np.exp(-0.5))
N_DMA_SEMS = 40
SAME_ENGINE_SYNC = True
GRAN = 64
STOP = None
ABLK = None
BIS = 0


class Op:
    __slots__ = ("eng", "fn", "deps", "dma", "idx", "signal", "tick", "dsem", "dval", "prev_dval", "sw", "gen")

    def __init__(self, eng, fn, dma):
        self.eng = eng
        self.fn = fn
        self.dma = dma
        self.deps = set()
        self.signal = False
        self.tick = 0
        self.dsem = None
        self.dval = 0
        self.prev_dval = 0
        self.sw = None
        self.gen = 0


class Prog:
    ENGS = ("pe", "act", "dve", "pool", "sp")

    def __init__(self, nc):
        self.nc = nc
        self.ops = []
        self.res = {}
        self.ndma = 0
        self.swgen = {}

    def add(self, eng, fn, reads=(), writes=(), dma=False, sw=None):
        op = Op(eng, fn, dma)
        op.idx = len(self.ops)
        res = self.res
        psr = [r for r in reads if r[0] == "ps"]
        if psr:
            reads = [r for r in reads if r[0] != "ps"]
            writes = list(writes) + psr
        for r in reads:
            st = res.get(r)
            if st is not None and st[0] is not None:
                op.deps.add(st[0])
        for w in writes:
            st = res.get(w)
            if st is not None:
                if st[0] is not None:
                    op.deps.add(st[0])
                op.deps.update(st[1])
        for r in reads:
            st = res.get(r)
            if st is None:
                res[r] = [None, [op.idx]]
            else:
                st[1].append(op.idx)
        for w in writes:
            res[w] = [op.idx, []]
        op.deps.discard(op.idx)
        if dma and sw is not None:
            op.sw = sw
            op.gen = self.swgen.get(sw, 0)
            self.swgen[sw] = op.gen + 1
        elif dma:
            op.dsem = self.ndma % N_DMA_SEMS
            self.ndma += 1
        self.ops.append(op)
        return op

    def emit(self):
        nc = self.nc
        ops = self.ops
        for op in ops:
            for d in op.deps:
                dop = ops[d]
                if dop.dma:
                    continue
                if dop.eng == op.eng and (dop.eng == "pe" or not SAME_ENGINE_SYNC):
                    continue
                dop.signal = True
        ticks = {e: 0 for e in self.ENGS}
        dcount = [0] * N_DMA_SEMS
        for op in ops:
            if op.dma and op.sw is not None:
                continue
            if op.dma:
                op.prev_dval = dcount[op.dsem]
                dcount[op.dsem] += 16
                op.dval = dcount[op.dsem]
            elif op.signal:
                ticks[op.eng] += 1
                op.tick = ticks[op.eng]
        per_eng = {e: [o for o in ops if o.eng == e] for e in self.ENGS}
        with contextlib.ExitStack() as es:
            esem = {e: es.enter_context(nc.semaphore("s_" + e)) for e in ("pe", "act", "dve", "pool")}
            dsems = [es.enter_context(nc.semaphore("d%d" % i)) for i in range(N_DMA_SEMS)]
            swsems = {}
            for i, slot in enumerate(sorted(self.swgen.keys(), key=str)):
                swsems[slot] = [es.enter_context(nc.semaphore("w%d_%d" % (i, j))) for j in range(2)]
            block = es.enter_context(nc.Block())

            def run(engname, eng):
                waited = {}

                def wait(key, sem, val):
                    if waited.get(key, 0) >= val:
                        return
                    eng.wait_ge(sem, val)
                    waited[key] = val

                for op in per_eng[engname]:
                    for d in sorted(op.deps):
                        dop = ops[d]
                        if dop.dma and dop.sw is not None:
                            wait(("sw", dop.sw, dop.gen), swsems[dop.sw][dop.gen % 2], 16)
                        elif dop.dma:
                            wait(("d", dop.dsem), dsems[dop.dsem], dop.dval)
                        else:
                            if dop.eng == engname and (engname == "pe" or not SAME_ENGINE_SYNC):
                                continue
                            wait(("e", dop.eng), esem[dop.eng], dop.tick)
                    if op.dma and op.sw is not None:
                        if op.gen >= 1:
                            wait(("sw", op.sw, op.gen - 1), swsems[op.sw][(op.gen - 1) % 2], 16)
                            eng.sem_clear(swsems[op.sw][(op.gen - 1) % 2])
                        op.fn(eng).then_inc(swsems[op.sw][op.gen % 2], 16)
                    elif op.dma:
                        if op.prev_dval > 0:
                            wait(("d", op.dsem), dsems[op.dsem], op.prev_dval)
                        op.fn(eng).then_inc(dsems[op.dsem], 16)
                    else:
                        ins = op.fn(eng)
                        if op.signal:
                            ins.then_inc(esem[engname], 1)
                for op in per_eng[engname]:
                    if op.dma and op.sw is None:
                        wait(("d", op.dsem), dsems[op.dsem], op.dval)

            if per_eng["pe"]:
                block.tensor(lambda e: run("pe", e))
            if per_eng["act"]:
                block.scalar(lambda e: run("act", e))
            if per_eng["dve"]:
                block.vector(lambda e: run("dve", e))
            if per_eng["pool"]:
                block.gpsimd(lambda e: run("pool", e))
            if per_eng["sp"]:
                block.sync(lambda e: run("sp", e))


class Tile:
    def __init__(self, arena, off, nwords, dtype, shape, parts=128):
        self.off = off
        self.n = nwords
        self.esz = 4 if dtype == F32 else 2
        ne = int(np.prod(shape))
        nwx = ne if dtype == F32 else (ne + 1) // 2
        v = arena[0:parts, off:off + nwx]
        if dtype != F32:
            v = v.bitcast(dtype)[:, 0:ne]
        self.shape = tuple(shape)
        if len(shape) == 2:
            v = v.rearrange("p (a b) -> p a b", b=shape[1])
        elif len(shape) == 3:
            v = v.rearrange("p (a b c) -> p a b c", b=shape[1], c=shape[2])
        self.ap = v
        self.inner = int(np.prod(shape[1:])) if len(shape) > 1 else 1

    def k(self, lo=None, hi=None):
        if lo is None:
            lo, hi = 0, self.n * 4 // self.esz
        w0 = self.off + (lo * self.esz) // 4
        w1 = self.off + (hi * self.esz + 3) // 4
        return [("sb", g) for g in range(w0 // GRAN, (w1 - 1) // GRAN + 1)]

    def ks(self, i, n=1):
        return self.k(i * self.inner, (i + n) * self.inner)


class Arena:
    def __init__(self, nc, es, nwords):
        self.t = es.enter_context(nc.sbuf_tensor("arena", [128, nwords], F32))
        self.ap = self.t[:]
        self.nwords = nwords
        self.cur = 0

    def alloc(self, shape, dtype=F32, parts=128):
        ne = int(np.prod(shape))
        nw = ne if dtype == F32 else (ne + 1) // 2
        nw = (nw + GRAN - 1) // GRAN * GRAN
        assert self.cur + nw <= self.nwords, ("arena overflow", self.cur, nw, self.nwords)
        t = Tile(self.ap, self.cur, nw, dtype, shape, parts)
        self.cur += nw
        return t


def build_nc(S, NSEQ, DEPTH=2, dbg=None):
    NB = S // 512
    NT = S // 128
    nc = bass.Bass("TRN2", target_bir_lowering=False)
    dram = lambda name, shape, kind="ExternalInput": nc.dram_tensor(name, shape, F32, kind=kind).ap()
    x_d = dram("x", [NSEQ, S, D])
    p_d = dram("p", [DEPTH, NSEQ, S, 256])
    ln_in_g = dram("ln_in_g", [D]); ln_in_b = dram("ln_in_b", [D])
    bias_d = dram("bias_tab", [8, 5, 128, 128])
    w_in = dram("w_in", [DEPTH, D, NIN])
    tok_mix = dram("tok_mix", [DEPTH, 1824])
    decay_base = dram("decay_base", [DEPTH, 512]); decay_up = dram("decay_up", [DEPTH, 64, 512])
    aaa_base = dram("aaa_base", [DEPTH, 512]); aaa_up = dram("aaa_up", [DEPTH, 64, 512])
    gate_up = dram("gate_up", [DEPTH, 160, 512])
    k_k = dram("k_k", [DEPTH, 512]); k_a = dram("k_a", [DEPTH, 512]); r_k = dram("r_k", [DEPTH, 512])
    vres_base = dram("vres_base", [DEPTH - 1, 512]); vres_down = dram("vres_down", [DEPTH - 1, 512, 32])
    vres_up = dram("vres_up", [DEPTH - 1, 32, 512])
    gn_g = dram("gn_g", [DEPTH, 512]); gn_b = dram("gn_b", [DEPTH, 512])
    w_a = dram("w_branch_attn", [DEPTH, 512, D]); w_b = dram("w_branch_rwkv", [DEPTH, 512, D])
    w_out = dram("w_out", [DEPTH, D, D])
    router_w = dram("router_w", [DEPTH, D, 36]); router_b = dram("router_b", [DEPTH, 36])
    exp_gate = dram("exp_gate", [DEPTH, 32, D, 256]); exp_up = dram("exp_up", [DEPTH, 32, D, 256])
    exp_down = dram("exp_down", [DEPTH, 32, 256, D])
    ple_proj = dram("ple_proj", [DEPTH, 256, D]); ple_gate = dram("ple_gate", [DEPTH, D, D])
    ln_g = dram("ln_g", [DEPTH, 3, D]); ln_b = dram("ln_b", [DEPTH, 3, D])
    y_d = dram("y", [NSEQ, S, D], kind="ExternalOutput")
    xs_d = dram("xs_scr", [S, D], kind="Internal")
    dramh = lambda name, shape: nc.dram_tensor(name, shape, BF16, kind="Internal").ap()
    xT_d = dramh("xT_scr", [KC, 128, S])
    w_in_h = dramh("w_in_h", [DEPTH, D, NIN]); w_a_h = dramh("w_a_h", [DEPTH, 512, D]); w_b_h = dramh("w_b_h", [DEPTH, 512, D])
    w_out_h = dramh("w_out_h", [DEPTH, D, D]); exp_gate_h = dramh("exp_gate_h", [DEPTH, 32, D, 256])
    exp_up_h = dramh("exp_up_h", [DEPTH, 32, D, 256]); exp_down_h = dramh("exp_down_h", [DEPTH, 32, 256, D])
    ple_proj_h = dramh("ple_proj_h", [DEPTH, 256, D]); ple_gate_h = dramh("ple_gate_h", [DEPTH, D, D])
    bias_h = dramh("bias_h", [8, 5, 128, 128])
    vf_d = dram("vf_scr", [4, 128, S], kind="Internal")
    dbg_d = {}
    if dbg:
        for name, shape in dbg.items():
            dbg_d[name] = dram("dbg_" + name, shape, kind="ExternalOutput")

    P = Prog(nc)
    es = contextlib.ExitStack()
    with es:
        A = Arena(nc, es, 53000)
        psum = es.enter_context(nc.psum_tensor("ps", [128, 8, 512], F32))
        bank_ctr = [0]

        def bank():
            b = bank_ctr[0] % 6
            bank_ctr[0] += 1
            return b

        def bank2():
            b = bank_ctr[0] % 6
            if b % 2:
                bank_ctr[0] += 1
                b = bank_ctr[0] % 6
            bank_ctr[0] += 2
            return b

        PK = lambda b: [("ps", b)]

        def MM(out, lhsT, rhs, start, stop, reads, writes):
            P.add("pe", lambda e: e.matmul(out, lhsT=lhsT, rhs=rhs, start=start, stop=stop), reads, writes)

        def TR(out, in_, ident, reads, writes):
            P.add("pe", lambda e: e.transpose(out, in_, ident), reads, writes)

        def ACT(out, in_, func, reads, writes, bias=None, scale=None):
            kw = {}
            if bias is not None:
                kw["bias"] = bias
            if scale is not None:
                kw["scale"] = scale
            P.add("act", lambda e: e.activation(out=out, in_=in_, func=func, **kw), reads, writes)

        def TT(eng, out, in0, in1, op, reads, writes):
            P.add(eng, lambda e: e.tensor_tensor(out=out, in0=in0, in1=in1, op=op), reads, writes)

        def TS(eng, out, in0, s1, op0, reads, writes, s2=None, op1=None):
            if op1 is None:
                P.add(eng, lambda e: e.tensor_scalar(out=out, in0=in0, scalar1=s1, scalar2=None, op0=op0), reads, writes)
            else:
                P.add(eng, lambda e: e.tensor_scalar(out=out, in0=in0, scalar1=s1, scalar2=s2, op0=op0, op1=op1), reads, writes)

        def STT(eng, out, in0, scalar, in1, op0, op1, reads, writes):
            P.add(eng, lambda e: e.scalar_tensor_tensor(out=out, in0=in0, scalar=scalar, in1=in1, op0=op0, op1=op1), reads, writes)

        def CP(eng, out, in_, reads, writes):
            if eng == "act":
                P.add("dve", lambda e: e.tensor_copy(out=out, in_=in_), reads, writes)
            else:
                P.add(eng, lambda e: e.tensor_copy(out=out, in_=in_), reads, writes)

        def RECIP(out, in_, reads, writes):
            P.add("dve", lambda e: e.reciprocal(out=out, in_=in_), reads, writes)

        def MEMSET(eng, ap, val, writes):
            P.add(eng, lambda e: e.memset(ap, val), (), writes)

        def DMA(eng, out, in_, reads, writes, sw=None, **kw):
            P.add(eng, lambda e: e.dma_start(out=out, in_=in_, **kw), reads, writes, dma=True, sw=sw)

        ident = A.alloc([128]); identb = A.alloc([128], BF16)
        onesbd = A.alloc([128])
        maskU = A.alloc([4, 128], parts=64)
        maskL = A.alloc([2, 64], parts=64)
        identT = A.alloc([2, 64], parts=64)
        rmask = A.alloc([512])
        biasT = A.alloc([40, 128], BF16)
        KI = ident.k(); KIB = identb.k()
        MEMSET("pool", ident.ap, 0.0, KI)
        P.add("pool", lambda e: e.affine_select(out=ident.ap, in_=ident.ap, pattern=[[-1, 128]], compare_op=ALU.not_equal,
                                                fill=1.0, base=0, channel_multiplier=1), KI, KI)
        CP("pool", identb.ap, ident.ap, KI, KIB)
        MEMSET("pool", onesbd.ap, 0.0, onesbd.k())
        MEMSET("pool", onesbd.ap[0:64, 0:64], 1.0, onesbd.k())
        MEMSET("pool", onesbd.ap[64:128, 64:128], 1.0, onesbd.k())
        MEMSET("pool", maskU.ap, 1.0, maskU.k())
        for q in range(4):
            P.add("pool", (lambda q: lambda e: e.affine_select(out=maskU.ap[:, q, 0:64], in_=maskU.ap[:, q, 0:64], pattern=[[1, 64]],
                  compare_op=ALU.is_ge, fill=0.0, base=-1, channel_multiplier=-1))(q), maskU.k(), maskU.k())
            P.add("pool", (lambda q: lambda e: e.affine_select(out=maskU.ap[:, q, 64:128], in_=maskU.ap[:, q, 64:128], pattern=[[1, 64]],
                  compare_op=ALU.is_ge, fill=0.0, base=0, channel_multiplier=-1))(q), maskU.k(), maskU.k())
        MEMSET("pool", maskL.ap, 1.0, maskL.k())
        for q in range(2):
            P.add("pool", (lambda q: lambda e: e.affine_select(out=maskL.ap[:, q, :], in_=maskL.ap[:, q, :], pattern=[[-1, 64]],
                  compare_op=ALU.is_ge, fill=0.0, base=-1, channel_multiplier=1))(q), maskL.k(), maskL.k())
        CP("pool", identT.ap[:, 0, :], ident.ap[0:64, 0:64], KI, identT.k())
        CP("pool", identT.ap[:, 1, :], ident.ap[0:64, 0:64], KI, identT.k())
        MEMSET("pool", rmask.ap, 1.0, rmask.k())
        MEMSET("pool", rmask.ap.rearrange("p (c t) -> p c t", t=64)[:, :, 0:1], 0.0, rmask.k())

        xstg = [A.alloc([KC, 128], BF16) for _ in range(2)]
        xsc = [0]
        NWB = 4
        wpool = [A.alloc([KC, 512], BF16) for _ in range(NWB)]
        wctr = [0]

        def wbuf():
            t = wpool[wctr[0] % NWB]
            t.idx = wctr[0] % NWB
            wctr[0] += 1
            return t

        def cast_load(dst, src, dkeys):
            DMA("sp", dst, src, ["wh_all"], dkeys)

        lnp = A.alloc([2, D])
        comb = A.alloc([NT, 32])
        prm = A.alloc([16, 4])
        PRM = {n: i for i, n in enumerate(["decay_base", "aaa_base", "k_k", "k_a", "r_k", "gn_g", "gn_b", "vres_base"])}
        mu = A.alloc([2, 16])
        lora = A.alloc([3, 512])
        vrd = A.alloc([4, 32]); vru = A.alloc([512], parts=32)
        rw = A.alloc([KC, 36]); rb = A.alloc([36])
        lastc = A.alloc([16])
        Sst = A.alloc([4, 64])

        mark_common = A.cur

        def precast():
            stg = [A.alloc([4096]) for _ in range(4)]
            outb = [A.alloc([4096], BF16) for _ in range(4)]
            cnt = [0]
            allk = []

            def chunk(src3, dst3):
                a, b_ = src3.shape[1], src3.shape[2]
                i = cnt[0]; cnt[0] += 1
                st_ = stg[i % 4]; ob = outb[i % 4]
                sv = st_.ap[:, 0:a * b_].rearrange("p (a b) -> p a b", b=b_)
                ov = ob.ap[:, 0:a * b_].rearrange("p (a b) -> p a b", b=b_)
                DMA("sp", sv, src3, (), st_.k())
                CP("dve", ov, sv, st_.k(), ob.k())
                key = ("whc", i)
                DMA("act", dst3, ov, ob.k(), [key])
                allk.append(key)

            def cast2d(src2, dst2):
                R, C = src2.shape
                nrb = R // 128
                if C <= 4096:
                    Astep = max(1, 4096 // C)
                    sv = src2.rearrange("(a p) n -> p a n", p=128)
                    dv = dst2.rearrange("(a p) n -> p a n", p=128)
                    for a0 in range(0, nrb, Astep):
                        a1 = min(nrb, a0 + Astep)
                        chunk(sv[:, a0:a1, :], dv[:, a0:a1, :])
                else:
                    sv = src2.rearrange("(a p) n -> p a n", p=128)
                    dv = dst2.rearrange("(a p) n -> p a n", p=128)
                    for a0 in range(nrb):
                        for c0 in range(0, C, 4096):
                            c1 = min(C, c0 + 4096)
                            chunk(sv[:, a0:a0 + 1, c0:c1], dv[:, a0:a0 + 1, c0:c1])

            cast2d(w_in.rearrange("l k n -> (l k) n"), w_in_h.rearrange("l k n -> (l k) n"))
            cast2d(w_a.rearrange("l k n -> (l k) n"), w_a_h.rearrange("l k n -> (l k) n"))
            cast2d(w_b.rearrange("l k n -> (l k) n"), w_b_h.rearrange("l k n -> (l k) n"))
            cast2d(w_out.rearrange("l k n -> (l k) n"), w_out_h.rearrange("l k n -> (l k) n"))
            cast2d(ple_proj.rearrange("l k n -> (l k) n"), ple_proj_h.rearrange("l k n -> (l k) n"))
            cast2d(ple_gate.rearrange("l k n -> (l k) n"), ple_gate_h.rearrange("l k n -> (l k) n"))
            cast2d(bias_d.rearrange("h j k q -> (h j k) q"), bias_h.rearrange("h j k q -> (h j k) q"))
            DMA("sp", stg[0].ap[0:1, 0:8], ln_in_g[0:8].rearrange("(o n) -> o n", o=1), allk, stg[0].k() + ["wh_all"])
            A.cur = mark_common


        def load_ln(gsrc, bsrc, lp=None):
            lp = lp or lnp
            DMA("sp", lp.ap[:, 0, :], gsrc.partition_broadcast(128), (), lp.ks(0))
            DMA("sp", lp.ap[:, 1, :], bsrc.partition_broadcast(128), (), lp.ks(1))

        def layer_norm_g(h, hk, out_ap, out_keys, st, mv, lp=None):
            lp = lp or lnp
            for c in range(2):
                P.add("dve", (lambda c: lambda e: e.bn_stats(out=st.ap[:, c, :], in_=h[:, c * 512:(c + 1) * 512]))(c), hk, st.k())
            P.add("dve", lambda e: e.bn_aggr(out=mv.ap[:, 0:2], in_=st.ap), st.k(), mv.k())
            TS("dve", mv.ap[:, 2:3], mv.ap[:, 1:2], LN_EPS, ALU.add, mv.k(), mv.k())
            yield
            ACT(mv.ap[:, 2:3], mv.ap[:, 2:3], AF.Sqrt, mv.k(), mv.k())
            yield
            RECIP(mv.ap[:, 2:3], mv.ap[:, 2:3], mv.k(), mv.k())
            TS("dve", h, h, mv.ap[:, 0:1], ALU.subtract, hk + mv.k(), hk, s2=mv.ap[:, 2:3], op1=ALU.mult)
            yield
            TT("dve", h, h, lp.ap[:, 0, :], ALU.mult, hk + lp.ks(0), hk)
            yield
            TT("pool", out_ap, h, lp.ap[:, 1, :], ALU.add, hk + lp.ks(1), out_keys)
            yield

        def layer_norm(h, hk, out_ap, out_keys, st, mv, lp=None):
            for _ in layer_norm_g(h, hk, out_ap, out_keys, st, mv, lp):
                pass

        def interleave(gens, width):
            gens = list(gens)
            nxt = 0
            active = []
            while nxt < len(gens) or active:
                while len(active) < width and nxt < len(gens):
                    active.append(gens[nxt])
                    nxt += 1
                for g_ in list(active):
                    try:
                        next(g_)
                    except StopIteration:
                        active.remove(g_)

        def to_feature_major(xt_tok, xk, tok0, f32_out=None, sb=None, to_dram=True):
            if to_dram:
                stg = xstg[xsc[0] % 2]
                xsc[0] += 1
            for half in range(2):
                b = bank()
                for cc in range(4):
                    c = half * 4 + cc
                    TR(psum[:, b, cc * 128:(cc + 1) * 128], xt_tok[:, c * 128:(c + 1) * 128], ident.ap, xk + KI, PK(b))
                src = psum[:, b, :].rearrange("p (c t) -> p c t", t=128)
                if sb is not None:
                    tl, ncols, col0 = sb
                    wk = []
                    for c in range(half * 4, half * 4 + 4):
                        wk += tl.k(c * ncols + col0, c * ncols + col0 + 128)
                    CP("dve", tl.ap[:, half * 4:half * 4 + 4, col0:col0 + 128], src, PK(b), wk)
                if to_dram:
                    CP("dve", stg.ap[:, half * 4:half * 4 + 4, :], src, PK(b), stg.ks(half * 4, 4))
                if f32_out is not None:
                    CP("dve", f32_out.ap[:, half * 4:half * 4 + 4, :], src, PK(b), f32_out.ks(half * 4, 4))
            if to_dram:
                DMA("sp", xT_d[:, :, tok0:tok0 + 128].rearrange("c p t -> p c t"), stg.ap, stg.k(), [("xTd", tok0 // 128)])

        def load_layer_params(l):
            fm = lambda src: src.rearrange("(c p) -> p c", p=128)
            lst = [("decay_base", decay_base[l]), ("aaa_base", aaa_base[l]), ("k_k", k_k[l]), ("k_a", k_a[l]),
                   ("r_k", r_k[l]), ("gn_g", gn_g[l]), ("gn_b", gn_b[l])]
            if l > 0:
                lst.append(("vres_base", vres_base[l - 1]))
            for name, src in lst:
                DMA("sp", prm.ap[:, PRM[name], :], fm(src), (), prm.k(), allow_slow_non_contiguous=True)
            DMA("sp", mu.ap[:, 0, 0:14], tok_mix[l, 0:1792].rearrange("(c p) -> p c", p=128), (), mu.k(), allow_slow_non_contiguous=True)
            DMA("sp", mu.ap[0:32, 0, 14:15], tok_mix[l, 1792:1824].rearrange("(p o) -> p o", o=1), (), mu.k())
            TS("pool", mu.ap[:, 1, 0:14], mu.ap[:, 0, 0:14], -1.0, ALU.mult, mu.k(), mu.k(), s2=1.0, op1=ALU.add)
            TS("pool", mu.ap[0:32, 1, 14:15], mu.ap[0:32, 0, 14:15], -1.0, ALU.mult, mu.k(), mu.k(), s2=1.0, op1=ALU.add)
            DMA("sp", lora.ap[0:64, 0, :], decay_up[l], (), lora.ks(0))
            DMA("sp", lora.ap[64:128, 0, :], aaa_up[l], (), lora.ks(0))
            DMA("sp", lora.ap[:, 1, :], gate_up[l, 0:128, :], (), lora.ks(1))
            DMA("sp", lora.ap[0:32, 2, :], gate_up[l, 128:160, :], (), lora.ks(2))
            if l > 0:
                DMA("sp", vrd.ap, vres_down[l - 1].rearrange("(c p) r -> p c r", p=128), (), vrd.k())
                DMA("sp", vru.ap, vres_up[l - 1], (), vru.k())
            DMA("sp", rw.ap, router_w[l].rearrange("(k p) n -> p k n", p=128), (), rw.k())
            DMA("sp", rb.ap, router_b[l].partition_broadcast(128), (), rb.k())

        def wload(src_ap, shape):
            t = wbuf()
            n = int(np.prod(shape))
            v = t.ap.rearrange("p a b -> p (a b)")[:, 0:n]
            if len(shape) == 2:
                v = v.rearrange("p (a b) -> p a b", b=shape[1])
            cast_load(v, src_ap, t.k())
            return v, t.k()

        def phaseA(s, l):
            A.cur = mark_common
            kT = A.alloc([4, 1024], BF16); vring = A.alloc([8, 8, 65], BF16)
            xTa = A.alloc([KC, 1024], BF16)
            yAT = A.alloc([4, 512], BF16); yBT = A.alloc([4, 512], BF16)
            st6s = [A.alloc([2, 6]) for _ in range(2)]; mv4s = [A.alloc([4]) for _ in range(2)]
            lgs = [A.alloc([36]) for _ in range(2)]; rts = [A.alloc([64]) for _ in range(2)]
            twd = A.alloc([512]); tgd1 = A.alloc([512]); tgd2 = A.alloc([512], parts=32)
            v_all = A.alloc([4, 512]); vd = A.alloc([512], parts=32)
            mark_att = A.cur
            qT = A.alloc([4, 512], BF16)
            PT = [A.alloc([4, 128], BF16) for _ in range(6)]
            ytok = A.alloc([512]); rc = A.alloc([8])
            end_att = A.cur
            A.cur = mark_att
            NSL = 4
            tokT = [A.alloc([3, 128], parts=64) for _ in range(NSL)]
            AT = [A.alloc([4, 128], parts=64) for _ in range(NSL)]
            X0 = [A.alloc([2, 64], parts=64) for _ in range(NSL)]
            Xn = [[A.alloc([4, 64], parts=64) for _ in range(2)] for _ in range(NSL)]
            Tt = [[A.alloc([2, 64], parts=64) for _ in range(2)] for _ in range(NSL)]
            G0 = [A.alloc([2, 64], parts=64) for _ in range(NSL)]
            A.cur = max(A.cur, end_att)
            mark_r = A.cur
            t_r = A.alloc([512]); t_k = A.alloc([512]); t_sg = A.alloc([512]); t_cum = A.alloc([512])
            t_pin = A.alloc([512]); t_inv = A.alloc([512]); t_a = A.alloc([512]); t_kk = A.alloc([512])
            t_sq = A.alloc([512]); t_km = A.alloc([512]); t_bons = [A.alloc([512]) for _ in range(2)]
            bt = A.alloc([512]); kt = A.alloc([512]); yT = A.alloc([512]); gnt = A.alloc([512])
            ar = A.alloc([8, 2, 64]); pC = A.alloc([8]); t_vf = A.alloc([512])
            Wsb = A.alloc([2, 64], parts=64); Usb = A.alloc([2, 64], parts=64); Stmp = A.alloc([64])
            end_r = A.cur
            A.cur = mark_r
            sga = A.alloc([512]); sgb = A.alloc([512]); m1 = A.alloc([512]); m2 = A.alloc([512])
            zT = A.alloc([8, 512], BF16)
            hbufs = [A.alloc([D]) for _ in range(2)]; x0s = [A.alloc([D]) for _ in range(2)]; x1fms = [A.alloc([KC, 128]) for _ in range(2)]
            A.cur = max(A.cur, end_r)
            print("phaseA arena words", A.cur, "of", A.nwords)

            load_layer_params(l)
            load_ln(ln_g[l, 0], ln_b[l, 0])
            MEMSET("pool", lastc.ap, 0.0, lastc.k())
            MEMSET("pool", Sst.ap, 0.0, Sst.k())
            MEMSET("pool", vring.ap[:, :, :, 64:65], 1.0, vring.k())
            wv = w_in_h[l].rearrange("(k p) n -> p k n", p=128)

            def shift(pc_ap, np_, mc, out_ap, out_keys, b):
                m = mu.ap[0:np_, 0, mc:mc + 1]; om = mu.ap[0:np_, 1, mc:mc + 1]
                TS("dve", out_ap, pc_ap, om, ALU.mult, PK(b) + mu.k(), out_keys)
                STT("dve", out_ap[:, 1:512], pc_ap[:, 0:511], m, out_ap[:, 1:512], ALU.mult, ALU.add, PK(b) + out_keys + mu.k(), out_keys)
                STT("dve", out_ap[:, 0:1], lastc.ap[0:np_, mc:mc + 1], m, out_ap[:, 0:1], ALU.mult, ALU.add, lastc.k() + out_keys + mu.k(), out_keys)
                CP("dve", lastc.ap[0:np_, mc:mc + 1], pc_ap[:, 511:512], PK(b), lastc.k())

            def load_xblock(bb):
                rp_ = (bb % 2) * 512
                wk_ = []
                for c in range(KC):
                    wk_ += xTa.k(c * 1024 + rp_, c * 1024 + rp_ + 512)
                DMA("sp", xTa.ap[:, :, rp_:rp_ + 512], xT_d[:, :, bb * 512:(bb + 1) * 512].rearrange("c p t -> p c t"),
                    [("xTd", bb * 4 + i) for i in range(4)], wk_)

            load_xblock(0)
            for b in range(NB):
                t0 = b * 512
                xr = (b % 2) * 512
                if b + 1 < NB:
                    load_xblock(b + 1)
                xk = []
                for c in range(KC):
                    xk += xTa.k(c * 1024 + xr, c * 1024 + xr + 512)

                def proj_fm(wt, wk, col, ncol=128, kparts=128):
                    bk = bank()
                    for k in range(KC):
                        MM(psum[0:ncol, bk, :], wt[:, k, col:col + ncol], xTa.ap[:, k, xr:xr + 512], k == 0, k == KC - 1, wk + xk, PK(bk))
                    return bk

                wt, wk = wload(wv[:, :, 0:512], [KC, 512])
                for c in range(4):
                    bk = proj_fm(wt, wk, c * 128)
                    TS("dve", qT.ap[:, c, :], psum[:, bk, :], 0.125, ALU.mult, PK(bk), qT.ks(c))
                wt, wk = wload(wv[:, :, 512:1024], [KC, 512])
                rp = (b % 2) * 512
                for c in range(4):
                    bk = proj_fm(wt, wk, c * 128)
                    CP("act", kT.ap[:, c, rp:rp + 512], psum[:, bk, :], PK(bk), kT.k(c * 1024 + rp, c * 1024 + rp + 512))
                wt, wk = wload(wv[:, :, 1024:1536], [KC, 512])
                for tt in range(4):
                    bk = bank()
                    for k in range(KC):
                        MM(psum[:, bk, :], xTa.ap[:, k, xr + tt * 128:xr + tt * 128 + 128], wt[:, k, :], k == 0, k == KC - 1, wk + xk, PK(bk))
                    slot = (b % 2) * 4 + tt
                    CP("dve", vring.ap[:, slot, :, 0:64], psum[:, bk, :].rearrange("p (h d) -> p h d", d=64), PK(bk), vring.ks(slot))

                for qi in range(4):
                    qb = b * 4 + qi
                    js = [j for j in range(5) if qb - 4 + j >= 0]
                    for hg in range(2):
                        pts = {}
                        for ji, j in enumerate(js):
                            kb = qb - 4 + j
                            ro = (kb % 8) * 128
                            bk = bank()
                            for hh in range(4):
                                h = hg * 4 + hh
                                c = h // 2; r0 = (h % 2) * 64
                                MM(psum[:, bk, hh * 128:(hh + 1) * 128], kT.ap[r0:r0 + 64, c, ro:ro + 128], qT.ap[r0:r0 + 64, c, qi * 128:(qi + 1) * 128],
                                   True, False, kT.k(c * 1024 + ro, c * 1024 + ro + 128) + qT.ks(c), PK(bk))
                                MM(psum[:, bk, hh * 128:(hh + 1) * 128], identb.ap, biasT.ap[:, h * 5 + j, :], False, True, KIB + biasT.ks(h * 5 + j), PK(bk))
                            pt = PT[ji]
                            ACT(pt.ap.rearrange("p h q -> p (h q)"), psum[:, bk, :], AF.Exp, PK(bk), pt.k())
                            pts[j] = pt
                        bk = bank()
                        pv = psum[:, bk, 0:260].rearrange("p (h d) -> p h d", d=65)
                        for hh in range(4):
                            h = hg * 4 + hh
                            for ji, j in enumerate(js):
                                kb = qb - 4 + j
                                slot = kb % 8
                                MM(pv[:, hh, :], pts[j].ap[:, hh, :], vring.ap[:, slot, h, :], ji == 0, ji == len(js) - 1,
                                   pts[j].k() + vring.ks(slot), PK(bk))
                        RECIP(rc.ap[:, hg * 4:hg * 4 + 4].rearrange("p (h o) -> p h o", o=1), pv[:, :, 64:65], PK(bk), rc.k())
                        TT("dve", ytok.ap[:, hg * 256:(hg + 1) * 256].rearrange("p (h d) -> p h d", d=64), pv[:, :, 0:64],
                           rc.ap[:, hg * 4:hg * 4 + 4].rearrange("p (h o) -> p h o", o=1).to_broadcast([128, 4, 64]), ALU.mult,
                           PK(bk) + rc.k(), ytok.k())
                    bk = bank()
                    for c in range(4):
                        TR(psum[:, bk, c * 128:(c + 1) * 128], ytok.ap[:, c * 128:(c + 1) * 128], ident.ap, ytok.k() + KI, PK(bk))
                    wkeys = []
                    for c in range(4):
                        wkeys += yAT.k(c * 512 + qi * 128, c * 512 + qi * 128 + 128)
                    CP("act", yAT.ap[:, :, qi * 128:(qi + 1) * 128], psum[:, bk, :].rearrange("p (c t) -> p c t", t=128), PK(bk), wkeys)

                if BIS == 4:
                    continue
                wt_r, wk_r = wload(wv[:, :, 1536:2048], [KC, 512])
                wt_k, wk_k = wload(wv[:, :, 2048:2560], [KC, 512])
                wt_v, wk_v = wload(wv[:, :, 2560:3072], [KC, 512])
                for hp in range(4):
                    bk = proj_fm(wt_v, wk_v, hp * 128)
                    shift(psum[:, bk, :], 128, 8 + hp, v_all.ap[:, hp, :], v_all.ks(hp), bk)
                wt_l, wk_l = wload(wv[:, :, 3072:3360], [KC, 288])
                bk = proj_fm(wt_l, wk_l, 0)
                shift(psum[:, bk, :], 128, 12, twd.ap, twd.k(), bk)
                bk = proj_fm(wt_l, wk_l, 128)
                shift(psum[:, bk, :], 128, 13, tgd1.ap, tgd1.k(), bk)
                bk = proj_fm(wt_l, wk_l, 256, ncol=32)
                shift(psum[0:32, bk, :], 32, 14, tgd2.ap, tgd2.k(), bk)
                ACT(twd.ap[0:64, :], twd.ap[0:64, :], AF.Tanh, twd.k(), twd.k())
                ACT(tgd1.ap, tgd1.ap, AF.Sigmoid, tgd1.k(), tgd1.k())
                ACT(tgd2.ap, tgd2.ap, AF.Sigmoid, tgd2.k(), tgd2.k())
                if l == 0:
                    for hp in range(4):
                        DMA("sp", vf_d[hp, :, t0:t0 + 512], v_all.ap[:, hp, :], v_all.ks(hp), [("vf", hp, b)])
                else:
                    bk = bank()
                    for hp in range(4):
                        MM(psum[0:32, bk, :], vrd.ap[:, hp, :], v_all.ap[:, hp, :], hp == 0, hp == 3, vrd.k() + v_all.ks(hp), PK(bk))
                    CP("act", vd.ap, psum[0:32, bk, :], PK(bk), vd.k())

                def pp(name, hp):
                    return prm.ap[:, PRM[name], hp:hp + 1]

                c3 = lambda t: t.ap.rearrange("p (c t) -> p c t", t=64)
                arv = ar.ap

                def prep1(hp):
                    va = v_all.ap[:, hp, :]; vk = v_all.ks(hp)
                    bk = proj_fm(wt_r, wk_r, hp * 128)
                    shift(psum[:, bk, :], 128, hp, t_r.ap, t_r.k(), bk)
                    yield
                    bk = proj_fm(wt_k, wk_k, hp * 128)
                    shift(psum[:, bk, :], 128, 4 + hp, t_k.ap, t_k.k(), bk)
                    yield
                    if l > 0:
                        bk = bank()
                        MM(psum[:, bk, :], vru.ap[0:32, hp * 128:(hp + 1) * 128], vd.ap, True, True, vru.k() + vd.k(), PK(bk))
                        ACT(t_sq.ap, psum[:, bk, :], AF.Sigmoid, PK(bk) + prm.k(), t_sq.k(), bias=pp("vres_base", hp))
                        DMA("sp", t_vf.ap, vf_d[hp, :, t0:t0 + 512], [("vf", hp, b)], t_vf.k())
                        TT("dve", t_vf.ap, t_vf.ap, va, ALU.subtract, t_vf.k() + vk, t_vf.k())
                        yield
                        TT("dve", t_vf.ap, t_vf.ap, t_sq.ap, ALU.mult, t_vf.k() + t_sq.k(), t_vf.k())
                        TT("dve", va, va, t_vf.ap, ALU.add, vk + t_vf.k(), vk)
                        yield
                    bk = bank()
                    MM(psum[:, bk, :], lora.ap[0:64, 0, hp * 128:(hp + 1) * 128], twd.ap[0:64, :], True, True, lora.ks(0) + twd.k(), PK(bk))
                    ACT(t_sg.ap, psum[:, bk, :], AF.Sigmoid, PK(bk) + prm.k(), t_sg.k(), bias=pp("decay_base", hp))
                    yield
                    P.add("dve", lambda e: e.tensor_tensor_scan(out=t_cum.ap, data0=rmask.ap, data1=t_sg.ap, initial=0.0, op0=ALU.mult, op1=ALU.add),
                          rmask.k() + t_sg.k(), t_cum.k())
                    yield
                    ACT(t_pin.ap, t_cum.ap, AF.Exp, t_cum.k(), t_pin.k(), scale=-C0)
                    ACT(t_inv.ap, t_cum.ap, AF.Exp, t_cum.k(), t_inv.k(), scale=C0)
                    TT("dve", t_sg.ap, t_cum.ap, t_sg.ap, ALU.subtract, t_cum.k() + t_sg.k(), t_sg.k())
                    yield
                    ACT(t_cum.ap, t_sg.ap, AF.Exp, t_sg.k(), t_cum.k(), scale=-C0)
                    bk = bank()
                    MM(psum[:, bk, :], lora.ap[64:128, 0, hp * 128:(hp + 1) * 128], twd.ap[64:128, :], True, True, lora.ks(0) + twd.k(), PK(bk))
                    ACT(t_a.ap, psum[:, bk, :], AF.Sigmoid, PK(bk) + prm.k(), t_a.k(), bias=pp("aaa_base", hp))
                    yield
                    TS("dve", t_kk.ap, t_k.ap, pp("k_k", hp), ALU.mult, t_k.k() + prm.k(), t_kk.k())
                    TT("pool", t_sq.ap, t_kk.ap, t_kk.ap, ALU.mult, t_kk.k(), t_sq.k())
                    yield
                    bk = bank()
                    MM(psum[:, bk, :], onesbd.ap, t_sq.ap, True, True, onesbd.k() + t_sq.k(), PK(bk))
                    TS("dve", t_sq.ap, psum[:, bk, :], 1e-24, ALU.max, PK(bk), t_sq.k())
                    yield
                    ACT(t_sq.ap, t_sq.ap, AF.Sqrt, t_sq.k(), t_sq.k())
                    yield
                    RECIP(t_sq.ap, t_sq.ap, t_sq.k(), t_sq.k())
                    TT("dve", t_kk.ap, t_kk.ap, t_sq.ap, ALU.mult, t_kk.k() + t_sq.k(), t_kk.k())
                    yield
                    TS("dve", t_km.ap, t_a.ap, 1.0, ALU.subtract, t_a.k() + prm.k(), t_km.k(), s2=pp("k_a", hp), op1=ALU.mult)
                    STT("dve", t_km.ap, t_km.ap, 1.0, t_k.ap, ALU.add, ALU.mult, t_km.k() + t_k.k(), t_km.k())
                    yield
                    STT("dve", t_sq.ap, t_r.ap, pp("r_k", hp), t_km.ap, ALU.mult, ALU.mult, t_r.k() + t_km.k() + prm.k(), t_sq.k())
                    TT("dve", t_a.ap, t_a.ap, t_kk.ap, ALU.mult, t_a.k() + t_kk.k(), t_a.k())
                    yield

                def prep2(hp):
                    va = v_all.ap[:, hp, :]; vk = v_all.ks(hp)
                    bk = bank()
                    MM(psum[:, bk, :], onesbd.ap, t_sq.ap, True, True, onesbd.k() + t_sq.k(), PK(bk))
                    t_bon = t_bons[hp % 2]
                    TT("dve", t_bon.ap, psum[:, bk, :], va, ALU.mult, PK(bk) + vk, t_bon.k())
                    CP("dve", pC.ap, c3(t_pin)[:, :, 63], t_pin.k(), pC.k())
                    STT("dve", arv[:, :, 0, :], c3(t_kk), -1.0, c3(t_cum), ALU.mult, ALU.mult, t_kk.k() + t_cum.k(), ar.k())
                    TT("pool", arv[:, :, 1, :], c3(t_r), c3(t_pin), ALU.mult, t_r.k() + t_pin.k(), ar.k())
                    TT("dve", bt.ap, t_a.ap, t_inv.ap, ALU.mult, t_a.k() + t_inv.k(), bt.k())
                    TT("pool", kt.ap, t_km.ap, t_inv.ap, ALU.mult, t_km.k() + t_inv.k(), kt.k())

                def scan_gen(hp):
                    va = v_all.ap[:, hp, :]; vk = v_all.ks(hp)
                    atv4 = lambda at: at.ap.rearrange("p (h m) x -> p h m x", h=2)

                    def stage1(c, sl):
                        cs = slice(c * 64, (c + 1) * 64)
                        tk_ = tokT[sl]; at = AT[sl]; x0t = X0[sl]; g0 = G0[sl]
                        bk = bank()
                        TR(psum[0:64, bk, 0:128], va[:, cs], ident.ap, vk + KI, PK(bk))
                        TR(psum[0:64, bk, 128:256], bt.ap[:, cs], ident.ap, bt.k() + KI, PK(bk))
                        TR(psum[0:64, bk, 256:384], kt.ap[:, cs], ident.ap, kt.k() + KI, PK(bk))
                        CP("dve", tk_.ap, psum[0:64, bk, 0:384].rearrange("p (a b) -> p a b", b=128), PK(bk), tk_.k())
                        bk = bank2()
                        for h2 in range(2):
                            rows = slice(h2 * 64, h2 * 64 + 64)
                            rhs = arv[rows, c, :, :].rearrange("p a b -> p (a b)")
                            MM(psum[0:64, bk + h2, 0:128], bt.ap[rows, cs], rhs, True, True, bt.k() + ar.k(), PK(bk + h2))
                            MM(psum[0:64, bk + h2, 128:256], kt.ap[rows, cs], rhs, True, True, kt.k() + ar.k(), PK(bk + h2))
                        TT("dve", at.ap.rearrange("p (h m) x -> p h (m x)", h=2), psum[0:64, bk:bk + 2, 0:256],
                           maskU.ap.rearrange("p (h m) x -> p h (m x)", h=2), ALU.mult, PK(bk) + PK(bk + 1) + maskU.k(), at.k())
                        yield
                        bk = bank2()
                        for h2 in range(2):
                            rows = slice(h2 * 64, h2 * 64 + 64)
                            MM(psum[0:64, bk + h2, 0:64], arv[rows, c, 0, :], bt.ap[rows, cs], True, True, bt.k() + ar.k(), PK(bk + h2))
                        TT("dve", x0t.ap, psum[0:64, bk:bk + 2, 0:64], maskL.ap, ALU.mult, PK(bk) + PK(bk + 1) + maskL.k(), x0t.k())
                        bk = bank()
                        for h2 in range(2):
                            MM(psum[0:64, bk, h2 * 64:(h2 + 1) * 64], at.ap[:, h2 * 2 + 1, 0:64], tk_.ap[:, 0, h2 * 64:(h2 + 1) * 64], True, True, at.k() + tk_.k(), PK(bk))
                        CP("dve", g0.ap, psum[0:64, bk, 0:128].rearrange("p (a b) -> p a b", b=64), PK(bk), g0.k())
                        tt_cur = Tt[sl][0]
                        TT("pool", tt_cur.ap, atv4(at)[:, :, 0, 0:64], identT.ap, ALU.add, at.k() + identT.k(), tt_cur.k())
                        yield
                        Xc = x0t.ap; Xtc = atv4(at)[:, :, 0, 0:64]
                        xck = x0t.k(); xtk = at.k()
                        for lvl in range(1, 6):
                            xn = Xn[sl][lvl % 2]
                            bk = bank()
                            for h2 in range(2):
                                MM(psum[0:64, bk, h2 * 64:(h2 + 1) * 64], Xtc[:, h2, :], Xc[:, h2, :], True, True, xck + xtk, PK(bk))
                                if lvl < 5:
                                    MM(psum[0:64, bk, 128 + h2 * 64:128 + (h2 + 1) * 64], Xc[:, h2, :], Xtc[:, h2, :], True, True, xck + xtk, PK(bk))
                            ncol = 256 if lvl < 5 else 128
                            CP("dve", xn.ap.rearrange("p a b -> p (a b)")[:, 0:ncol], psum[0:64, bk, 0:ncol], PK(bk), xn.k())
                            yield
                            Xc = xn.ap[:, 0:2, :]; Xtc = xn.ap[:, 2:4, :]; xck = xn.k(); xtk = xn.k()
                            bk = bank()
                            for h2 in range(2):
                                MM(psum[0:64, bk, h2 * 64:(h2 + 1) * 64], Xc[:, h2, :], tt_cur.ap[:, h2, :], True, True, xck + tt_cur.k(), PK(bk))
                            tt_new = Tt[sl][lvl % 2]
                            TT("dve", tt_new.ap, psum[0:64, bk, 0:128].rearrange("p (a b) -> p a b", b=64), tt_cur.ap, ALU.add, PK(bk) + tt_cur.k(), tt_new.k())
                            tt_cur = tt_new
                            yield

                    def stage2(c, sl):
                        tk_ = tokT[sl]; at = AT[sl]; g0 = G0[sl]; tt_cur = Tt[sl][1]
                        sk = Sst.ks(hp)
                        bk = bank2()
                        for h2 in range(2):
                            rows = slice(h2 * 64, h2 * 64 + 64)
                            MM(psum[0:64, bk + h2, 0:64], arv[rows, c, 0, :], Sst.ap[rows, hp, :], True, True, ar.k() + sk, PK(bk + h2))
                        TT("dve", Wsb.ap, psum[0:64, bk:bk + 2, 0:64], g0.ap, ALU.add, PK(bk) + PK(bk + 1) + g0.k(), Wsb.k())
                        yield
                        bk = bank()
                        for h2 in range(2):
                            MM(psum[0:64, bk, h2 * 64:(h2 + 1) * 64], tt_cur.ap[:, h2, :], Wsb.ap[:, h2, :], True, True, tt_cur.k() + Wsb.k(), PK(bk))
                        CP("dve", Usb.ap, psum[0:64, bk, 0:128].rearrange("p (a b) -> p a b", b=64), PK(bk), Usb.k())
                        TS("pool", Stmp.ap, Sst.ap[:, hp, :], pC.ap[:, c:c + 1], ALU.mult, sk + pC.k(), Stmp.k())
                        yield
                        for h2 in range(2):
                            rows = slice(h2 * 64, h2 * 64 + 64)
                            yo = psum[rows, 6, c * 64:(c + 1) * 64]
                            if h2 == 0:
                                MM(yo, Sst.ap[rows, hp, :], arv[rows, c, 1, :], True, False, sk + ar.k(), PK(6))
                            else:
                                MM(psum[rows, 7, c * 64:(c + 1) * 64], Sst.ap[rows, hp, :], arv[rows, c, 1, :], True, True, sk + ar.k(), PK(7))
                            MM(yo, Usb.ap[:, h2, :], at.ap[:, h2 * 2, 64:128], h2 == 1, False, Usb.k() + at.k(), PK(6))
                            MM(yo, tk_.ap[:, 0, h2 * 64:(h2 + 1) * 64], at.ap[:, h2 * 2 + 1, 64:128], False, True, tk_.k() + at.k(), PK(6))
                        bk = bank()
                        for h2 in range(2):
                            rows = slice(h2 * 64, h2 * 64 + 64)
                            so = psum[rows, bk, 0:64]
                            MM(so, tk_.ap[:, 1, h2 * 64:(h2 + 1) * 64], Usb.ap[:, h2, :], True, False, tk_.k() + Usb.k(), PK(bk))
                            MM(so, tk_.ap[:, 2, h2 * 64:(h2 + 1) * 64], tk_.ap[:, 0, h2 * 64:(h2 + 1) * 64], False, True, tk_.k(), PK(bk))
                        STT("dve", Sst.ap[:, hp, :], psum[:, bk, 0:64], pC.ap[:, c:c + 1], Stmp.ap, ALU.mult, ALU.add, PK(bk) + pC.k() + Stmp.k(), sk)
                        yield


                    s1_next = 0; s1_active = []; s1_done = set(); s2_c = 0; s2_gen = None; s2_done = 0
                    while s2_done < 8:
                        while len(s1_active) < 3 and s1_next < 8 and s1_next - s2_done < NSL:
                            s1_active.append((s1_next, stage1(s1_next, s1_next % NSL)))
                            s1_next += 1
                        for item in list(s1_active):
                            cc_, g_ = item
                            try:
                                next(g_)
                            except StopIteration:
                                s1_active.remove(item)
                                s1_done.add(cc_)
                        if s2_gen is None and s2_c < 8 and s2_c in s1_done:
                            s2_gen = stage2(s2_c, s2_c % NSL)
                        if s2_gen is not None:
                            try:
                                next(s2_gen)
                            except StopIteration:
                                s2_gen = None
                                s2_c += 1
                                s2_done += 1
                        yield

                def fin_head(hp):
                    CP("dve", yT.ap, psum[:, 6, :], PK(6), yT.k())
                    TT("dve", yT.ap[64:128, :], psum[64:128, 7, :], yT.ap[64:128, :], ALU.add, PK(7) + yT.k(), yT.k())

                def fin_gen(hp):
                    t_bon = t_bons[hp % 2]
                    bk = bank()
                    MM(psum[:, bk, :], onesbd.ap, yT.ap, True, True, onesbd.k() + yT.k(), PK(bk))
                    STT("dve", yT.ap, psum[:, bk, :], -1.0 / 64, yT.ap, ALU.mult, ALU.add, PK(bk) + yT.k(), yT.k())
                    TT("pool", gnt.ap, yT.ap, yT.ap, ALU.mult, yT.k(), gnt.k())
                    yield
                    bk = bank()
                    MM(psum[:, bk, :], onesbd.ap, gnt.ap, True, True, onesbd.k() + gnt.k(), PK(bk))
                    TS("dve", gnt.ap, psum[:, bk, :], 1.0 / 64, ALU.mult, PK(bk), gnt.k(), s2=GN_EPS, op1=ALU.add)
                    ACT(gnt.ap, gnt.ap, AF.Sqrt, gnt.k(), gnt.k())
                    yield
                    RECIP(gnt.ap, gnt.ap, gnt.k(), gnt.k())
                    TT("dve", yT.ap, yT.ap, gnt.ap, ALU.mult, yT.k() + gnt.k(), yT.k())
                    TS("dve", yT.ap, yT.ap, pp("gn_g", hp), ALU.mult, yT.k() + prm.k(), yT.k(), s2=pp("gn_b", hp), op1=ALU.add)
                    TT("pool", yT.ap, yT.ap, t_bon.ap, ALU.add, yT.k() + t_bon.k(), yT.k())
                    yield
                    bk = bank()
                    MM(psum[:, bk, :], lora.ap[:, 1, hp * 128:(hp + 1) * 128], tgd1.ap, True, False, lora.ks(1) + tgd1.k(), PK(bk))
                    MM(psum[:, bk, :], lora.ap[0:32, 2, hp * 128:(hp + 1) * 128], tgd2.ap, False, True, lora.ks(2) + tgd2.k(), PK(bk))
                    TT("dve", yBT.ap[:, hp, :], psum[:, bk, :], yT.ap, ALU.mult, PK(bk) + yT.k(), yBT.ks(hp))
                    yield

                for _ in prep1(0):
                    pass
                prep2(0)
                pend_fin = None
                for hp in range(4):
                    gens = [scan_gen(hp)]
                    if pend_fin is not None:
                        gens.append(pend_fin)
                    if hp + 1 < 4:
                        gens.append(prep1(hp + 1))
                    interleave(gens, 3)
                    fin_head(hp)
                    pend_fin = fin_gen(hp)
                    if hp + 1 < 4:
                        prep2(hp + 1)
                for _ in pend_fin:
                    pass

                for half in range(2):
                    t = wbuf()
                    mhv = t.ap.rearrange("p a b -> p (a b)").rearrange("p (w k n) -> p w k n", w=2, k=4)
                    cast_load(mhv[:, 0], w_a_h[l][:, half * 512:(half + 1) * 512].rearrange("(k p) n -> p k n", p=128), t.k())
                    cast_load(mhv[:, 1], w_b_h[l][:, half * 512:(half + 1) * 512].rearrange("(k p) n -> p k n", p=128), t.k())
                    mhk = t.k()
                    wga, wgak = wload(wv[:, :, 3360 + half * 512:3360 + (half + 1) * 512], [KC, 512])
                    wgb, wgbk = wload(wv[:, :, 4384 + half * 512:4384 + (half + 1) * 512], [KC, 512])
                    for cc in range(4):
                        c = half * 4 + cc
                        bka = bank()
                        for k in range(4):
                            MM(psum[:, bka, :], mhv[:, 0, k, cc * 128:(cc + 1) * 128], yAT.ap[:, k, :], k == 0, k == 3, mhk + yAT.ks(k), PK(bka))
                        bkg = proj_fm(wga, wgak, cc * 128)
                        ACT(sga.ap, psum[:, bkg, :], AF.Sigmoid, PK(bkg), sga.k())
                        TT("dve", m1.ap, psum[:, bka, :], sga.ap, ALU.mult, PK(bka) + sga.k(), m1.k())
                        bkb = bank()
                        for k in range(4):
                            MM(psum[:, bkb, :], mhv[:, 1, k, cc * 128:(cc + 1) * 128], yBT.ap[:, k, :], k == 0, k == 3, mhk + yBT.ks(k), PK(bkb))
                        bkg = proj_fm(wgb, wgbk, cc * 128)
                        ACT(sgb.ap, psum[:, bkg, :], AF.Sigmoid, PK(bkg), sgb.k())
                        TT("dve", m2.ap, psum[:, bkb, :], sgb.ap, ALU.mult, PK(bkb) + sgb.k(), m2.k())
                        TT("pool", zT.ap[:, c, :], m1.ap, m2.ap, ALU.add, m1.k() + m2.k(), zT.ks(c))
                wo = []
                for kh in range(2):
                    wo.append(wload(w_out_h[l][kh * 512:(kh + 1) * 512, :].rearrange("(k p) n -> p k n", p=128), [4, 1024]))
                def ln1_tile(tt):
                    ti = b * 4 + tt
                    hbuf = hbufs[tt % 2]; x0 = x0s[tt % 2]; x1fm = x1fms[tt % 2]
                    st6 = st6s[tt % 2]; mv4 = mv4s[tt % 2]; lg = lgs[tt % 2]; rt = rts[tt % 2]
                    DMA("sp", x0.ap, xs_d[ti * 128:(ti + 1) * 128, :], [("xs", ti)], x0.k())
                    for nh in range(2):
                        bk = bank()
                        for k in range(KC):
                            wvv, wkk = wo[k // 4]
                            MM(psum[:, bk, :], zT.ap[:, k, tt * 128:(tt + 1) * 128], wvv[:, k % 4, nh * 512:(nh + 1) * 512], k == 0, k == KC - 1,
                               zT.ks(k) + wkk, PK(bk))
                        STT("dve", hbuf.ap[:, nh * 512:(nh + 1) * 512], x0.ap[:, nh * 512:(nh + 1) * 512], ALPHA, psum[:, bk, :], ALU.mult, ALU.add,
                            x0.k() + PK(bk), hbuf.k())
                    yield
                    yield from layer_norm_g(hbuf.ap, hbuf.k(), hbuf.ap, hbuf.k(), st6, mv4)
                    DMA("sp", xs_d[ti * 128:(ti + 1) * 128, :], hbuf.ap, hbuf.k(), [("xs", ti)])
                    to_feature_major(hbuf.ap, hbuf.k(), ti * 128, f32_out=x1fm)
                    yield
                    yield from router_g(l, ti, x1fm, lg, rt)

                interleave([ln1_tile(tt) for tt in range(4)], 2)

        def router_g(l, ti, x1fm, lg, rt):
            bk = bank()
            for k in range(KC):
                MM(psum[:, bk, 0:36], x1fm.ap[:, k, :], rw.ap[:, k, :], k == 0, k == KC - 1, x1fm.ks(k) + rw.k(), PK(bk))
            TT("dve", lg.ap, psum[:, bk, 0:36], rb.ap, ALU.add, PK(bk) + rb.k(), lg.k())
            K = lg.k() + rt.k()
            r = rt.ap
            g4 = lg.ap[:, 0:4]
            P.add("dve", lambda e: e.reduce_max(out=r[:, 0:1], in_=g4, axis=mybir.AxisListType.X), K, K)
            TS("dve", r[:, 4:8], g4, r[:, 0:1], ALU.subtract, K, K)
            ACT(r[:, 4:8], r[:, 4:8], AF.Exp, K, K)
            P.add("dve", lambda e: e.reduce_sum(out=r[:, 1:2], in_=r[:, 4:8], axis=mybir.AxisListType.X), K, K)
            RECIP(r[:, 1:2], r[:, 1:2], K, K)
            yield
            TS("dve", r[:, 8:12], g4, r[:, 0:1], ALU.is_ge, K, K)
            ev = lg.ap[:, 4:36].rearrange("p (g e) -> p g e", e=8)
            TT("dve", r[:, 32:64].rearrange("p (g e) -> p g e", e=8), ev, r[:, 8:12].rearrange("p (g o) -> p g o", o=1).to_broadcast([128, 4, 8]),
               ALU.mult, K, K)
            P.add("dve", lambda e: e.tensor_reduce(out=r[:, 12:20], in_=r[:, 32:64].rearrange("p (g e) -> p e g", e=8), axis=mybir.AxisListType.X,
                                                   op=ALU.add), K, K)
            sel = r[:, 12:20]
            P.add("dve", lambda e: e.reduce_max(out=r[:, 2:3], in_=sel, axis=mybir.AxisListType.X), K, K)
            TS("dve", r[:, 20:28], sel, r[:, 2:3], ALU.is_ge, K, K)
            STT("dve", r[:, 32:40], r[:, 20:28], -1e30, sel, ALU.mult, ALU.add, K, K)
            P.add("dve", lambda e: e.reduce_max(out=r[:, 3:4], in_=r[:, 32:40], axis=mybir.AxisListType.X), K, K)
            TS("dve", r[:, 40:48], sel, r[:, 3:4], ALU.is_ge, K, K)
            yield
            TT("dve", r[:, 48:49], r[:, 3:4], r[:, 2:3], ALU.subtract, K, K)
            ACT(r[:, 48:49], r[:, 48:49], AF.Exp, K, K)
            TS("dve", r[:, 48:49], r[:, 48:49], 1.0, ALU.add, K, K)
            RECIP(r[:, 49:50], r[:, 48:49], K, K)
            yield
            TS("dve", r[:, 50:51], r[:, 49:50], -1.0, ALU.mult, K, K, s2=1.0, op1=ALU.add)
            TT("dve", r[:, 49:51], r[:, 49:51], r[:, 1:2].to_broadcast([128, 2]), ALU.mult, K, K)
            TT("dve", r[:, 40:48], r[:, 40:48], r[:, 20:28], ALU.subtract, K, K)
            TS("dve", r[:, 40:48], r[:, 40:48], r[:, 50:51], ALU.mult, K, K)
            STT("dve", r[:, 40:48], r[:, 20:28], r[:, 49:50], r[:, 40:48], ALU.mult, ALU.add, K, K)
            cv = comb.ap[:, ti, :].rearrange("p (g e) -> p g e", e=8)
            TT("dve", cv, r[:, 8:12].rearrange("p (g o) -> p g o", o=1).to_broadcast([128, 4, 8]),
               r[:, 40:48].rearrange("p (o e) -> p o e", o=1).to_broadcast([128, 4, 8]), ALU.mult, K, comb.ks(ti))
            yield

        def phaseB(s, l, last):
            A.cur = mark_common
            acc = A.alloc([NT, D])
            xT = A.alloc([KC, S], BF16)
            for c in range(KC):
                DMA("sp", xT.ap[:, c, :], xT_d[c], [("xTd", i) for i in range(NT)], xT.ks(c))
            hT = [A.alloc([2, 512], BF16) for _ in range(2)]
            sil = [[A.alloc([512], BF16) for _ in range(2)] for _ in range(2)]
            x1s = [A.alloc([D]) for _ in range(2)]; st6s = [A.alloc([2, 6]) for _ in range(2)]; mv4s = [A.alloc([4]) for _ in range(2)]
            pts = [A.alloc([256]) for _ in range(2)]; pTs = [A.alloc([2, 128], BF16) for _ in range(2)]
            gsigs = [A.alloc([D]) for _ in range(2)]; lnp3 = A.alloc([2, D])
            assert Sst.off + Sst.n - lora.off >= 2048
            stgB = [Tile(A.ap, lora.off + i * 1024, 1024, F32, [1024]) for i in range(2)]
            sbc = [0]

            def stage_cast(dst, src, dkeys):
                a, b_ = src.shape[1], src.shape[2]
                step = max(1, 1024 // b_)
                for a0 in range(0, a, step):
                    a1 = min(a, a0 + step)
                    i = sbc[0]; sbc[0] += 1
                    st_ = stgB[i % 2]
                    sv = st_.ap[:, 0:(a1 - a0) * b_].rearrange("p (a b) -> p a b", b=b_)
                    DMA("sp", sv, src[:, a0:a1, :], (), st_.k())
                    CP("pool", dst[:, a0:a1, :], sv, st_.k(), dkeys)
            print("phaseB arena words", A.cur, "of", A.nwords)
            def load_expert(e):
                t = wbuf()
                gu = t.ap
                stage_cast(gu[:, :, 0:256], exp_gate[l, e].rearrange("(k p) n -> p k n", p=128), t.k())
                stage_cast(gu[:, :, 256:512], exp_up[l, e].rearrange("(k p) n -> p k n", p=128), t.k())
                t2 = wbuf()
                wd = t2.ap.rearrange("p a b -> p (a b)")[:, 0:2048].rearrange("p (k n) -> p k n", k=2)
                stage_cast(wd, exp_down[l, e].rearrange("(k p) n -> p k n", p=128), t2.k())
                return gu, t.k(), wd, t2.k()

            bctr = [0]

            def bankB():
                b_ = bctr[0] % 8
                bctr[0] += 1
                return b_

            def GU(i, e, b, w):
                gu, guk, wd, wdk = w
                t0 = b * 512
                xk = []
                for c in range(KC):
                    xk += xT.k(c * S + t0, c * S + t0 + 512)
                ht = hT[i % 2]
                for dc in range(2):
                    bg = bankB()
                    for k in range(KC):
                        MM(psum[:, bg, :], gu[:, k, dc * 128:(dc + 1) * 128], xT.ap[:, k, t0:t0 + 512], k == 0, k == KC - 1, guk + xk, PK(bg))
                    bu = bankB()
                    for k in range(KC):
                        MM(psum[:, bu, :], gu[:, k, 256 + dc * 128:256 + (dc + 1) * 128], xT.ap[:, k, t0:t0 + 512], k == 0, k == KC - 1, guk + xk, PK(bu))
                    sl = sil[i % 2][dc]
                    ACT(sl.ap, psum[:, bg, :], AF.Silu, PK(bg), sl.k())
                    TT("dve", ht.ap[:, dc, :], psum[:, bu, :], sl.ap, ALU.mult, PK(bu) + sl.k(), ht.ks(dc))

            def DN(i, e, b, w):
                gu, guk, wd, wdk = w
                ht = hT[i % 2]
                for tt in range(4):
                    ti = b * 4 + tt
                    for nh in range(2):
                        bk = bankB()
                        for k in range(2):
                            MM(psum[:, bk, :], ht.ap[:, k, tt * 128:(tt + 1) * 128], wd[:, k, nh * 512:(nh + 1) * 512], k == 0, k == 1, ht.ks(k) + wdk, PK(bk))
                        av = acc.ap[:, ti, nh * 512:(nh + 1) * 512]
                        ak = acc.k(ti * D + nh * 512, ti * D + (nh + 1) * 512)
                        if e == 0:
                            TS("dve", av, psum[:, bk, :], comb.ap[:, ti, e:e + 1], ALU.mult, PK(bk) + comb.ks(ti), ak)
                        else:
                            STT("dve", av, psum[:, bk, :], comb.ap[:, ti, e:e + 1], av, ALU.mult, ALU.add, PK(bk) + comb.ks(ti) + ak, ak)

            items = [(e, b) for e in range(32) for b in range(NB)]
            wts = {0: load_expert(0), 1: load_expert(1)}
            GU(0, 0, 0, wts[0])
            for i, (e, b) in enumerate(items):
                if i + 1 < len(items):
                    e2, b2 = items[i + 1]
                    GU(i + 1, e2, b2, wts[e2])
                DN(i, e, b, wts[e])
                if b == NB - 1:
                    del wts[e]
                    if e + 2 < 32:
                        wts[e + 2] = load_expert(e + 2)
            wpg = []
            for kh in range(2):
                wpg.append(wload(ple_gate_h[l][kh * 512:(kh + 1) * 512, :].rearrange("(k p) n -> p k n", p=128), [4, 1024]))
            wpp, wppk = wload(ple_proj_h[l].rearrange("(k p) n -> p k n", p=128), [2, 1024])
            load_ln(ln_g[l, 1], ln_b[l, 1])
            load_ln(ln_g[l, 2], ln_b[l, 2], lnp3)
            def ep_tile(ti):
                hk = acc.ks(ti)
                h = acc.ap[:, ti, :]
                x1 = x1s[ti % 2]; st6 = st6s[ti % 2]; mv4 = mv4s[ti % 2]; pt = pts[ti % 2]; pT = pTs[ti % 2]; gsig = gsigs[ti % 2]
                DMA("sp", x1.ap, xs_d[ti * 128:(ti + 1) * 128, :], [("xs", ti)], x1.k())
                DMA("sp", pt.ap, p_d[l, s, ti * 128:(ti + 1) * 128, :], (), pt.k())
                STT("dve", h, x1.ap, ALPHA, h, ALU.mult, ALU.add, x1.k() + hk, hk)
                yield from layer_norm_g(h, hk, h, hk, st6, mv4)
                to_feature_major(h, hk, ti * 128, sb=(xT, S, ti * 128), to_dram=False)
                bk = bank()
                for c in range(2):
                    TR(psum[:, bk, c * 128:(c + 1) * 128], pt.ap[:, c * 128:(c + 1) * 128], ident.ap, pt.k() + KI, PK(bk))
                CP("dve", pT.ap, psum[:, bk, 0:256].rearrange("p (c t) -> p c t", t=128), PK(bk), pT.k())
                yield
                xk = []
                for c in range(KC):
                    xk += xT.k(c * S + ti * 128, c * S + ti * 128 + 128)
                for nh in range(2):
                    bk = bank()
                    for k in range(KC):
                        wvv, wkk = wpg[k // 4]
                        MM(psum[:, bk, :], xT.ap[:, k, ti * 128:(ti + 1) * 128], wvv[:, k % 4, nh * 512:(nh + 1) * 512], k == 0, k == KC - 1, xk + wkk, PK(bk))
                    ACT(gsig.ap[:, nh * 512:(nh + 1) * 512], psum[:, bk, :], AF.Sigmoid, PK(bk), gsig.k())
                    bk = bank()
                    for k in range(2):
                        MM(psum[:, bk, :], pT.ap[:, k, :], wpp[:, k, nh * 512:(nh + 1) * 512], k == 0, k == 1, pT.k() + wppk, PK(bk))
                    TT("dve", gsig.ap[:, nh * 512:(nh + 1) * 512], psum[:, bk, :], gsig.ap[:, nh * 512:(nh + 1) * 512], ALU.mult, PK(bk) + gsig.k(), gsig.k())
                    yield
                STT("dve", h, h, ALPHA, gsig.ap, ALU.mult, ALU.add, hk + gsig.k(), hk)
                yield from layer_norm_g(h, hk, h, hk, st6, mv4, lnp3)
                if last:
                    DMA("sp", y_d[s, ti * 128:(ti + 1) * 128, :], h, hk, [("y", s, ti)])
                else:
                    DMA("sp", xs_d[ti * 128:(ti + 1) * 128, :], h, hk, [("xs", ti)])
                    to_feature_major(h, hk, ti * 128)
                yield

            interleave([ep_tile(ti) for ti in range(NT)], 2)

        for s in range(NSEQ if STOP != "c" else 0):
            A.cur = mark_common
            load_ln(ln_in_g, ln_in_b)
            p0_x = [A.alloc([D]) for _ in range(2)]
            p0_st = A.alloc([2, 6]); p0_mv = A.alloc([4])
            for i in range(NT):
                xt = p0_x[i % 2]
                DMA("sp", xt.ap, x_d[s, i * 128:(i + 1) * 128, :], (), xt.k())
                if BIS != 1:
                    layer_norm(xt.ap, xt.k(), xt.ap, xt.k(), p0_st, p0_mv)
                DMA("sp", xs_d[i * 128:(i + 1) * 128, :], xt.ap, xt.k(), [("xs", i)])
                if BIS not in (1, 2):
                    to_feature_major(xt.ap, xt.k(), i * 128)
            if s == 0:
                precast()
                cast_load(biasT.ap, bias_h.rearrange("h j k q -> k (h j) q"), biasT.k())
            if STOP == "p0":
                break
            for l in range(DEPTH):
                phaseA(s, l)
                if STOP == "A":
                    break
                phaseB(s, l, l == DEPTH - 1)
                if STOP == "B":
                    break
            if STOP:
                break
        P.emit()
    return nc


REL_CLIP = 128
_NC_CACHE = {}


def _bias_table(rel_bias):
    k = np.arange(128)[:, None]
    q = np.arange(128)[None, :]
    tabs = []
    for j in range(5):
        dist = q - k + 128 * (4 - j)
        idx = np.clip(dist, -REL_CLIP, REL_CLIP) + REL_CLIP
        t = rel_bias[:, idx]
        kc = (k // 64) + 2 * (j - 4)
        qc = q // 64
        valid = (kc <= qc) & (kc >= qc - 8)
        t = np.where(valid[None], t, np.float32(-1e30))
        tabs.append(t)
    return np.ascontiguousarray(np.stack(tabs, axis=1).astype(np.float32))


def prep_inputs(inputs, S, NSEQ, ncores):
    f = lambda a: np.ascontiguousarray(np.asarray(a, dtype=np.float32))
    shared = {
        "ln_in_g": f(inputs["ln_in_g"]), "ln_in_b": f(inputs["ln_in_b"]),
        "bias_tab": _bias_table(f(inputs["rel_bias"])),
        "w_in": f(inputs["w_in"]), "tok_mix": f(inputs["tok_mix"]),
        "decay_base": f(inputs["decay_base"]), "decay_up": f(inputs["decay_up"]),
        "aaa_base": f(inputs["aaa_base"]), "aaa_up": f(inputs["aaa_up"]), "gate_up": f(inputs["gate_up"]),
        "k_k": f(inputs["k_k"]), "k_a": f(inputs["k_a"]), "r_k": f(inputs["r_k"]).reshape(-1, 512),
        "vres_base": f(inputs["vres_base"]), "vres_down": f(inputs["vres_down"]), "vres_up": f(inputs["vres_up"]),
        "gn_g": f(inputs["gn_g"]), "gn_b": f(inputs["gn_b"]),
        "w_branch_attn": f(inputs["w_branch_attn"]), "w_branch_rwkv": f(inputs["w_branch_rwkv"]), "w_out": f(inputs["w_out"]),
        "router_w": np.ascontiguousarray(np.concatenate([f(inputs["router_grp"]), f(inputs["router_exp"])], axis=-1)),
        "router_b": np.ascontiguousarray(np.concatenate([f(inputs["router_grp_bias"]), f(inputs["router_exp_bias"])], axis=-1)),
        "exp_gate": f(inputs["exp_gate"]), "exp_up": f(inputs["exp_up"]), "exp_down": f(inputs["exp_down"]),
        "ple_proj": f(inputs["ple_proj"]), "ple_gate": f(inputs["ple_gate"]),
        "ln_g": f(inputs["ln_g"]), "ln_b": f(inputs["ln_b"]),
    }
    x = f(inputs["x"]); p = f(inputs["p"])
    maps = []
    for c in range(ncores):
        m = dict(shared)
        m["x"] = np.ascontiguousarray(x[c * NSEQ:(c + 1) * NSEQ])
        m["p"] = np.ascontiguousarray(p[:, c * NSEQ:(c + 1) * NSEQ])
        maps.append(m)
    return maps


def kernel(**inputs):
    x = np.asarray(inputs["x"])
    B, S, _ = x.shape
    ncores = 8
    NSEQ = B // ncores
    key = (S, NSEQ)
    if key not in _NC_CACHE:
        _NC_CACHE[key] = build_nc(S, NSEQ)
    nc = _NC_CACHE[key]
    maps = prep_inputs(inputs, S, NSEQ, ncores)
    res = run_bass_kernel_spmd(nc, maps, core_ids=list(range(ncores)))
    return np.concatenate([np.asarray(r["y"], dtype=np.float32) for r in res.results], axis=0)
```
